# Optimizing a Trainium2 kernel written in Bass

```python
import numpy as np
import jax
import jax.numpy as jnp
from jax import lax

D_MODEL = 4096
BATCH = 8
SEQ = 2048
DEPTH = 1

HEAD_DIM = 128
MIX_WIDTH = D_MODEL
POOL_WINDOWS = (2, 4, 8, 16)
POOL_WIDTH = MIX_WIDTH // 4
POOL_GROUP = POOL_WIDTH // len(POOL_WINDOWS)
NSA_WIDTH = MIX_WIDTH - POOL_WIDTH
NSA_HEADS = NSA_WIDTH // HEAD_DIM
NSA_GROUP = 6
NSA_KV_HEADS = NSA_HEADS // NSA_GROUP
N_BRANCH = 3
KV_WIDTH = N_BRANCH * 2 * NSA_KV_HEADS * HEAD_DIM
GATE_WIDTH = NSA_HEADS * N_BRANCH
IN_WIDTH = POOL_WIDTH + NSA_WIDTH + KV_WIDTH + GATE_WIDTH
CMP_BLOCK = 32
CMP_STRIDE = 16
CMP_HIDDEN = 256
SEL_BLOCK = 64
SEL_TOPN = 16
WINDOW = 512
Q_BLOCK = 16
FORCE_BONUS = 1000.0
NEG_SCORE = -1e9
ROPE_THETA = 10000.0
PEER_HEADS = 8
PEER_KEY_DIM = 256
PEER_NKEYS = 128
PEER_EXPERTS = PEER_NKEYS * PEER_NKEYS
PEER_TOPK = 16
PEER_CHUNK = 64
NORM_EPS = 1e-6
N_MOD = 6

kernel_name = 'hybrid_pool_nsa_peer_block'


def rmsnorm(x, g):
    xf = x.astype(jnp.float32)
    y = xf * lax.rsqrt(jnp.mean(xf * xf, axis=-1, keepdims=True) + NORM_EPS)
    return (y * g.astype(jnp.float32)).astype(x.dtype)


def rope(x, pos):
    half = HEAD_DIM // 2
    inv = ROPE_THETA ** (-jnp.arange(half, dtype=jnp.float32) / half)
    ang = pos[:, None] * inv[None, :]
    cos = jnp.cos(ang)[:, None, :]
    sin = jnp.sin(ang)[:, None, :]
    xf = x.astype(jnp.float32)
    x1, x2 = xf[..., :half], xf[..., half:]
    return jnp.concatenate([x1 * cos - x2 * sin, x2 * cos + x1 * sin], axis=-1).astype(x.dtype)


def masked_softmax(s, mask):
    s = jnp.where(mask, s.astype(jnp.float32), -jnp.inf)
    m = jnp.max(s, axis=-1, keepdims=True)
    m = jnp.where(jnp.isfinite(m), m, 0.0)
    p = jnp.exp(s - m)
    den = jnp.sum(p, axis=-1, keepdims=True)
    return p / jnp.where(den > 0, den, 1.0)


def pool_mixer(p, w_pool, pool_scale):
    B_, T_, _ = p.shape
    pg = p.reshape(B_, T_, len(POOL_WINDOWS), POOL_GROUP).astype(jnp.float32)
    cs = jnp.cumsum(pg, axis=1)
    count = jnp.arange(1, T_ + 1, dtype=jnp.float32)[None, :, None]
    groups = []
    for gi, w in enumerate(POOL_WINDOWS):
        c_g = cs[:, :, gi]
        lag = jnp.pad(c_g, ((0, 0), (w, 0), (0, 0)))[:, :T_]
        groups.append((c_g - lag) / jnp.minimum(count, float(w)) - pg[:, :, gi])
    d = jnp.stack(groups, axis=2).astype(p.dtype)
    y = jnp.einsum('btgc,gcd->btgd', d, w_pool)
    return y.reshape(B_, T_, POOL_WIDTH) * pool_scale


def compress_blocks(k_raw, blk, pe, w1, w2):
    kb = k_raw[:, blk] + pe[None, None, :, None, :]
    hid = jax.nn.gelu(jnp.einsum('bnlgd,ldh->bngh', kb, w1), approximate=False)
    return jnp.einsum('bngh,hd->bngd', hid, w2)


def cmp_to_sel_matrix(n_cmp, n_sel):
    cst = np.arange(n_cmp)[:, None] * CMP_STRIDE
    sst = np.arange(n_sel)[None, :] * SEL_BLOCK
    ov = np.clip(np.minimum(cst + CMP_BLOCK, sst + SEL_BLOCK) - np.maximum(cst, sst), 0, None)
    return jnp.asarray(ov / CMP_BLOCK, dtype=jnp.float32)


def nsa_mixer(q_in, kv_in, g_in, q_norm_g, k_norm_g, cmp_pe, cmp_w1, cmp_w2):
    B_, T_, _ = q_in.shape
    G, R, Dh = NSA_KV_HEADS, NSA_GROUP, HEAD_DIM
    pos = jnp.arange(T_, dtype=jnp.float32)
    qn = rmsnorm(q_in.reshape(B_, T_, NSA_HEADS, Dh), q_norm_g)
    qr = rope(qn, pos).reshape(B_, T_, G, R, Dh)
    qn = qn.reshape(B_, T_, G, R, Dh)
    kv = kv_in.reshape(B_, T_, N_BRANCH, 2, G, Dh)
    n_cmp = (T_ - CMP_BLOCK) // CMP_STRIDE + 1
    blk = np.arange(n_cmp)[:, None] * CMP_STRIDE + np.arange(CMP_BLOCK)[None, :]
    k_cmp = rmsnorm(compress_blocks(kv[:, :, 0, 0], blk, cmp_pe[0], cmp_w1[0], cmp_w2[0]), k_norm_g[0])
    v_cmp = compress_blocks(kv[:, :, 0, 1], blk, cmp_pe[1], cmp_w1[1], cmp_w2[1])
    cmp_end = jnp.arange(n_cmp) * CMP_STRIDE + (CMP_BLOCK - 1)
    n_sel = T_ // SEL_BLOCK
    top_n = min(SEL_TOPN, n_sel)
    sel_map = cmp_to_sel_matrix(n_cmp, n_sel)
    sel_ids = jnp.arange(n_sel)
    sel_start = sel_ids * SEL_BLOCK
    k_sel = rope(rmsnorm(kv[:, :, 1, 0], k_norm_g[1]), pos)
    k_sel = k_sel.reshape(B_, n_sel, SEL_BLOCK, G, Dh).transpose(0, 3, 1, 2, 4)
    v_sel = kv[:, :, 1, 1].reshape(B_, n_sel, SEL_BLOCK, G, Dh).transpose(0, 3, 1, 2, 4)
    pad = ((0, 0), (WINDOW, 0), (0, 0), (0, 0))
    k_win = jnp.pad(rope(rmsnorm(kv[:, :, 2, 0], k_norm_g[2]), pos), pad)
    v_win = jnp.pad(kv[:, :, 2, 1], pad)
    gates = jax.nn.sigmoid(g_in.astype(jnp.float32)).reshape(B_, T_, G, R, N_BRANCH)
    b_ix = jnp.arange(B_)[:, None, None, None]
    g_ix = jnp.arange(G)[None, None, :, None]
    scale = Dh ** -0.5

    def block_step(qb):
        t0 = qb * Q_BLOCK
        t = t0 + jnp.arange(Q_BLOCK)
        qn_b = lax.dynamic_slice_in_dim(qn, t0, Q_BLOCK, axis=1)
        qr_b = lax.dynamic_slice_in_dim(qr, t0, Q_BLOCK, axis=1)
        g_b = lax.dynamic_slice_in_dim(gates, t0, Q_BLOCK, axis=1)
        s_c = jnp.einsum('bqgrd,bngd->bqgrn', qn_b, k_cmp) * scale
        m_c = (cmp_end[None, :] <= t[:, None])[None, :, None, None, :]
        p_c = masked_softmax(s_c, m_c)
        o_c = jnp.einsum('bqgrn,bngd->bqgrd', p_c, v_cmp)
        imp = jnp.einsum('bqgrn,nj->bqgj', p_c, sel_map)
        cur = t // SEL_BLOCK
        forced = (sel_ids[None, :] == 0) | (sel_ids[None, :] == cur[:, None]) | (sel_ids[None, :] == cur[:, None] - 1)
        valid = sel_start[None, :] <= t[:, None]
        score = jnp.where(valid[None, :, None, :], imp + jnp.where(forced, FORCE_BONUS, 0.0)[None, :, None, :], NEG_SCORE)
        _, idx = lax.top_k(score, top_n)
        k_g = k_sel[b_ix, g_ix, idx]
        v_g = v_sel[b_ix, g_ix, idx]
        s_s = jnp.einsum('bqgrd,bqgkld->bqgrkl', qr_b, k_g) * scale
        tok = idx[..., None] * SEL_BLOCK + jnp.arange(SEL_BLOCK)
        m_s = (tok <= t[None, :, None, None, None]).reshape(B_, Q_BLOCK, G, 1, top_n * SEL_BLOCK)
        p_s = masked_softmax(s_s.reshape(B_, Q_BLOCK, G, R, top_n * SEL_BLOCK), m_s)
        o_s = jnp.einsum('bqgrn,bqgnd->bqgrd', p_s, v_g.reshape(B_, Q_BLOCK, G, top_n * SEL_BLOCK, Dh))
        k_wb = lax.dynamic_slice_in_dim(k_win, t0, WINDOW + Q_BLOCK, axis=1)
        v_wb = lax.dynamic_slice_in_dim(v_win, t0, WINDOW + Q_BLOCK, axis=1)
        s_w = jnp.einsum('bqgrd,bsgd->bqgrs', qr_b, k_wb) * scale
        spos = t0 - WINDOW + jnp.arange(WINDOW + Q_BLOCK)
        dist = t[:, None] - spos[None, :]
        m_w = ((dist >= 0) & (dist < WINDOW) & (spos[None, :] >= 0))[None, :, None, None, :]
        p_w = masked_softmax(s_w, m_w)
        o_w = jnp.einsum('bqgrs,bsgd->bqgrd', p_w, v_wb)
        o = g_b[..., 0:1] * o_c + g_b[..., 1:2] * o_s + g_b[..., 2:3] * o_w
        return o.reshape(B_, Q_BLOCK, NSA_WIDTH).astype(q_in.dtype)

    out = lax.map(block_step, jnp.arange(T_ // Q_BLOCK))
    return out.transpose(1, 0, 2, 3).reshape(B_, T_, NSA_WIDTH)


def peer_ffn(h, w_pq, peer_keys, peer_u, peer_v):
    B_, T_, D_ = h.shape
    q = jnp.dot(h, w_pq).reshape(B_, T_, PEER_HEADS, 2, PEER_KEY_DIM // 2).astype(jnp.float32)
    s = jnp.einsum('bthcd,hckd->bthck', q, peer_keys.astype(jnp.float32))
    v, i = lax.top_k(s, PEER_TOPK)
    cand_s = (v[..., 0, :, None] + v[..., 1, None, :]).reshape(B_, T_, PEER_HEADS, PEER_TOPK * PEER_TOPK)
    cand_i = (i[..., 0, :, None] * PEER_NKEYS + i[..., 1, None, :]).reshape(B_, T_, PEER_HEADS, PEER_TOPK * PEER_TOPK)
    top_s, sel = lax.top_k(cand_s, PEER_TOPK)
    e_idx = jnp.take_along_axis(cand_i, sel, axis=-1)
    g = jax.nn.softmax(top_s, axis=-1)
    n_chunks = (B_ * T_) // PEER_CHUNK
    xt = h.reshape(n_chunks, PEER_CHUNK, D_)
    et = e_idx.reshape(n_chunks, PEER_CHUNK, PEER_HEADS * PEER_TOPK)
    gt = g.reshape(n_chunks, PEER_CHUNK, PEER_HEADS * PEER_TOPK)

    def chunk(args):
        xc, ec, gc = args
        u = peer_u[ec]
        a = jax.nn.gelu(jnp.einsum('nd,nkd->nk', xc, u).astype(jnp.float32), approximate=False)
        return jnp.einsum('nk,nkd->nd', (gc * a).astype(xc.dtype), peer_v[ec])

    y = lax.map(chunk, (xt, et, gt))
    return y.reshape(B_, T_, D_)


def hybrid_layer(x, c, w_ada, b_ada, norm1_g, norm2_g, w_in, w_out, w_pool, pool_scale,
                 q_norm_g, k_norm_g, cmp_pe, cmp_w1, cmp_w2, w_pq, peer_keys, peer_u, peer_v):
    mod = jnp.dot(jax.nn.silu(c), w_ada) + b_ada
    shift1, scale1, gate1, shift2, scale2, gate2 = [m[:, None, :] for m in jnp.split(mod, N_MOD, axis=-1)]
    h = rmsnorm(x, norm1_g) * (1.0 + scale1) + shift1
    proj = jnp.dot(h, w_in)
    p_in, q_in, kv_in, g_in = jnp.split(
        proj, [POOL_WIDTH, POOL_WIDTH + NSA_WIDTH, POOL_WIDTH + NSA_WIDTH + KV_WIDTH], axis=-1)
    y_pool = pool_mixer(p_in, w_pool, pool_scale)
    y_nsa = nsa_mixer(q_in, kv_in, g_in, q_norm_g, k_norm_g, cmp_pe, cmp_w1, cmp_w2)
    x = x + gate1 * jnp.dot(jnp.concatenate([y_pool, y_nsa], axis=-1), w_out)
    h = rmsnorm(x, norm2_g) * (1.0 + scale2) + shift2
    x = x + gate2 * peer_ffn(h, w_pq, peer_keys, peer_u, peer_v)
    return x


def setup_inputs(seed: int = 0) -> dict:
    key = jax.random.key(seed)
    ks = jax.random.split(key, 19)

    def nrm(k, shape, scale):
        return jax.random.normal(k, shape, jnp.float32) * scale

    return {
        'x': nrm(ks[0], (BATCH, SEQ, D_MODEL), 1.0),
        'c': nrm(ks[1], (BATCH, D_MODEL), 1.0),
        'w_ada': nrm(ks[2], (DEPTH, D_MODEL, N_MOD * D_MODEL), D_MODEL ** -0.5),
        'b_ada': nrm(ks[3], (DEPTH, N_MOD * D_MODEL), 0.01),
        'norm1_g': 1.0 + nrm(ks[4], (DEPTH, D_MODEL), 0.01),
        'norm2_g': 1.0 + nrm(ks[5], (DEPTH, D_MODEL), 0.01),
        'w_in': nrm(ks[6], (DEPTH, D_MODEL, IN_WIDTH), D_MODEL ** -0.5),
        'w_out': nrm(ks[7], (DEPTH, MIX_WIDTH, D_MODEL), MIX_WIDTH ** -0.5),
        'w_pool': nrm(ks[8], (DEPTH, len(POOL_WINDOWS), POOL_GROUP, POOL_GROUP), POOL_GROUP ** -0.5),
        'pool_scale': 1.0 + nrm(ks[9], (DEPTH, POOL_WIDTH), 0.02),
        'q_norm_g': 1.0 + nrm(ks[10], (DEPTH, HEAD_DIM), 0.01),
        'k_norm_g': 1.0 + nrm(ks[11], (DEPTH, N_BRANCH, HEAD_DIM), 0.01),
        'cmp_pe': nrm(ks[12], (DEPTH, 2, CMP_BLOCK, HEAD_DIM), 0.02),
        'cmp_w1': nrm(ks[13], (DEPTH, 2, CMP_BLOCK, HEAD_DIM, CMP_HIDDEN), (CMP_BLOCK * HEAD_DIM) ** -0.5),
        'cmp_w2': nrm(ks[14], (DEPTH, 2, CMP_HIDDEN, HEAD_DIM), CMP_HIDDEN ** -0.5),
        'w_pq': nrm(ks[15], (DEPTH, D_MODEL, PEER_HEADS * PEER_KEY_DIM), D_MODEL ** -0.5),
        'peer_keys': nrm(ks[16], (DEPTH, PEER_HEADS, 2, PEER_NKEYS, PEER_KEY_DIM // 2), (PEER_KEY_DIM // 2) ** -0.5),
        'peer_u': nrm(ks[17], (DEPTH, PEER_EXPERTS, D_MODEL), D_MODEL ** -0.5),
        'peer_v': nrm(ks[18], (DEPTH, PEER_EXPERTS, D_MODEL), PEER_HEADS ** -0.5),
    }


def reference(x, c, w_ada, b_ada, norm1_g, norm2_g, w_in, w_out, w_pool, pool_scale,
              q_norm_g, k_norm_g, cmp_pe, cmp_w1, cmp_w2, w_pq, peer_keys, peer_u, peer_v):
    for l in range(DEPTH):
        x = hybrid_layer(x, c, w_ada[l], b_ada[l], norm1_g[l], norm2_g[l], w_in[l], w_out[l],
                         w_pool[l], pool_scale[l], q_norm_g[l], k_norm_g[l], cmp_pe[l], cmp_w1[l],
                         cmp_w2[l], w_pq[l], peer_keys[l], peer_u[l], peer_v[l])
    return x
```

```python
import contextlib
import numpy as np
import concourse.bass as bass
import concourse.mybir as mybir
from concourse.bass_utils import run_bass_kernel_spmd

F32 = mybir.dt.float32
BF16 = mybir.dt.bfloat16
I32 = mybir.dt.int32
U32 = mybir.dt.uint32
AF = mybir.ActivationFunctionType
ALU = mybir.AluOpType

T = 2048
D = 4096
KC = 32
NH = 24
G = 4
R = 6
NCMP = 127
NSEL = 32
INW = 7240
EPS = 1e-6
SCALE = 128 ** -0.5


class MK:
    NLANES = 12

    def __init__(self, nc):
        self.nc = nc
        self.es = contextlib.ExitStack()
        self.eng = {"pe": nc.tensor, "dve": nc.vector, "act": nc.scalar,
                    "pool": nc.gpsimd, "sp": nc.sync}
        self.sem = {}
        self.cnt = {}
        for n in self.eng:
            self.sem[n] = self.es.enter_context(nc.semaphore("sem_" + n))
            self.cnt[n] = 0
        self.lanes = []
        for i in range(self.NLANES):
            n = "lane%d" % i
            self.sem[n] = self.es.enter_context(nc.semaphore(n))
            self.cnt[n] = 0
            self.lanes.append(n)
        self.lane_rr = 0
        self.clanes = []
        for i in range(3):
            n = "clane%d" % i
            self.sem[n] = self.es.enter_context(nc.semaphore(n))
            self.cnt[n] = 0
            self.clanes.append(n)
        self.clane_rr = 0
        self.waited = {}
        self.last_w = {}
        self.readers = {}
        self.ninst = 0
        self.log = []

    @staticmethod
    def key(x):
        if isinstance(x, (str, tuple)):
            return x
        if hasattr(x, "tensor"):
            return x.tensor.name
        return x.name

    def _wait(self, engname, semname, val):
        if val <= 0:
            return
        k = (engname, semname)
        if self.waited.get(k, 0) >= val:
            return
        self.waited[k] = val
        self.eng[engname].wait_ge(self.sem[semname], val)
        self.ninst += 1
        self.log.append((engname, "w", semname, val))

    def _deps(self, engname, r, w):
        need = {}
        for x in r:
            lw = self.last_w.get(self.key(x))
            if lw:
                need[lw[0]] = max(need.get(lw[0], 0), lw[1])
        for x in w:
            k = self.key(x)
            lw = self.last_w.get(k)
            if lw:
                need[lw[0]] = max(need.get(lw[0], 0), lw[1])
            for (s, v) in self.readers.get(k, []):
                need[s] = max(need.get(s, 0), v)
        for s, v in need.items():
            if s == "pe" and engname == "pe":
                continue
            self._wait(engname, s, v)

    def _record(self, tok, r, w):
        for x in r:
            lst = self.readers.setdefault(self.key(x), [])
            lst[:] = [(s, v) for (s, v) in lst if s != tok[0]]
            lst.append(tok)
        for x in w:
            k = self.key(x)
            self.last_w[k] = tok
            self.readers[k] = []

    def op(self, engname, fn, r=(), w=()):
        self._deps(engname, r, w)
        inst = fn(self.eng[engname])
        self.cnt[engname] += 1
        inst.then_inc(self.sem[engname], 1)
        self.ninst += 1
        self.log.append((engname, "i", engname, 1))
        self._record((engname, self.cnt[engname]), r, w)
        return inst

    def _lane(self):
        lane = self.lanes[self.lane_rr]
        self.lane_rr = (self.lane_rr + 1) % self.NLANES
        return lane

    def dma(self, out, in_, r=(), w=(), q="sp", conv=False, **kw):
        if conv:
            lane = self.clanes[self.clane_rr]
            self.clane_rr = (self.clane_rr + 1) % len(self.clanes)
        else:
            lane = self._lane()
        self._deps(q, r, w)
        self._wait(q, lane, self.cnt[lane])
        inst = self.eng[q].dma_start(out=out, in_=in_, **kw)
        self.cnt[lane] += 16
        inst.then_inc(self.sem[lane], 16)
        self.ninst += 1
        self.log.append((q, "i", lane, 16))
        self._record((lane, self.cnt[lane]), r, w)
        return inst

    def gather(self, out, table, idx_ap, r=(), w=()):
        lane = self._lane()
        q = "pool"
        self._deps(q, r, w)
        self._wait(q, lane, self.cnt[lane])
        inst = self.nc.gpsimd.indirect_dma_start(
            out=out, out_offset=None, in_=table,
            in_offset=bass.IndirectOffsetOnAxis(ap=idx_ap, axis=0))
        self.cnt[lane] += 16
        inst.then_inc(self.sem[lane], 16)
        self.ninst += 1
        self.log.append((q, "i", lane, 16))
        self._record((lane, self.cnt[lane]), r, w)
        return inst

    def barrier(self):
        for e in self.eng:
            for s in self.sem:
                if s == e:
                    continue
                self._wait(e, s, self.cnt[s])
        self.last_w = {}
        self.readers = {}

    def finish(self, engname="sp"):
        for s in self.sem:
            self._wait(engname, s, self.cnt[s])

    def sb(self, stack, name, shape, dt):
        return stack.enter_context(self.nc.sbuf_tensor("s_" + name, shape, dt))

    def ps(self, stack, name, shape, dt=F32):
        return stack.enter_context(self.nc.psum_tensor("p_" + name, shape, dt))


def make_consts():
    c = {}
    c["ident_f"] = np.eye(128, dtype=np.float32)
    c["ones_f"] = np.ones((128, 128), np.float32)
    rot = np.zeros((128, 128), np.float32)
    for m in range(128):
        rot[(m + 64) % 128, m] = 1.0
    c["rotm"] = rot
    half = 64
    inv = (10000.0 ** (-np.arange(half, dtype=np.float32) / half)).astype(np.float32)
    pos = np.arange(T, dtype=np.float32)
    ang = (pos[:, None] * inv[None, :]).astype(np.float32)
    cos = np.cos(ang).astype(np.float32).T
    sin = np.sin(ang).astype(np.float32).T
    c["cosT"] = np.concatenate([cos, cos], 0).astype(np.float32)
    c["sinT"] = np.concatenate([-sin, sin], 0).astype(np.float32)
    n = np.arange(128)
    t = np.arange(T)
    cm = ((16 * n[:, None] + 31) <= t[None, :]) & (n[:, None] < NCMP)
    c["cmpmask"] = cm.astype(np.float32)
    cst = np.arange(NCMP)[:, None] * 16
    sst = np.arange(NSEL)[None, :] * 64
    ov = np.clip(np.minimum(cst + 32, sst + 64) - np.maximum(cst, sst), 0, None)
    sm = np.zeros((128, NSEL), np.float32)
    sm[:NCMP] = ov / 32.0
    c["selmap"] = sm
    j = np.arange(NSEL)
    cur = t // 64
    forced = (j[None, :] == 0) | (j[None, :] == cur[:, None]) | (j[None, :] == cur[:, None] - 1)
    valid = (j[None, :] * 64) <= t[:, None]
    c["tb"] = (np.where(forced, 1000.0, 0.0) + np.where(valid, 0.0, -1e9)).astype(np.float32)
    eb = np.zeros((NSEL, 16, 128), np.float32)
    for m in range(16):
        for kk in range(128):
            eb[2 * m + kk // 64, m, kk] = 1.0
    c["eb"] = eb
    kk = np.arange(128)[:, None]
    q = np.arange(512)[None, :]
    caus = np.zeros((128, 4, 512), np.float32)
    for d in range(4):
        caus[:, d, :] = ((q - kk) >= 128 * d)
    c["caus"] = caus
    wm = np.zeros((128, 8, 512), np.float32)
    for d in range(-4, 4):
        dist = q - kk - 128 * d
        wm[:, d + 4, :] = (dist >= 0) & (dist < 512)
    c["wmask"] = wm
    ic = np.zeros((128, 4, T), np.float32)
    for gi, w in enumerate((2, 4, 8, 16)):
        ic[:, gi, :] = 1.0 / np.minimum(t + 1, w).astype(np.float32)[None, :]
    c["invcnt"] = ic
    c["iota256"] = np.tile(np.arange(256, dtype=np.float32)[None, :], (128, 1))
    return c


CONST_SHAPES = {
    "ident_f": [128, 128], "ones_f": [128, 128], "rotm": [128, 128],
    "cosT": [128, T], "sinT": [128, T], "cmpmask": [128, T], "selmap": [128, NSEL],
    "tb": [T, NSEL], "eb": [NSEL, 16, 128], "caus": [128, 4, 512], "wmask": [128, 8, 512],
    "invcnt": [128, 4, T], "iota256": [128, 256],
}

INPUT_SHAPES = {
    "x": [T, D], "c_l": [128, KC], "w_ada": [D, 6 * D], "b_ada": [1, 6 * D],
    "g1_l": [128, KC], "g2_l": [128, KC], "g2_row": [1, D],
    "w_in": [D, INW], "w_out": [D, D], "w_pool": [4, 256, 256], "pscale_l": [128, 8],
    "qkg_l": [128, 4], "pe_l": [2, 128, 32], "cmp_w1": [2, 32, 128, 256], "cmp_w2": [2, 256, 128],
    "w_pq": [D, 2048], "peer_keys": [16, 128, 128], "peer_u": [16384, D], "peer_v": [16384, D],
}

SCRATCH = {
    "mod_d": ([6 * D], F32),
    "pT_d": ([1024, T], F32),
    "qn_d": ([NH, 128, T], BF16),
    "qr_d": ([NH, 128, T], BF16),
    "kc_d": ([2, G, 128, T], BF16),
    "ks_d": ([G, 128, T], BF16),
    "kw_d": ([G, 128, T], BF16),
    "vs_d": ([T, 512], BF16),
    "vw_d": ([T, 512], BF16),
    "gT_d": ([72, T], F32),
    "yT_d": ([D, T], BF16),
    "x1_d": ([T, D], F32),
    "eidx_d": ([T, 128], I32),
    "gts_d": ([T, 128], F32),
    "ef_d": ([T, 128], F32),
}
BIG_SCRATCH = {
    "u16_d": ([16384, D], BF16),
    "v16_d": ([16384, D], BF16),
    "wi16_d": ([D, INW], BF16),
    "wo16_d": ([D, D], BF16),
    "wq16_d": ([D, 2048], BF16),
}


def build(phases=("mod", "inproj", "pool", "cmp", "attn", "outproj", "peer"), debug=False,
          inputs_needed=None, ntb=4, dbg=99):
    nc = bass.Bass("TRN2", target_bir_lowering=False)
    k = MK(nc)
    es = k.es
    ins = {}
    for name, shp in list(INPUT_SHAPES.items()) + list(CONST_SHAPES.items()):
        if inputs_needed is not None and name not in inputs_needed:
            continue
        ins[name] = nc.dram_tensor(name, shp, F32, kind="ExternalInput").ap()
    out = nc.dram_tensor("out", [T, D], F32, kind="ExternalOutput").ap()
    sc = {}
    for name, (shp, dt) in SCRATCH.items():
        sc[name] = nc.dram_tensor(name, shp, dt, kind="ExternalOutput" if debug else "Internal").ap()
    for name, (shp, dt) in BIG_SCRATCH.items():
        sc[name] = nc.dram_tensor(name, shp, dt, kind="Internal").ap()
    conv_jobs = []
    if "peer" in phases:
        for i in range(32):
            rs_ = slice(i * 512, (i + 1) * 512)
            conv_jobs.append(("u16_d", "peer_u", rs_))
            conv_jobs.append(("v16_d", "peer_v", rs_))

    def conv_step(n=1):
        for _ in range(n):
            if conv_jobs:
                dn, sn, rs_ = conv_jobs.pop(0)
                k.dma(sc[dn][rs_, :], ins[sn][rs_, :], w=[dn], q="pool", conv=True)

    ident_f = k.sb(es, "ident_f_sb", [128, 128], F32)
    ident_b = k.sb(es, "ident_b_sb", [128, 128], BF16)
    ones_f = k.sb(es, "ones_f_sb", [128, 128], F32)
    ones_b = k.sb(es, "ones_b_sb", [128, 128], BF16)
    modT = k.sb(es, "modT", [128, 192], F32)
    A1 = k.sb(es, "A1", [128, KC], F32)
    A2 = k.sb(es, "A2", [128, KC], F32)
    kcmpT = k.sb(es, "kcmpT", [128, G, 128], BF16)
    vcmp = k.sb(es, "vcmp", [128, G, 128], BF16)
    PB = [k.ps(es, "bank%d" % i, [128, 512]) for i in range(8)]
    PBb = [b.bitcast(BF16) for b in PB]
    k.dma(ident_f[:], ins["ident_f"][:, :], w=[ident_f])
    k.dma(ones_f[:], ins["ones_f"][:, :], w=[ones_f])
    k.op("dve", lambda e: e.tensor_copy(out=ident_b[:], in_=ident_f[:]), r=[ident_f], w=[ident_b])
    k.op("dve", lambda e: e.tensor_copy(out=ones_b[:], in_=ones_f[:]), r=[ones_f], w=[ones_b])

    for dn, sn, nrow, step in (("wi16_d", "w_in", D, 256), ("wo16_d", "w_out", D, 512), ("wq16_d", "w_pq", D, 1024)):
        if sn in ins:
            for r0 in range(0, nrow, step):
                k.dma(sc[dn][r0:r0 + step, :], ins[sn][r0:r0 + step, :], w=[dn], q="pool", conv=True)

    if "mod" in phases:
        with contextlib.ExitStack() as st:
            cl = k.sb(st, "cl", [128, KC], F32)
            scl = k.sb(st, "scl", [128, KC], F32)
            wt = [k.sb(st, "wada%d" % i, [128, 16, 512], F32) for i in range(2)]
            brow = [k.sb(st, "brow%d" % i, [1, 512], F32) for i in range(2)]
            mrow = [k.sb(st, "mrow%d" % i, [1, 512], F32) for i in range(2)]
            psm = [PB[0], PB[1]]
            k.dma(cl[:], ins["c_l"][:, :], w=[cl])
            k.op("act", lambda e: e.activation(out=scl[:], in_=cl[:], func=AF.Silu), r=[cl], w=[scl])
            wv = ins["w_ada"].rearrange("(k p) f -> p k f", p=128)
            modv = sc["mod_d"].rearrange("(a f) -> a f", a=1)
            for fb in range(48):
                ps = psm[fb % 2]
                br = brow[fb % 2]
                mr = mrow[fb % 2]
                fs = slice(fb * 512, (fb + 1) * 512)
                k.dma(br[:], ins["b_ada"][0:1, fs], w=[br])
                for hf in range(2):
                    w_ = wt[hf]
                    k.dma(w_[:], wv[:, hf * 16:(hf + 1) * 16, fs], w=[w_],
                          q=("sp" if hf == 0 else "act"))
                    for kk in range(16):
                        kg = hf * 16 + kk
                        k.op("pe", lambda e: e.matmul(ps[0:1, :], lhsT=scl[:, kg:kg + 1], rhs=w_[:, kk, :],
                                                      start=(kg == 0), stop=(kg == 31)),
                             r=[scl, w_], w=[ps])
                k.op("dve", lambda e: e.tensor_tensor(out=mr[:], in0=ps[0:1, :], in1=br[:], op=ALU.add),
                     r=[ps, br], w=[mr])
                k.dma(modv[0:1, fs], mr[:], r=[mr], w=["mod_d"])
        k.barrier()

    with contextlib.ExitStack() as st:
        m1 = k.sb(st, "m1", [96, 128], F32)
        m2 = k.sb(st, "m2", [96, 128], F32)
        g1 = k.sb(st, "g1l", [128, KC], F32)
        g2 = k.sb(st, "g2l", [128, KC], F32)
        pst = PB[2]
        mv = sc["mod_d"].rearrange("(c p) -> c p", p=128)
        k.dma(m1[:], mv[0:96, :], r=["mod_d"], w=[m1])
        k.dma(m2[:], mv[96:192, :], r=["mod_d"], w=[m2])
        k.dma(g1[:], ins["g1_l"][:, :], w=[g1])
        k.dma(g2[:], ins["g2_l"][:, :], w=[g2])
        k.op("pe", lambda e: e.transpose(out=pst[:, 0:96], in_=m1[:], identity=ident_f[0:96, 0:96]), r=[m1, ident_f], w=[pst])
        k.op("pe", lambda e: e.transpose(out=pst[:, 96:192], in_=m2[:], identity=ident_f[0:96, 0:96]), r=[m2, ident_f], w=[pst])
        k.op("dve", lambda e: e.tensor_copy(out=modT[:], in_=pst[:, 0:192]), r=[pst], w=[modT])
        k.op("dve", lambda e: e.scalar_tensor_tensor(out=A1[:], in0=modT[:, 32:64], scalar=1.0, in1=g1[:],
                                                     op0=ALU.add, op1=ALU.mult), r=[modT, g1], w=[A1])
        k.op("dve", lambda e: e.scalar_tensor_tensor(out=A2[:], in0=modT[:, 128:160], scalar=1.0, in1=g2[:],
                                                     op0=ALU.add, op1=ALU.mult), r=[modT, g2], w=[A2])
    k.barrier()
    B1 = modT[:, 0:32]
    B2 = modT[:, 96:128]

    def hT_keys():
        return [("hT", kk, tt) for kk in range(KC) for tt in range(4)]

    def norm_block(st_bufs, src, tb, Atab, Btab, hT):
        xt, xn, junk, ss, pT = st_bufs
        for tt in range(4):
            t0 = tb * 512 + tt * 128
            k.dma(xt[:], src[t0:t0 + 128, :], w=[xt])
            k.op("dve", lambda e: e.memset(ss[:], 0.0), w=[ss])
            k.op("act", lambda e: e.activation(out=junk[:], in_=xt[:], func=AF.Square, accum_out=ss[:, 0:1]),
                 r=[xt, ss], w=[junk, ss])
            k.op("dve", lambda e: e.tensor_scalar(out=ss[:, 1:2], in0=ss[:, 0:1], scalar1=1.0 / D, scalar2=EPS,
                                                  op0=ALU.mult, op1=ALU.add), r=[ss], w=[ss])
            k.op("act", lambda e: e.activation(out=ss[:, 3:4], in_=ss[:, 1:2], func=AF.Sqrt), r=[ss], w=[ss])
            k.op("dve", lambda e: e.reciprocal(out=ss[:, 2:3], in_=ss[:, 3:4]), r=[ss], w=[ss])
            k.op("dve", lambda e: e.tensor_scalar(out=xn[:], in0=xt[:], scalar1=ss[:, 2:3], scalar2=None,
                                                  op0=ALU.mult), r=[xt, ss], w=[xn])
            if dbg == -1:
                continue
            for kg in range(KC // 4):
                p = pT[kg % 2]
                for q4 in range(4):
                    kk = kg * 4 + q4
                    sl = slice(q4 * 128, q4 * 128 + 128)
                    k.op("pe", lambda e: e.transpose(out=p[:, sl], in_=xn[:, kk * 128:(kk + 1) * 128], identity=ident_b[:]),
                         r=[xn, ident_b], w=[p])
                for q4 in range(4):
                    kk = kg * 4 + q4
                    sl = slice(q4 * 128, q4 * 128 + 128)
                    k.op("dve", lambda e: e.tensor_scalar(out=hT[:, kk, tt * 128:(tt + 1) * 128], in0=p[:, sl],
                                                          scalar1=Atab[:, kk:kk + 1], scalar2=Btab[:, kk:kk + 1],
                                                          op0=ALU.mult, op1=ALU.add),
                         r=[p], w=[hT])

    if "inproj" in phases:
        with contextlib.ExitStack() as st:
            xt = k.sb(st, "xt", [128, D], F32)
            xn = k.sb(st, "xn", [128, D], BF16)
            junk = k.sb(st, "junk", [128, D], BF16)
            ss = k.sb(st, "ss", [128, 4], F32)
            pT = [PBb[0], PBb[1]]
            hT = k.sb(st, "hT", [128, KC, 512], BF16)
            W = [k.sb(st, "W%d" % i, [128, KC, 512], BF16) for i in range(2)]
            cosT = k.sb(st, "cosT", [128, T], F32)
            sinT = k.sb(st, "sinT", [128, T], F32)
            rotm = k.sb(st, "rotm", [128, 128], F32)
            qkg = k.sb(st, "qkg", [128, 4], F32)
            psA = [PB[2], PB[3]]
            ps2 = PB[4]
            ps3 = PB[5]
            raw = k.sb(st, "raw", [128, 512], F32)
            sq = k.sb(st, "sq", [128, 512], F32)
            rs = k.sb(st, "rs", [128, 512], F32)
            qn = k.sb(st, "qn", [128, 512], F32)
            t1 = k.sb(st, "t1", [128, 512], F32)
            t2 = k.sb(st, "t2", [128, 512], F32)
            ob = [k.sb(st, "ob%d" % i, [128, 512], BF16) for i in range(2)]
            of = [k.sb(st, "of%d" % i, [128, 512], F32) for i in range(2)]
            oqr = k.sb(st, "oqr", [128, 512], BF16)
            k.dma(cosT[:], ins["cosT"][:, :], w=[cosT])
            k.dma(sinT[:], ins["sinT"][:, :], w=[sinT])
            k.dma(rotm[:], ins["rotm"][:, :], w=[rotm])
            k.dma(qkg[:], ins["qkg_l"][:, :], w=[qkg])
            wv = sc["wi16_d"].rearrange("(k p) f -> p k f", p=128)
            cnt = [0]
            for tb in range(ntb):
                ts = slice(tb * 512, (tb + 1) * 512)
                if dbg != 0:
                    norm_block((xt, xn, junk, ss, pT), ins["x"], tb, A1, B1, hT)
                if dbg <= 1:
                    continue
                for ct in range(15):
                    if dbg == 2 and ct > 0:
                        continue
                    if dbg == 3 and ct not in (2,):
                        continue
                    if dbg == 4 and ct not in (11,):
                        continue
                    if dbg == 5 and ct not in (14,):
                        continue
                    if dbg == 6 and ct not in (8,):
                        continue
                    ncol = 512 if ct < 14 else 72
                    Wt = W[ct % 2]
                    for pc in range(4):
                        k.dma(Wt[:, pc * 8:(pc + 1) * 8, 0:ncol], wv[:, pc * 8:(pc + 1) * 8, ct * 512:ct * 512 + ncol], r=["wi16_d"], w=[Wt],
                              q=("sp" if pc % 2 == 0 else "act"))
                    if ct in (11, 13):
                        dst = sc["vs_d"] if ct == 11 else sc["vw_d"]
                        for tt in range(4):
                            ps = psA[cnt[0] % 2]
                            o = ob[cnt[0] % 2]
                            cnt[0] += 1
                            for kk in range(KC):
                                k.op("pe", lambda e: e.matmul(ps[:], lhsT=hT[:, kk, tt * 128:(tt + 1) * 128], rhs=Wt[:, kk, :],
                                                              start=(kk == 0), stop=(kk == KC - 1)), r=[hT, Wt], w=[ps])
                            k.op("act", lambda e: e.activation(out=o[:], in_=ps[:], func=AF.Identity), r=[ps], w=[o])
                            k.dma(dst[tb * 512 + tt * 128: tb * 512 + (tt + 1) * 128, :], o[:], r=[o], w=[dst])
                        continue
                    nsub = (ncol + 127) // 128
                    for sub in range(nsub):
                        mrows = min(128, ncol - sub * 128)
                        ps = psA[cnt[0] % 2]
                        o = ob[cnt[0] % 2]
                        o32 = of[cnt[0] % 2]
                        cnt[0] += 1
                        for kk in range(KC):
                            k.op("pe", lambda e: e.matmul(ps[0:mrows, :], lhsT=Wt[:, kk, sub * 128:sub * 128 + mrows], rhs=hT[:, kk, :],
                                                          start=(kk == 0), stop=(kk == KC - 1)), r=[hT, Wt], w=[ps])
                        fc = ct * 4 + sub
                        if ct < 2:
                            k.op("act", lambda e: e.activation(out=o32[:], in_=ps[:], func=AF.Identity), r=[ps], w=[o32])
                            k.dma(sc["pT_d"][fc * 128:(fc + 1) * 128, ts], o32[:], r=[o32], w=["pT_d"])
                        elif ct in (8, 9):
                            k.op("act", lambda e: e.activation(out=o[:], in_=ps[:], func=AF.Identity), r=[ps], w=[o])
                            k.dma(sc["kc_d"][ct - 8, sub, :, ts], o[:], r=[o], w=["kc_d"])
                        elif ct == 14:
                            k.op("act", lambda e: e.activation(out=o32[0:72, :], in_=ps[0:72, :], func=AF.Sigmoid), r=[ps], w=[o32])
                            k.dma(sc["gT_d"][:, ts], o32[0:72, :], r=[o32], w=["gT_d"])
                        else:
                            if ct < 8:
                                gcol = 0
                            elif ct == 10:
                                gcol = 2
                            else:
                                gcol = 3
                            k.op("act", lambda e: e.activation(out=raw[:], in_=ps[:], func=AF.Identity), r=[ps], w=[raw])
                            k.op("act", lambda e: e.activation(out=sq[:], in_=ps[:], func=AF.Square), r=[ps], w=[sq])
                            k.op("pe", lambda e: e.matmul(ps2[:], lhsT=ones_f[:], rhs=sq[:], start=True, stop=True), r=[ones_f, sq], w=[ps2])
                            k.op("dve", lambda e: e.tensor_scalar(out=rs[:], in0=ps2[:], scalar1=1.0 / 128, scalar2=EPS,
                                                                  op0=ALU.mult, op1=ALU.add), r=[ps2], w=[rs])
                            k.op("act", lambda e: e.activation(out=sq[:], in_=rs[:], func=AF.Sqrt), r=[rs], w=[sq])
                            k.op("dve", lambda e: e.reciprocal(out=rs[:], in_=sq[:]), r=[sq], w=[rs])
                            k.op("dve", lambda e: e.scalar_tensor_tensor(out=qn[:], in0=raw[:], scalar=qkg[:, gcol:gcol + 1], in1=rs[:],
                                                                         op0=ALU.mult, op1=ALU.mult), r=[raw, qkg, rs], w=[qn])
                            if ct < 8:
                                h = fc - 8
                                k.op("act", lambda e: e.activation(out=o[:], in_=qn[:], func=AF.Identity), r=[qn], w=[o])
                                k.dma(sc["qn_d"][h, :, ts], o[:], r=[o], w=["qn_d"])
                            k.op("pe", lambda e: e.matmul(ps3[:], lhsT=rotm[:], rhs=qn[:], start=True, stop=True), r=[rotm, qn], w=[ps3])
                            k.op("dve", lambda e: e.tensor_tensor(out=t1[:], in0=qn[:], in1=cosT[:, ts], op=ALU.mult), r=[qn, cosT], w=[t1])
                            k.op("dve", lambda e: e.tensor_tensor(out=t2[:], in0=ps3[:], in1=sinT[:, ts], op=ALU.mult), r=[ps3, sinT], w=[t2])
                            k.op("dve", lambda e: e.tensor_tensor(out=t1[:], in0=t1[:], in1=t2[:], op=ALU.add), r=[t1, t2], w=[t1])
                            ob2 = oqr
                            k.op("act", lambda e: e.activation(out=ob2[:], in_=t1[:], func=AF.Identity), r=[t1], w=[ob2])
                            if ct < 8:
                                k.dma(sc["qr_d"][fc - 8, :, ts], ob2[:], r=[ob2], w=["qr_d"])
                            elif ct == 10:
                                k.dma(sc["ks_d"][sub, :, ts], ob2[:], r=[ob2], w=["ks_d"])
                            else:
                                k.dma(sc["kw_d"][sub, :, ts], ob2[:], r=[ob2], w=["kw_d"])
        k.barrier()


    if "pool" in phases:
        with contextlib.ExitStack() as st:
            pt = k.sb(st, "pl_pt", [128, T], F32)
            sa = k.sb(st, "pl_sa", [128, T], F32)
            sb_ = k.sb(st, "pl_sb", [128, T], F32)
            inv = k.sb(st, "pl_inv", [128, 4, T], F32)
            dT = k.sb(st, "pl_dT", [128, 2, T], BF16)
            wp = k.sb(st, "pl_wp", [128, 2, 256], F32)
            wpb = k.sb(st, "pl_wpb", [128, 2, 256], BF16)
            psl = k.sb(st, "pl_psl", [128, 8], F32)
            yo = [k.sb(st, "pl_yo%d" % i, [128, 512], BF16) for i in range(2)]
            k.dma(inv[:], ins["invcnt"][:, :, :], w=[inv])
            k.dma(psl[:], ins["pscale_l"][:, :], w=[psl])
            cnt = 0
            for gi in range(4):
                wwin = (2, 4, 8, 16)[gi]
                k.dma(wp[:], ins["w_pool"][gi].rearrange("(cc p) d -> p cc d", p=128), w=[wp])
                k.op("dve", lambda e: e.tensor_copy(out=wpb[:], in_=wp[:]), r=[wp], w=[wpb])
                for cc in range(2):
                    ch = gi * 2 + cc
                    k.dma(pt[:], sc["pT_d"][ch * 128:(ch + 1) * 128, :], r=["pT_d"], w=[pt])
                    cur = pt
                    bufs = [sa, sb_]
                    bi = 0
                    sh = 1
                    while sh < wwin:
                        nxt = bufs[bi]
                        bi ^= 1
                        k.op("dve", lambda e: e.tensor_tensor(out=nxt[:, sh:T], in0=cur[:, sh:T], in1=cur[:, 0:T - sh], op=ALU.add),
                             r=[cur], w=[nxt])
                        k.op("dve", lambda e: e.tensor_copy(out=nxt[:, 0:sh], in_=cur[:, 0:sh]), r=[cur], w=[nxt])
                        cur = nxt
                        sh *= 2
                    tmp = bufs[bi]
                    k.op("dve", lambda e: e.tensor_tensor(out=tmp[:], in0=cur[:], in1=inv[:, gi, :], op=ALU.mult), r=[cur, inv], w=[tmp])
                    k.op("dve", lambda e: e.tensor_tensor(out=dT[:, cc, :], in0=tmp[:], in1=pt[:], op=ALU.subtract), r=[tmp, pt], w=[dT])
                for dc in range(2):
                    for tb in range(4):
                        ps = PB[cnt % 2]
                        o = yo[cnt % 2]
                        cnt += 1
                        for cc in range(2):
                            k.op("pe", lambda e: e.matmul(ps[:], lhsT=wpb[:, cc, dc * 128:(dc + 1) * 128], rhs=dT[:, cc, tb * 512:(tb + 1) * 512],
                                                          start=(cc == 0), stop=(cc == 1)), r=[wpb, dT], w=[ps])
                        col = gi * 2 + dc
                        k.op("dve", lambda e: e.tensor_scalar(out=o[:], in0=ps[:], scalar1=psl[:, col:col + 1], scalar2=None, op0=ALU.mult),
                             r=[ps, psl], w=[o])
                        k.dma(sc["yT_d"][col * 128:(col + 1) * 128, tb * 512:(tb + 1) * 512], o[:], r=[o], w=["yT_d"])
        k.barrier()

    k.op("dve", lambda e: e.memset(kcmpT[:], 0.0), w=[kcmpT])
    k.op("dve", lambda e: e.memset(vcmp[:], 0.0), w=[vcmp])
    if "cmp" in phases:
        with contextlib.ExitStack() as st:
            src = k.sb(st, "cp_src", [128, T], BF16)
            w1s = [k.sb(st, "cp_w1s%d" % i, [128, 8, 256], F32) for i in range(2)]
            w1b = k.sb(st, "cp_w1b", [128, 32, 256], BF16)
            w2s = k.sb(st, "cp_w2s", [128, 2, 128], F32)
            w2b = k.sb(st, "cp_w2b", [128, 2, 128], BF16)
            pel = k.sb(st, "cp_pel", [128, 32], F32)
            peb = k.sb(st, "cp_peb", [128, 32], BF16)
            hidT = k.sb(st, "cp_hidT", [128, 2, 128], BF16)
            hpre = k.sb(st, "cp_hpre", [128, 128], F32)
            bias = k.sb(st, "cp_bias", [128, 2], F32)
            kf = k.sb(st, "cp_kf", [128, 128], F32)
            sq = k.sb(st, "cp_sq", [128, 128], F32)
            rs = k.sb(st, "cp_rs", [128, 128], F32)
            qkg = k.sb(st, "cp_qkg", [128, 4], F32)
            k.dma(qkg[:], ins["qkg_l"][:, :], w=[qkg])
            k.op("dve", lambda e: e.memset(hidT[:], 0.0), w=[hidT])
            for kv in range(2):
                w1v = ins["cmp_w1"][kv].rearrange("l d h -> d l h")
                for pc in range(4):
                    stg = w1s[pc % 2]
                    k.dma(stg[:], w1v[:, pc * 8:(pc + 1) * 8, :], w=[stg])
                    k.op("pool", lambda e: e.tensor_copy(out=w1b[:, pc * 8:(pc + 1) * 8, :], in_=stg[:]), r=[stg], w=[w1b])
                k.dma(w2s[:], ins["cmp_w2"][kv].rearrange("(hc p) d -> p hc d", p=128), w=[w2s])
                k.op("dve", lambda e: e.tensor_copy(out=w2b[:], in_=w2s[:]), r=[w2s], w=[w2b])
                k.dma(pel[:], ins["pe_l"][kv], w=[pel])
                k.op("dve", lambda e: e.tensor_copy(out=peb[:], in_=pel[:]), r=[pel], w=[peb])
                for hc in range(2):
                    ps = PB[4]
                    for l in range(32):
                        k.op("pe", lambda e: e.matmul(ps[:, 0:1], lhsT=w1b[:, l, hc * 128:(hc + 1) * 128], rhs=peb[:, l:l + 1],
                                                      start=(l == 0), stop=(l == 31)), r=[w1b, peb], w=[ps])
                    k.op("dve", lambda e: e.tensor_copy(out=bias[:, hc:hc + 1], in_=ps[:, 0:1]), r=[ps], w=[bias])
                for g in range(G):
                    k.dma(src[:], sc["kc_d"][kv, g, :, :], r=["kc_d"], w=[src])
                    for hc in range(2):
                        ps = PB[hc]
                        for l in range(32):
                            k.op("pe", lambda e: e.matmul(ps[:, 0:127], lhsT=w1b[:, l, hc * 128:(hc + 1) * 128],
                                                          rhs=src[:, l:l + 16 * 126 + 1:16],
                                                          start=(l == 0), stop=(l == 31)), r=[w1b, src], w=[ps])
                        k.op("dve", lambda e: e.tensor_scalar(out=hpre[:, 0:127], in0=ps[:, 0:127], scalar1=bias[:, hc:hc + 1], scalar2=None,
                                                              op0=ALU.add), r=[ps, bias], w=[hpre])
                        k.op("act", lambda e: e.activation(out=hidT[:, hc, 0:127], in_=hpre[:, 0:127], func=AF.Gelu), r=[hpre], w=[hidT])
                    if kv == 0:
                        ps = PB[2]
                        for hc in range(2):
                            k.op("pe", lambda e: e.matmul(ps[:, 0:127], lhsT=w2b[:, hc, :], rhs=hidT[:, hc, 0:127],
                                                          start=(hc == 0), stop=(hc == 1)), r=[w2b, hidT], w=[ps])
                        k.op("act", lambda e: e.activation(out=kf[:, 0:127], in_=ps[:, 0:127], func=AF.Identity), r=[ps], w=[kf])
                        k.op("act", lambda e: e.activation(out=sq[:, 0:127], in_=ps[:, 0:127], func=AF.Square), r=[ps], w=[sq])
                        k.op("pe", lambda e: e.matmul(PB[3][:, 0:127], lhsT=ones_f[:], rhs=sq[:, 0:127], start=True, stop=True),
                             r=[ones_f, sq], w=[PB[3]])
                        k.op("dve", lambda e: e.tensor_scalar(out=rs[:, 0:127], in0=PB[3][:, 0:127], scalar1=1.0 / 128, scalar2=EPS,
                                                              op0=ALU.mult, op1=ALU.add), r=[PB[3]], w=[rs])
                        k.op("act", lambda e: e.activation(out=sq[:, 0:127], in_=rs[:, 0:127], func=AF.Sqrt), r=[rs], w=[sq])
                        k.op("dve", lambda e: e.reciprocal(out=rs[:, 0:127], in_=sq[:, 0:127]), r=[sq], w=[rs])
                        k.op("dve", lambda e: e.scalar_tensor_tensor(out=kcmpT[:, g, 0:127], in0=kf[:, 0:127], scalar=qkg[:, 1:2],
                                                                     in1=rs[:, 0:127], op0=ALU.mult, op1=ALU.mult),
                             r=[kf, qkg, rs], w=[kcmpT])
                    else:
                        ps = PB[2]
                        for hc in range(2):
                            k.op("pe", lambda e: e.matmul(ps[0:127, 0:128], lhsT=hidT[:, hc, 0:127], rhs=w2b[:, hc, :],
                                                          start=(hc == 0), stop=(hc == 1)), r=[w2b, hidT], w=[ps])
                        k.op("act", lambda e: e.activation(out=vcmp[0:127, g, :], in_=ps[0:127, 0:128], func=AF.Identity), r=[ps], w=[vcmp])
        k.barrier()

    if "attn" in phases:
        with contextlib.ExitStack() as st:
            ksT = k.sb(st, "at_ksT", [128, T], BF16)
            kwT = k.sb(st, "at_kwT", [128, T], BF16)
            vs = k.sb(st, "at_vs", [128, 16, 128], BF16)
            vw = k.sb(st, "at_vw", [128, 16, 128], BF16)
            qn6 = k.sb(st, "at_qn6", [128, R, 512], BF16)
            qr6 = k.sb(st, "at_qr6", [128, R, 512], BF16)
            cmpm = k.sb(st, "at_cmpm", [128, T], F32)
            selmap_s = k.sb(st, "at_selmap", [128, NSEL], F32)
            tbl = k.sb(st, "at_tbl", [128, 16, NSEL], F32)
            eb = k.sb(st, "at_eb", [NSEL, 16, 128], F32)
            caus = k.sb(st, "at_caus", [128, 4, 512], F32)
            wmask = k.sb(st, "at_wmask", [128, 8, 512], F32)
            Pf = k.sb(st, "at_Pf", [128, 512], F32)
            Pn = k.sb(st, "at_Pn", [128, 512], F32)
            rden = k.sb(st, "at_rden", [128, 512], F32)
            Pb = [k.sb(st, "at_Pb%d" % i, [128, 512], BF16) for i in range(4)]
            score4 = k.sb(st, "at_score4", [128, 4, NSEL], F32)
            work = k.sb(st, "at_work", [128, NSEL], F32)
            m8 = k.sb(st, "at_m8", [128, 16], F32)
            selm = k.sb(st, "at_selm", [128, NSEL], F32)
            selT = k.sb(st, "at_selT", [NSEL, T], F32)
            M = k.sb(st, "at_M", [128, 16, 512], BF16)
            gb2 = [k.sb(st, "at_gb%d" % i, [128, 3, 512], F32) for i in range(2)]
            yacc = k.sb(st, "at_y", [128, 512], F32)
            wgt = k.sb(st, "at_wgt", [128, 512], F32)
            tmpo = k.sb(st, "at_tmpo", [128, 512], F32)
            yb = k.sb(st, "at_yb", [128, 512], BF16)
            k.dma(cmpm[:], ins["cmpmask"][:, :], w=[cmpm])
            k.dma(selmap_s[:], ins["selmap"][:, :], w=[selmap_s])
            k.dma(tbl[:], ins["tb"].rearrange("(tt p) j -> p tt j", p=128), w=[tbl])
            k.dma(eb[:], ins["eb"][:, :, :], w=[eb])
            k.dma(caus[:], ins["caus"][:, :, :], w=[caus])
            k.dma(wmask[:], ins["wmask"][:, :, :], w=[wmask])

            bcount = [0]

            def combine(b, O, Dn, gb):
                k.op("dve", lambda e: e.tensor_scalar(out=wgt[:], in0=Dn[:], scalar1=1e-30, scalar2=None, op0=ALU.max), r=[Dn], w=[wgt])
                k.op("dve", lambda e: e.reciprocal(out=wgt[:], in_=wgt[:]), r=[wgt], w=[wgt])
                k.op("dve", lambda e: e.tensor_tensor(out=wgt[:], in0=wgt[:], in1=gb[:, b, :], op=ALU.mult), r=[wgt, gb], w=[wgt])
                if b == 0:
                    k.op("dve", lambda e: e.tensor_tensor(out=yacc[:], in0=O[:], in1=wgt[:], op=ALU.mult), r=[O, wgt], w=[yacc])
                else:
                    k.op("dve", lambda e: e.tensor_tensor(out=tmpo[:], in0=O[:], in1=wgt[:], op=ALU.mult), r=[O, wgt], w=[tmpo])
                    k.op("pool", lambda e: e.tensor_tensor(out=yacc[:], in0=yacc[:], in1=tmpo[:], op=ALU.add), r=[yacc, tmpo], w=[yacc])

            for g in range(G):
                k.dma(ksT[:], sc["ks_d"][g, :, :], r=["ks_d"], w=[ksT])
                k.dma(kwT[:], sc["kw_d"][g, :, :], r=["kw_d"], w=[kwT])
                k.dma(vs[:], sc["vs_d"][:, g * 128:(g + 1) * 128].rearrange("(m p) d -> p m d", p=128), r=["vs_d"], w=[vs])
                k.dma(vw[:], sc["vw_d"][:, g * 128:(g + 1) * 128].rearrange("(m p) d -> p m d", p=128), r=["vw_d"], w=[vw])
                for c in range(4):
                    ts = slice(c * 512, (c + 1) * 512)
                    k.dma(qn6[:], sc["qn_d"][g * R:(g + 1) * R, :, ts].rearrange("r d t -> d r t"), r=["qn_d"], w=[qn6])
                    for r_ in range(R):
                        S = PB[r_ % 2]
                        k.op("pe", lambda e: e.matmul(S[:], lhsT=kcmpT[:, g, :], rhs=qn6[:, r_, :], start=True, stop=True), r=[kcmpT, qn6], w=[S])
                        k.op("act", lambda e: e.activation(out=Pf[:], in_=S[:], func=AF.Exp, scale=SCALE), r=[S], w=[Pf])
                        k.op("dve", lambda e: e.tensor_tensor(out=Pf[:], in0=Pf[:], in1=cmpm[:, ts], op=ALU.mult), r=[Pf, cmpm], w=[Pf])
                        k.op("pe", lambda e: e.matmul(PB[2][:], lhsT=ones_f[:], rhs=Pf[:], start=True, stop=True), r=[ones_f, Pf], w=[PB[2]])
                        k.op("dve", lambda e: e.tensor_scalar(out=rden[:], in0=PB[2][:], scalar1=1e-30, scalar2=None, op0=ALU.max), r=[PB[2]], w=[rden])
                        k.op("dve", lambda e: e.reciprocal(out=rden[:], in_=rden[:]), r=[rden], w=[rden])
                        k.op("dve", lambda e: e.tensor_tensor(out=Pn[:], in0=Pf[:], in1=rden[:], op=ALU.mult), r=[Pf, rden], w=[Pn])
                        for tt in range(4):
                            k.op("pe", lambda e: e.matmul(PB[3][:, tt * 32:(tt + 1) * 32], lhsT=Pn[:, tt * 128:(tt + 1) * 128], rhs=selmap_s[:, :],
                                                          start=(r_ == 0), stop=(r_ == R - 1)), r=[Pn, selmap_s], w=[PB[3]])
                    k.op("dve", lambda e: e.tensor_tensor(out=score4[:].rearrange("p a b -> p (a b)"), in0=PB[3][:, 0:128],
                                                          in1=tbl[:, c * 4:(c + 1) * 4, :].rearrange("p a b -> p (a b)"), op=ALU.add),
                         r=[PB[3], tbl], w=[score4])
                    for tt in range(4):
                        sc_ = score4[:, tt, :]
                        k.op("dve", lambda e: e.max(out=m8[:, 0:8], in_=sc_), r=[score4], w=[m8])
                        k.op("dve", lambda e: e.match_replace(out=work[:], in_to_replace=m8[:, 0:8], in_values=sc_, imm_value=-3e9), r=[score4, m8], w=[work])
                        k.op("dve", lambda e: e.max(out=m8[:, 8:16], in_=work[:]), r=[work], w=[m8])
                        k.op("dve", lambda e: e.tensor_scalar(out=selm[:], in0=sc_, scalar1=m8[:, 15:16], scalar2=None, op0=ALU.is_ge), r=[score4, m8], w=[selm])
                        k.op("pe", lambda e: e.transpose(out=PB[4][0:NSEL, tt * 128:(tt + 1) * 128], in_=selm[:], identity=ident_f[:]), r=[selm, ident_f], w=[PB[4]])
                    k.op("act", lambda e: e.activation(out=selT[:, ts], in_=PB[4][0:NSEL, :], func=AF.Identity), r=[PB[4]], w=[selT])
                for c in range(4):
                    ts = slice(c * 512, (c + 1) * 512)
                    nm = 4 * c + 4
                    for m in range(nm):
                        k.op("pe", lambda e: e.matmul(PB[7][:], lhsT=eb[:, m, :], rhs=selT[:, ts], start=True, stop=True), r=[eb, selT], w=[PB[7]])
                        if m >= 4 * c:
                            k.op("dve", lambda e: e.tensor_tensor(out=M[:, m, :], in0=PB[7][:], in1=caus[:, m - 4 * c, :], op=ALU.mult), r=[PB[7], caus], w=[("M", m)])
                        else:
                            k.op("act", lambda e: e.activation(out=M[:, m, :], in_=PB[7][:], func=AF.Identity), r=[PB[7]], w=[("M", m)])
                    k.dma(qn6[:], sc["qn_d"][g * R:(g + 1) * R, :, ts].rearrange("r d t -> d r t"), r=["qn_d"], w=[qn6])
                    k.dma(qr6[:], sc["qr_d"][g * R:(g + 1) * R, :, ts].rearrange("r d t -> d r t"), r=["qr_d"], w=[qr6])
                    jobs = []
                    for r_ in range(R):
                        h = g * R + r_
                        jobs.append(dict(kT=kcmpT[:, g, :], q=qn6[:, r_, :], mask=cmpm[:, ts], mk=cmpm, v=vcmp[:, g, :], br=0, h=h,
                                         first=True, last=True, kk=[kcmpT, qn6], vk=vcmp))
                        for m in range(nm):
                            jobs.append(dict(kT=ksT[:, m * 128:(m + 1) * 128], q=qr6[:, r_, :], mask=M[:, m, :], mk=("M", m), v=vs[:, m, :], br=1, h=h,
                                             first=(m == 0), last=(m == nm - 1), kk=[ksT, qr6], vk=vs))
                        m0 = max(0, 4 * c - 4)
                        for m in range(m0, nm):
                            jobs.append(dict(kT=kwT[:, m * 128:(m + 1) * 128], q=qr6[:, r_, :], mask=wmask[:, m - 4 * c + 4, :], mk=wmask, v=vw[:, m, :], br=2, h=h,
                                             first=(m == m0), last=(m == nm - 1), kk=[kwT, qr6], vk=vw))
                    Sb = [PB[0], PB[1], PB[2]]
                    sets = [(PB[3], PB[4]), (PB[5], PB[6])]
                    LOOK = 2
                    nj = len(jobs)
                    for i in range(nj + LOOK):
                        if i < nj:
                            j = jobs[i]
                            S = Sb[i % 3]
                            if j["br"] == 0:
                                hp = j["h"] % 2
                                if (j["h"] * 4 + c) % 3 != 0:
                                    conv_step(1)
                                for b_ in range(3):
                                    k.dma(gb2[hp][:, b_, :], sc["gT_d"][j["h"] * 3 + b_:j["h"] * 3 + b_ + 1, ts].partition_broadcast(128), r=["gT_d"], w=[gb2[hp]])
                            k.op("pe", lambda e: e.matmul(S[:], lhsT=j["kT"], rhs=j["q"], start=True, stop=True), r=j["kk"], w=[S])
                        if i >= LOOK:
                            ii = i - LOOK
                            j = jobs[ii]
                            S = Sb[ii % 3]
                            P = Pb[ii % 4]
                            if j["first"]:
                                bcount[0] += 1
                            O, Dn = sets[bcount[0] % 2]
                            k.op("act", lambda e: e.activation(out=P[:], in_=S[:], func=AF.Exp, scale=SCALE), r=[S], w=[P])
                            k.op("pool" if j["br"] == 2 else "dve", lambda e: e.tensor_tensor(out=P[:], in0=P[:], in1=j["mask"], op=ALU.mult), r=[P, j["mk"]], w=[P])
                            k.op("pe", lambda e: e.matmul(O[:], lhsT=j["v"], rhs=P[:], start=j["first"], stop=j["last"]), r=[j["vk"], P], w=[O])
                            k.op("pe", lambda e: e.matmul(Dn[:], lhsT=ones_b[:], rhs=P[:], start=j["first"], stop=j["last"]), r=[ones_b, P], w=[Dn])
                            if j["last"]:
                                combine(j["br"], O, Dn, gb2[j["h"] % 2])
                                if j["br"] == 2:
                                    hh = j["h"]
                                    k.op("act", lambda e: e.activation(out=yb[:], in_=yacc[:], func=AF.Identity), r=[yacc], w=[yb])
                                    k.dma(sc["yT_d"][1024 + hh * 128:1024 + (hh + 1) * 128, ts], yb[:], r=[yb], w=["yT_d"])
        k.barrier()

    if "outproj" in phases:
        with contextlib.ExitStack() as st:
            yT = k.sb(st, "op_yT", [128, KC, 512], BF16)
            W = [k.sb(st, "op_W%d" % i, [128, KC, 512], BF16) for i in range(2)]
            g1b = k.sb(st, "op_g1b", [128, D], F32)
            xt2 = [k.sb(st, "op_xt%d" % i, [128, 512], F32) for i in range(2)]
            o2 = [k.sb(st, "op_o%d" % i, [128, 512], F32) for i in range(2)]
            modrow = sc["mod_d"].rearrange("(a f) -> a f", a=1)
            k.dma(g1b[:], modrow[0:1, 2 * D:3 * D].partition_broadcast(128), r=["mod_d"], w=[g1b])
            yv = sc["yT_d"].rearrange("(k p) t -> p k t", p=128)
            wv = sc["wo16_d"].rearrange("(k p) f -> p k f", p=128)
            cnt = 0
            for tb in range(4):
                ts = slice(tb * 512, (tb + 1) * 512)
                for pc in range(4):
                    k.dma(yT[:, pc * 8:(pc + 1) * 8, :], yv[:, pc * 8:(pc + 1) * 8, ts], r=["yT_d"], w=[yT])
                for fb in range(8):
                    fs = slice(fb * 512, (fb + 1) * 512)
                    Wt = W[fb % 2]
                    for pc in range(4):
                        k.dma(Wt[:, pc * 8:(pc + 1) * 8, :], wv[:, pc * 8:(pc + 1) * 8, fs], r=["wo16_d"], w=[Wt], q=("sp" if pc % 2 == 0 else "act"))
                    for tt in range(4):
                        ps = PB[cnt % 2]
                        xx = xt2[cnt % 2]
                        oo = o2[cnt % 2]
                        cnt += 1
                        rows = slice(tb * 512 + tt * 128, tb * 512 + (tt + 1) * 128)
                        k.dma(xx[:], ins["x"][rows, fs], w=[xx])
                        for kk in range(KC):
                            k.op("pe", lambda e: e.matmul(ps[:], lhsT=yT[:, kk, tt * 128:(tt + 1) * 128], rhs=Wt[:, kk, :],
                                                          start=(kk == 0), stop=(kk == KC - 1)), r=[yT, Wt], w=[ps])
                        k.op("dve", lambda e: e.tensor_tensor(out=oo[:], in0=ps[:], in1=g1b[:, fs], op=ALU.mult), r=[ps, g1b], w=[oo])
                        k.op("dve", lambda e: e.tensor_tensor(out=oo[:], in0=oo[:], in1=xx[:], op=ALU.add), r=[oo, xx], w=[oo])
                        k.dma(sc["x1_d"][rows, fs], oo[:], r=[oo], w=["x1_d"])
        k.barrier()

    if "peer" in phases:
        with contextlib.ExitStack() as st:
            xt = k.sb(st, "pr_xt", [128, D], F32)
            xn = k.sb(st, "pr_xn", [128, D], BF16)
            junk = k.sb(st, "pr_junk", [128, D], BF16)
            ss = k.sb(st, "pr_ss", [128, 4], F32)
            hT = k.sb(st, "pr_hT", [128, KC, 512], BF16)
            Wt2 = [k.sb(st, "pr_W%d" % i, [128, KC, 512], BF16) for i in range(2)]
            kraw = k.sb(st, "pr_kraw", [128, 16, 128], F32)
            keysT = k.sb(st, "pr_keysT", [128, 16, 128], F32)
            qT = k.sb(st, "pr_qT", [128, 16, 512], F32)
            S = k.sb(st, "pr_S", [128, 16, 128], F32)
            wk = k.sb(st, "pr_wk", [128, 256], F32)
            v2 = k.sb(st, "pr_v2", [128, 2, 16], F32)
            i2u = k.sb(st, "pr_i2u", [128, 2, 16], U32)
            i2f = k.sb(st, "pr_i2f", [128, 2, 16], F32)
            cand = k.sb(st, "pr_cand", [128, 16, 16], F32)
            cidx = k.sb(st, "pr_cidx", [128, 16, 16], F32)
            tops = k.sb(st, "pr_tops", [128, 16], F32)
            ef = k.sb(st, "pr_ef", [128, 128], F32)
            ei = k.sb(st, "pr_ei", [128, 128], I32)
            gt = k.sb(st, "pr_gt", [128, 128], F32)
            sm = k.sb(st, "pr_sm", [128, 4], F32)
            ex = k.sb(st, "pr_ex", [128, 16], F32)
            iot = k.sb(st, "pr_iota", [128, 256], F32)
            wk2 = k.sb(st, "pr_wk2", [128, 256], F32)
            posu = k.sb(st, "pr_posu", [128, 16], U32)
            posf = k.sb(st, "pr_posf", [128, 16], F32)
            k.dma(iot[:], ins["iota256"][:, :], w=[iot])
            k.dma(kraw[:], ins["peer_keys"].rearrange("a n d -> n a d"), w=[kraw])
            for hc in range(16):
                pk = PB[4 + (hc // 4) % 2]
                k.op("pe", lambda e: e.transpose(out=pk[:, (hc % 4) * 128:(hc % 4 + 1) * 128], in_=kraw[:, hc, :], identity=ident_f[:]),
                     r=[kraw, ident_f], w=[pk])
                if hc % 4 == 3:
                    k.op("act", lambda e: e.activation(out=keysT[:, hc - 3:hc + 1, :].rearrange("p a b -> p (a b)"), in_=pk[:], func=AF.Identity),
                         r=[pk], w=[keysT])
            wv = sc["wq16_d"].rearrange("(k p) f -> p k f", p=128)
            pT = [PBb[0], PBb[1]]
            cnt = 0
            for tb in range(4):
                norm_block((xt, xn, junk, ss, pT), sc["x1_d"], tb, A2, B2, hT)
                for wt_ in range(4):
                    Wt = Wt2[wt_ % 2]
                    for pc in range(4):
                        k.dma(Wt[:, pc * 8:(pc + 1) * 8, :], wv[:, pc * 8:(pc + 1) * 8, wt_ * 512:(wt_ + 1) * 512], r=["wq16_d"], w=[Wt], q=("sp" if pc % 2 == 0 else "act"))
                    for sub in range(4):
                        hc = wt_ * 4 + sub
                        ps = PB[2 + cnt % 2]
                        cnt += 1
                        for kk in range(KC):
                            k.op("pe", lambda e: e.matmul(ps[:], lhsT=Wt[:, kk, sub * 128:(sub + 1) * 128], rhs=hT[:, kk, :],
                                                          start=(kk == 0), stop=(kk == KC - 1)), r=[Wt, hT], w=[ps])
                        k.op("act", lambda e: e.activation(out=qT[:, hc, :], in_=ps[:], func=AF.Identity), r=[ps], w=[("qT", hc)])
                for tt in range(4):
                    rows = slice(tb * 512 + tt * 128, tb * 512 + (tt + 1) * 128)
                    for hc in range(16):
                        pk = PB[4 + (hc // 4) % 2]
                        k.op("pe", lambda e: e.matmul(pk[:, (hc % 4) * 128:(hc % 4 + 1) * 128], lhsT=qT[:, hc, tt * 128:(tt + 1) * 128], rhs=keysT[:, hc, :],
                                                      start=True, stop=True), r=[("qT", hc), keysT], w=[pk])
                        if hc % 4 == 3:
                            k.op("act", lambda e: e.activation(out=S[:, hc - 3:hc + 1, :].rearrange("p a b -> p (a b)"), in_=pk[:], func=AF.Identity),
                                 r=[pk], w=[("S", hc // 4)])
                    for h in range(8):
                        skey = ("S", h // 2)
                        for c2 in range(2):
                            sv = S[:, 2 * h + c2, :]
                            k.op("dve", lambda e: e.max(out=v2[:, c2, 0:8], in_=sv), r=[skey], w=[v2])
                            k.op("dve", lambda e: e.max_index(out=i2u[:, c2, 0:8], in_max=v2[:, c2, 0:8], in_values=sv), r=[skey, v2], w=[i2u])
                            k.op("dve", lambda e: e.match_replace(out=wk[:, 0:128], in_to_replace=v2[:, c2, 0:8], in_values=sv, imm_value=-1e30), r=[skey, v2], w=[wk])
                            k.op("dve", lambda e: e.max(out=v2[:, c2, 8:16], in_=wk[:, 0:128]), r=[wk], w=[v2])
                            k.op("dve", lambda e: e.max_index(out=i2u[:, c2, 8:16], in_max=v2[:, c2, 8:16], in_values=wk[:, 0:128]), r=[wk, v2], w=[i2u])
                        k.op("dve", lambda e: e.tensor_copy(out=i2f[:], in_=i2u[:]), r=[i2u], w=[i2f])
                        k.op("dve", lambda e: e.tensor_scalar(out=i2f[:, 0, :], in0=i2f[:, 0, :], scalar1=128.0, scalar2=None, op0=ALU.mult), r=[i2f], w=[i2f])
                        k.op("dve", lambda e: e.tensor_tensor(out=cand[:], in0=v2[:, 0, :].unsqueeze(2).to_broadcast([128, 16, 16]),
                                                              in1=v2[:, 1, :].unsqueeze(1).to_broadcast([128, 16, 16]), op=ALU.add), r=[v2], w=[cand])
                        k.op("dve", lambda e: e.tensor_tensor(out=cidx[:], in0=i2f[:, 0, :].unsqueeze(2).to_broadcast([128, 16, 16]),
                                                              in1=i2f[:, 1, :].unsqueeze(1).to_broadcast([128, 16, 16]), op=ALU.add), r=[i2f], w=[cidx])
                        cf = cand[:].rearrange("p a b -> p (a b)")
                        xf = cidx[:].rearrange("p a b -> p (a b)")
                        k.op("dve", lambda e: e.max(out=tops[:, 0:8], in_=cf), r=[cand], w=[tops])
                        k.op("dve", lambda e: e.match_replace(out=wk[:], in_to_replace=tops[:, 0:8], in_values=cf, imm_value=-1e30), r=[cand, tops], w=[wk])
                        k.op("dve", lambda e: e.max(out=tops[:, 8:16], in_=wk[:]), r=[wk], w=[tops])
                        k.op("dve", lambda e: e.max_index(out=posu[:, 0:8], in_max=tops[:, 0:8], in_values=cf), r=[cand, tops], w=[posu])
                        k.op("dve", lambda e: e.max_index(out=posu[:, 8:16], in_max=tops[:, 8:16], in_values=wk[:]), r=[wk, tops], w=[posu])
                        k.op("dve", lambda e: e.tensor_copy(out=posf[:], in_=posu[:]), r=[posu], w=[posf])
                        k.op("dve", lambda e: e.memset(ef[:, h * 16:(h + 1) * 16], 0.0), w=[("ef", h)])
                        for kk in range(16):
                            k.op("dve", lambda e: e.scalar_tensor_tensor(out=wk2[:], in0=iot[:], scalar=posf[:, kk:kk + 1], in1=xf,
                                                                         op0=ALU.is_equal, op1=ALU.mult, accum_out=ef[:, h * 16 + kk:h * 16 + kk + 1]),
                                 r=[iot, cidx, posf], w=[wk2, ("ef", h)])
                        k.op("dve", lambda e: e.tensor_scalar(out=ef[:, h * 16:(h + 1) * 16], in0=ef[:, h * 16:(h + 1) * 16], scalar1=16383.0, scalar2=0.0,
                                                              op0=ALU.min, op1=ALU.max), r=[("ef", h)], w=[("ef", h)])
                        k.op("dve", lambda e: e.tensor_scalar(out=sm[:, 0:1], in0=tops[:, 0:1], scalar1=-1.0, scalar2=None, op0=ALU.mult), r=[tops], w=[sm])
                        k.op("dve", lambda e: e.memset(sm[:, 1:2], 0.0), w=[sm])
                        k.op("act", lambda e: e.activation(out=ex[:], in_=tops[:], func=AF.Exp, bias=sm[:, 0:1], accum_out=sm[:, 1:2]), r=[tops, sm], w=[ex, sm])
                        k.op("dve", lambda e: e.reciprocal(out=sm[:, 2:3], in_=sm[:, 1:2]), r=[sm], w=[sm])
                        k.op("dve", lambda e: e.tensor_scalar(out=gt[:, h * 16:(h + 1) * 16], in0=ex[:], scalar1=sm[:, 2:3], scalar2=None, op0=ALU.mult),
                             r=[ex, sm], w=[("gt", h)])
                    k.op("dve", lambda e: e.tensor_copy(out=ei[:], in_=ef[:]), r=[("ef", h) for h in range(8)], w=[ei])
                    k.dma(sc["eidx_d"][rows, :], ei[:], r=[ei], w=["eidx_d"])
                    k.dma(sc["ef_d"][rows, :], ef[:], r=[("ef", h) for h in range(8)], w=["ef_d"])
                    k.dma(sc["gts_d"][rows, :], gt[:], r=[("gt", h) for h in range(8)], w=["gts_d"])
        k.barrier()

        conv_step(1000)
        with contextlib.ExitStack() as st:
            h2f = k.sb(st, "pe_h2f", [128, D], F32)
            h2b = k.sb(st, "pe_h2b", [128, D], BF16)
            A2b = k.sb(st, "pe_A2b", [128, D], F32)
            B2b = k.sb(st, "pe_B2b", [128, D], F32)
            Ug = [k.sb(st, "pe_Ug%d" % i, [128, D], BF16) for i in range(4)]
            Vg = [k.sb(st, "pe_Vg%d" % i, [128, D], BF16) for i in range(4)]
            x1c = [k.sb(st, "pe_x1c%d" % i, [128, 512], F32) for i in range(2)]
            g2c = [k.sb(st, "pe_g2c%d" % i, [128, 512], F32) for i in range(2)]
            junkb = k.sb(st, "pe_junk", [128, D], BF16)
            x1t = k.sb(st, "pe_x1t", [128, D], F32)
            Wall = k.sb(st, "pe_Wall", [128, 128 * 128], BF16)
            ei = k.sb(st, "pe_ei", [128, 128], I32)
            eft = k.sb(st, "pe_eft", [128, 128], F32)
            eiT2 = [k.sb(st, "pe_eiT%d" % i, [128, 128], I32) for i in range(2)]
            gt = k.sb(st, "pe_gt", [128, 128], F32)
            av = k.sb(st, "pe_a", [128, 128], F32)
            wv_ = k.sb(st, "pe_w", [128, 128], F32)
            wT = k.sb(st, "pe_wT", [128, 128], F32)
            ss = k.sb(st, "pe_ss", [128, 4], F32)
            modrow = sc["mod_d"].rearrange("(a f) -> a f", a=1)
            k.op("pool", lambda e: e.memset(Wall[:], 0.0), w=[Wall])
            k.dma(A2b[:], modrow[0:1, 4 * D:5 * D].partition_broadcast(128), r=["mod_d"], w=[A2b])
            k.dma(B2b[:], ins["g2_row"][0:1, :].partition_broadcast(128), w=[B2b])
            k.op("dve", lambda e: e.scalar_tensor_tensor(out=A2b[:], in0=A2b[:], scalar=1.0, in1=B2b[:], op0=ALU.add, op1=ALU.mult), r=[A2b, B2b], w=[A2b])
            k.dma(B2b[:], modrow[0:1, 3 * D:4 * D].partition_broadcast(128), r=["mod_d", A2b], w=[B2b])
            def prep(ti):
                rows = slice(ti * 128, (ti + 1) * 128)
                eT = eiT2[ti % 2]
                k.dma(x1t[:], sc["x1_d"][rows, :], r=["x1_d"], w=[x1t])
                k.dma(ei[:], sc["eidx_d"][rows, :], r=["eidx_d"], w=[ei])
                k.dma(eft[:], sc["ef_d"][rows, :], r=["ef_d"], w=[eft])
                k.dma(gt[:], sc["gts_d"][rows, :], r=["gts_d"], w=[gt])
                k.op("dve", lambda e: e.memset(ss[:], 0.0), w=[ss])
                k.op("act", lambda e: e.activation(out=junkb[:], in_=x1t[:], func=AF.Square, accum_out=ss[:, 0:1]), r=[x1t, ss], w=[junkb, ss])
                k.op("dve", lambda e: e.tensor_scalar(out=ss[:, 1:2], in0=ss[:, 0:1], scalar1=1.0 / D, scalar2=EPS, op0=ALU.mult, op1=ALU.add), r=[ss], w=[ss])
                k.op("act", lambda e: e.activation(out=ss[:, 3:4], in_=ss[:, 1:2], func=AF.Sqrt), r=[ss], w=[ss])
                k.op("dve", lambda e: e.reciprocal(out=ss[:, 2:3], in_=ss[:, 3:4]), r=[ss], w=[ss])
                k.op("dve", lambda e: e.scalar_tensor_tensor(out=h2f[:], in0=x1t[:], scalar=ss[:, 2:3], in1=A2b[:], op0=ALU.mult, op1=ALU.mult), r=[x1t, ss, A2b], w=[h2f])
                k.op("dve", lambda e: e.tensor_tensor(out=h2b[:], in0=h2f[:], in1=B2b[:], op=ALU.add), r=[h2f, B2b], w=[h2b])
                k.op("pe", lambda e: e.transpose(out=PB[0][:, 0:128], in_=eft[:], identity=ident_f[:]), r=[eft, ident_f], w=[PB[0]])
                k.op("dve", lambda e: e.tensor_copy(out=eT[:], in_=PB[0][:, 0:128]), r=[PB[0]], w=[eT])
                k.op("dve", lambda e: e.memset(av[:], 0.0), w=[av])

            def u_step(ti, s_):
                u = Ug[s_ % 4]
                k.gather(u[:], sc["u16_d"][:, :], ei[:, s_:s_ + 1], r=[ei, "u16_d"], w=[u])
                k.op("dve", lambda e: e.scalar_tensor_tensor(out=junkb[:], in0=u[:], scalar=1.0, in1=h2b[:], op0=ALU.mult, op1=ALU.mult,
                                                             accum_out=av[:, s_:s_ + 1]), r=[u, h2b], w=[junkb, ("av", s_)])

            def post(ti):
                k.op("act", lambda e: e.activation(out=wv_[:], in_=av[:], func=AF.Gelu), r=[av] + [("av", s_) for s_ in range(128)], w=[wv_])
                k.op("dve", lambda e: e.tensor_tensor(out=wv_[:], in0=wv_[:], in1=gt[:], op=ALU.mult), r=[wv_, gt], w=[wv_])
                k.op("pe", lambda e: e.transpose(out=PB[0][:, 0:128], in_=wv_[:], identity=ident_f[:]), r=[wv_, ident_f], w=[PB[0]])
                k.op("dve", lambda e: e.tensor_copy(out=Wall[:, 0:128 * 128:129], in_=PB[0][:, 0:128]), r=[PB[0]], w=[Wall])

            def v_step(ti, t_):
                u = Vg[t_ % 4]
                eT = eiT2[ti % 2]
                k.gather(u[:], sc["v16_d"][:, :], eT[:, t_:t_ + 1], r=[eT, "v16_d"], w=[u])
                for nb in range(8):
                    k.op("pe", lambda e: e.matmul(PB[nb][:], lhsT=Wall[:, t_ * 128:(t_ + 1) * 128], rhs=u[:, nb * 512:(nb + 1) * 512],
                                                  start=(t_ == 0), stop=(t_ == 127)), r=[Wall, u], w=[PB[nb]])

            def evac(ti):
                rows = slice(ti * 128, (ti + 1) * 128)
                for nb in range(8):
                    cs = slice(nb * 512, (nb + 1) * 512)
                    xc = x1c[nb % 2]
                    gc = g2c[nb % 2]
                    k.dma(xc[:], sc["x1_d"][rows, cs], r=["x1_d"], w=[xc])
                    k.dma(gc[:], modrow[0:1, 5 * D + nb * 512:5 * D + (nb + 1) * 512].partition_broadcast(128), r=["mod_d"], w=[gc])
                    k.op("dve", lambda e: e.tensor_tensor(out=h2f[:, cs], in0=PB[nb][:], in1=gc[:], op=ALU.mult), r=[PB[nb], gc, h2f], w=[("yo", nb)])
                    k.op("pool", lambda e: e.tensor_tensor(out=h2f[:, cs], in0=h2f[:, cs], in1=xc[:], op=ALU.add), r=[("yo", nb), xc], w=[("yo", nb)])
                k.dma(out[rows, :], h2f[:], r=[("yo", nb) for nb in range(8)], w=["out", h2f])

            prep(0)
            for s_ in range(128):
                u_step(0, s_)
            post(0)
            for ti in range(16):
                nxt = ti + 1 < 16
                if nxt:
                    prep(ti + 1)
                for s_ in range(128):
                    if nxt:
                        u_step(ti + 1, s_)
                    v_step(ti, s_)
                evac(ti)
                if nxt:
                    post(ti + 1)
        k.barrier()

    k.finish()
    es.close()
    return nc, k


def prep_core_inputs(inp, b, consts):
    f = lambda a: np.ascontiguousarray(a, dtype=np.float32)
    d = {}
    d["x"] = f(inp["x"][b])
    d["c_l"] = f(inp["c"][b].reshape(KC, 128).T)
    d["w_ada"] = f(inp["w_ada"][0])
    d["b_ada"] = f(inp["b_ada"][0].reshape(1, -1))
    d["g1_l"] = f(inp["norm1_g"][0].reshape(KC, 128).T)
    d["g2_l"] = f(inp["norm2_g"][0].reshape(KC, 128).T)
    d["g2_row"] = f(inp["norm2_g"][0].reshape(1, -1))
    d["w_in"] = f(inp["w_in"][0])
    d["w_out"] = f(inp["w_out"][0])
    d["w_pool"] = f(inp["w_pool"][0])
    d["pscale_l"] = f(inp["pool_scale"][0].reshape(8, 128).T)
    d["qkg_l"] = f(np.concatenate([inp["q_norm_g"][0][None, :], inp["k_norm_g"][0]], 0).T)
    d["pe_l"] = f(np.transpose(inp["cmp_pe"][0], (0, 2, 1)))
    d["cmp_w1"] = f(inp["cmp_w1"][0])
    d["cmp_w2"] = f(inp["cmp_w2"][0])
    d["w_pq"] = f(inp["w_pq"][0])
    d["peer_keys"] = f(inp["peer_keys"][0].reshape(16, 128, 128))
    d["peer_u"] = f(inp["peer_u"][0])
    d["peer_v"] = f(inp["peer_v"][0])
    d.update(consts)
    return d


def sim_deadlock(k):
    streams = {}
    for e in k.log:
        streams.setdefault(e[0], []).append(e)
    pc = {e: 0 for e in streams}
    sem = {}
    progress = True
    while progress:
        progress = False
        for e, lst in streams.items():
            while pc[e] < len(lst):
                _, kind, s, v = lst[pc[e]]
                if kind == "w":
                    if sem.get(s, 0) >= v:
                        pc[e] += 1
                        progress = True
                    else:
                        break
                else:
                    sem[s] = sem.get(s, 0) + v
                    pc[e] += 1
                    progress = True
    stuck = {e: (pc[e], len(l), l[pc[e]] if pc[e] < len(l) else None) for e, l in streams.items() if pc[e] < len(l)}
    return stuck, sem


_CACHE = {}


def kernel(**inputs):
    inp = {n: np.asarray(v) for n, v in inputs.items()}
    consts = make_consts()
    if "nc" not in _CACHE:
        _CACHE["nc"] = build()[0]
    nc = _CACHE["nc"]
    in_maps = [prep_core_inputs(inp, b, consts) for b in range(8)]
    res = run_bass_kernel_spmd(nc, in_maps, core_ids=list(range(8)))
    return np.stack([np.asarray(r["out"], dtype=np.float32) for r in res.results], axis=0)
```

```python
import contextlib
import numpy as np
import concourse.bass as bass
import concourse.mybir as mybir
from concourse.bass_utils import run_bass_kernel_spmd

F32 = mybir.dt.float32
BF16 = mybir.dt.bfloat16
I32 = mybir.dt.int32
U32 = mybir.dt.uint32
AF = mybir.ActivationFunctionType
ALU = mybir.AluOpType

T = 2048
D = 4096
KC = 32
NH = 24
G = 4
R = 6
NCMP = 127
NSEL = 32
INW = 7240
EPS = 1e-6
SCALE = 128 ** -0.5


class MK:
    NLANES = 12

    def __init__(self, nc):
        self.nc = nc
        self.es = contextlib.ExitStack()
        self.eng = {"pe": nc.tensor, "dve": nc.vector, "act": nc.scalar,
                    "pool": nc.gpsimd, "sp": nc.sync}
        self.sem = {}
        self.cnt = {}
        for n in self.eng:
            self.sem[n] = self.es.enter_context(nc.semaphore("sem_" + n))
            self.cnt[n] = 0
        self.lanes = []
        for i in range(self.NLANES):
            n = "lane%d" % i
            self.sem[n] = self.es.enter_context(nc.semaphore(n))
            self.cnt[n] = 0
            self.lanes.append(n)
        self.lane_rr = 0
        self.clanes = []
        for i in range(3):
            n = "clane%d" % i
            self.sem[n] = self.es.enter_context(nc.semaphore(n))
            self.cnt[n] = 0
            self.clanes.append(n)
        self.clane_rr = 0
        self.waited = {}
        self.last_w = {}
        self.readers = {}
        self.ninst = 0
        self.log = []

    @staticmethod
    def key(x):
        if isinstance(x, (str, tuple)):
            return x
        if hasattr(x, "tensor"):
            return x.tensor.name
        return x.name

    def _wait(self, engname, semname, val):
        if val <= 0:
            return
        k = (engname, semname)
        if self.waited.get(k, 0) >= val:
            return
        self.waited[k] = val
        self.eng[engname].wait_ge(self.sem[semname], val)
        self.ninst += 1
        self.log.append((engname, "w", semname, val))

    def _deps(self, engname, r, w):
        need = {}
        for x in r:
            lw = self.last_w.get(self.key(x))
            if lw:
                need[lw[0]] = max(need.get(lw[0], 0), lw[1])
        for x in w:
            k = self.key(x)
            lw = self.last_w.get(k)
            if lw:
                need[lw[0]] = max(need.get(lw[0], 0), lw[1])
            for (s, v) in self.readers.get(k, []):
                need[s] = max(need.get(s, 0), v)
        for s, v in need.items():
            if s == "pe" and engname == "pe":
                continue
            self._wait(engname, s, v)

    def _record(self, tok, r, w):
        for x in r:
            lst = self.readers.setdefault(self.key(x), [])
            lst[:] = [(s, v) for (s, v) in lst if s != tok[0]]
            lst.append(tok)
        for x in w:
            k = self.key(x)
            self.last_w[k] = tok
            self.readers[k] = []

    def op(self, engname, fn, r=(), w=()):
        self._deps(engname, r, w)
        inst = fn(self.eng[engname])
        self.cnt[engname] += 1
        inst.then_inc(self.sem[engname], 1)
        self.ninst += 1
        self.log.append((engname, "i", engname, 1))
        self._record((engname, self.cnt[engname]), r, w)
        return inst

    def _lane(self):
        lane = self.lanes[self.lane_rr]
        self.lane_rr = (self.lane_rr + 1) % self.NLANES
        return lane

    def dma(self, out, in_, r=(), w=(), q="sp", conv=False, **kw):
        if conv:
            lane = self.clanes[self.clane_rr]
            self.clane_rr = (self.clane_rr + 1) % len(self.clanes)
        else:
            lane = self._lane()
        self._deps(q, r, w)
        self._wait(q, lane, self.cnt[lane])
        inst = self.eng[q].dma_start(out=out, in_=in_, **kw)
        self.cnt[lane] += 16
        inst.then_inc(self.sem[lane], 16)
        self.ninst += 1
        self.log.append((q, "i", lane, 16))
        self._record((lane, self.cnt[lane]), r, w)
        return inst

    def gather(self, out, table, idx_ap, r=(), w=()):
        lane = self._lane()
        q = "pool"
        self._deps(q, r, w)
        self._wait(q, lane, self.cnt[lane])
        inst = self.nc.gpsimd.indirect_dma_start(
            out=out, out_offset=None, in_=table,
            in_offset=bass.IndirectOffsetOnAxis(ap=idx_ap, axis=0))
        self.cnt[lane] += 16
        inst.then_inc(self.sem[lane], 16)
        self.ninst += 1
        self.log.append((q, "i", lane, 16))
        self._record((lane, self.cnt[lane]), r, w)
        return inst

    def barrier(self):
        for e in self.eng:
            for s in self.sem:
                if s == e:
                    continue
                self._wait(e, s, self.cnt[s])
        self.last_w = {}
        self.readers = {}

    def finish(self, engname="sp"):
        for s in self.sem:
            self._wait(engname, s, self.cnt[s])

    def sb(self, stack, name, shape, dt):
        return stack.enter_context(self.nc.sbuf_tensor("s_" + name, shape, dt))

    def ps(self, stack, name, shape, dt=F32):
        return stack.enter_context(self.nc.psum_tensor("p_" + name, shape, dt))


def make_consts():
    c = {}
    c["ident_f"] = np.eye(128, dtype=np.float32)
    c["ones_f"] = np.ones((128, 128), np.float32)
    rot = np.zeros((128, 128), np.float32)
    for m in range(128):
        rot[(m + 64) % 128, m] = 1.0
    c["rotm"] = rot
    half = 64
    inv = (10000.0 ** (-np.arange(half, dtype=np.float32) / half)).astype(np.float32)
    pos = np.arange(T, dtype=np.float32)
    ang = (pos[:, None] * inv[None, :]).astype(np.float32)
    cos = np.cos(ang).astype(np.float32).T
    sin = np.sin(ang).astype(np.float32).T
    c["cosT"] = np.concatenate([cos, cos], 0).astype(np.float32)
    c["sinT"] = np.concatenate([-sin, sin], 0).astype(np.float32)
    n = np.arange(128)
    t = np.arange(T)
    cm = ((16 * n[:, None] + 31) <= t[None, :]) & (n[:, None] < NCMP)
    c["cmpmask"] = cm.astype(np.float32)
    cst = np.arange(NCMP)[:, None] * 16
    sst = np.arange(NSEL)[None, :] * 64
    ov = np.clip(np.minimum(cst + 32, sst + 64) - np.maximum(cst, sst), 0, None)
    sm = np.zeros((128, NSEL), np.float32)
    sm[:NCMP] = ov / 32.0
    c["selmap"] = sm
    j = np.arange(NSEL)
    cur = t // 64
    forced = (j[None, :] == 0) | (j[None, :] == cur[:, None]) | (j[None, :] == cur[:, None] - 1)
    valid = (j[None, :] * 64) <= t[:, None]
    c["tb"] = (np.where(forced, 1000.0, 0.0) + np.where(valid, 0.0, -1e9)).astype(np.float32)
    eb = np.zeros((NSEL, 16, 128), np.float32)
    for m in range(16):
        for kk in range(128):
            eb[2 * m + kk // 64, m, kk] = 1.0
    c["eb"] = eb
    kk = np.arange(128)[:, None]
    q = np.arange(512)[None, :]
    caus = np.zeros((128, 4, 512), np.float32)
    for d in range(4):
        caus[:, d, :] = ((q - kk) >= 128 * d)
    c["caus"] = caus
    wm = np.zeros((128, 8, 512), np.float32)
    for d in range(-4, 4):
        dist = q - kk - 128 * d
        wm[:, d + 4, :] = (dist >= 0) & (dist < 512)
    c["wmask"] = wm
    ic = np.zeros((128, 4, T), np.float32)
    for gi, w in enumerate((2, 4, 8, 16)):
        ic[:, gi, :] = 1.0 / np.minimum(t + 1, w).astype(np.float32)[None, :]
    c["invcnt"] = ic
    c["iota256"] = np.tile(np.arange(256, dtype=np.float32)[None, :], (128, 1))
    return c


CONST_SHAPES = {
    "ident_f": [128, 128], "ones_f": [128, 128], "rotm": [128, 128],
    "cosT": [128, T], "sinT": [128, T], "cmpmask": [128, T], "selmap": [128, NSEL],
    "tb": [T, NSEL], "eb": [NSEL, 16, 128], "caus": [128, 4, 512], "wmask": [128, 8, 512],
    "invcnt": [128, 4, T], "iota256": [128, 256],
}

INPUT_SHAPES = {
    "x": [T, D], "c_l": [128, KC], "w_ada": [D, 6 * D], "b_ada": [1, 6 * D],
    "g1_l": [128, KC], "g2_l": [128, KC], "g2_row": [1, D],
    "w_in": [D, INW], "w_out": [D, D], "w_pool": [4, 256, 256], "pscale_l": [128, 8],
    "qkg_l": [128, 4], "pe_l": [2, 128, 32], "cmp_w1": [2, 32, 128, 256], "cmp_w2": [2, 256, 128],
    "w_pq": [D, 2048], "peer_keys": [16, 128, 128], "peer_u": [16384, D], "peer_v": [16384, D],
}

SCRATCH = {
    "mod_d": ([6 * D], F32),
    "pT_d": ([1024, T], F32),
    "qn_d": ([NH, 128, T], BF16),
    "qr_d": ([NH, 128, T], BF16),
    "kc_d": ([2, G, 128, T], BF16),
    "ks_d": ([G, 128, T], BF16),
    "kw_d": ([G, 128, T], BF16),
    "vs_d": ([T, 512], BF16),
    "vw_d": ([T, 512], BF16),
    "gT_d": ([72, T], F32),
    "yT_d": ([D, T], BF16),
    "x1_d": ([T, D], F32),
    "eidx_d": ([T, 128], I32),
    "gts_d": ([T, 128], F32),
    "ef_d": ([T, 128], F32),
}
BIG_SCRATCH = {
    "u16_d": ([16384, D], BF16),
    "v16_d": ([16384, D], BF16),
    "wi16_d": ([D, INW], BF16),
    "wo16_d": ([D, D], BF16),
    "wq16_d": ([D, 2048], BF16),
}


def build(phases=("mod", "inproj", "pool", "cmp", "attn", "outproj", "peer"), debug=False,
          inputs_needed=None, ntb=4, dbg=99):
    nc = bass.Bass("TRN2", target_bir_lowering=False)
    k = MK(nc)
    es = k.es
    ins = {}
    for name, shp in list(INPUT_SHAPES.items()) + list(CONST_SHAPES.items()):
        if inputs_needed is not None and name not in inputs_needed:
            continue
        ins[name] = nc.dram_tensor(name, shp, F32, kind="ExternalInput").ap()
    out = nc.dram_tensor("out", [T, D], F32, kind="ExternalOutput").ap()
    sc = {}
    for name, (shp, dt) in SCRATCH.items():
        sc[name] = nc.dram_tensor(name, shp, dt, kind="ExternalOutput" if debug else "Internal").ap()
    for name, (shp, dt) in BIG_SCRATCH.items():
        sc[name] = nc.dram_tensor(name, shp, dt, kind="Internal").ap()
    conv_jobs = []
    if "peer" in phases:
        for i in range(32):
            rs_ = slice(i * 512, (i + 1) * 512)
            conv_jobs.append(("u16_d", "peer_u", rs_))
            conv_jobs.append(("v16_d", "peer_v", rs_))

    def conv_step(n=1):
        for _ in range(n):
            if conv_jobs:
                dn, sn, rs_ = conv_jobs.pop(0)
                k.dma(sc[dn][rs_, :], ins[sn][rs_, :], w=[dn], q="pool", conv=True)

    ident_f = k.sb(es, "ident_f_sb", [128, 128], F32)
    ident_b = k.sb(es, "ident_b_sb", [128, 128], BF16)
    ones_f = k.sb(es, "ones_f_sb", [128, 128], F32)
    ones_b = k.sb(es, "ones_b_sb", [128, 128], BF16)
    modT = k.sb(es, "modT", [128, 192], F32)
    A1 = k.sb(es, "A1", [128, KC], F32)
    A2 = k.sb(es, "A2", [128, KC], F32)
    kcmpT = k.sb(es, "kcmpT", [128, G, 128], BF16)
    vcmp = k.sb(es, "vcmp", [128, G, 128], BF16)
    PB = [k.ps(es, "bank%d" % i, [128, 512]) for i in range(8)]
    PBb = [b.bitcast(BF16) for b in PB]
    k.dma(ident_f[:], ins["ident_f"][:, :], w=[ident_f])
    k.dma(ones_f[:], ins["ones_f"][:, :], w=[ones_f])
    k.op("dve", lambda e: e.tensor_copy(out=ident_b[:], in_=ident_f[:]), r=[ident_f], w=[ident_b])
    k.op("dve", lambda e: e.tensor_copy(out=ones_b[:], in_=ones_f[:]), r=[ones_f], w=[ones_b])

    wconv_jobs = []
    for dn, sn, nrow, step in (("wi16_d", "w_in", D, 256), ("wo16_d", "w_out", D, 512), ("wq16_d", "w_pq", D, 1024)):
        if sn in ins:
            for r0 in range(0, nrow, step):
                wconv_jobs.append((dn, sn, r0, step))

    def wconv_step(n=1):
        for _ in range(n):
            if wconv_jobs:
                dn, sn, r0, step = wconv_jobs.pop(0)
                k.dma(sc[dn][r0:r0 + step, :], ins[sn][r0:r0 + step, :], w=[dn], q="pool", conv=True)

    if "mod" not in phases:
        wconv_step(1000)

    if "mod" in phases:
        with contextlib.ExitStack() as st:
            cl = k.sb(st, "cl", [128, KC], F32)
            scl = k.sb(st, "scl", [128, KC], BF16)
            wst = [k.sb(st, "wada_s%d" % i, [128, 16, 512], F32) for i in range(3)]
            wbb = [k.sb(st, "wada_b%d" % i, [128, 16, 512], BF16) for i in range(3)]
            brow = [k.sb(st, "brow%d" % i, [1, 512], F32) for i in range(2)]
            mrow = [k.sb(st, "mrow%d" % i, [1, 512], F32) for i in range(2)]
            psm = [PB[0], PB[1]]
            k.dma(cl[:], ins["c_l"][:, :], w=[cl])
            k.op("act", lambda e: e.activation(out=scl[:], in_=cl[:], func=AF.Silu), r=[cl], w=[scl])
            wv = ins["w_ada"].rearrange("(k p) f -> p k f", p=128)
            modv = sc["mod_d"].rearrange("(a f) -> a f", a=1)
            nh = 0
            for fb in range(48):
                ps = psm[fb % 2]
                br = brow[fb % 2]
                mr = mrow[fb % 2]
                fs = slice(fb * 512, (fb + 1) * 512)
                wconv_step(1)
                k.dma(br[:], ins["b_ada"][0:1, fs], w=[br])
                for hf in range(2):
                    ws_ = wst[nh % 3]
                    w_ = wbb[nh % 3]
                    ce = ("dve", "act", "pool")[nh % 3]
                    nh += 1
                    k.dma(ws_[:], wv[:, hf * 16:(hf + 1) * 16, fs], w=[ws_], q=("sp" if hf == 0 else "act"))
                    if ce == "act":
                        k.op("act", lambda e: e.activation(out=w_[:], in_=ws_[:], func=AF.Identity), r=[ws_], w=[w_])
                    else:
                        k.op(ce, lambda e: e.tensor_copy(out=w_[:], in_=ws_[:]), r=[ws_], w=[w_])
                    for kk in range(16):
                        kg = hf * 16 + kk
                        k.op("pe", lambda e: e.matmul(ps[0:1, :], lhsT=scl[:, kg:kg + 1], rhs=w_[:, kk, :],
                                                      start=(kg == 0), stop=(kg == 31)),
                             r=[scl, w_], w=[ps])
                k.op("dve", lambda e: e.tensor_tensor(out=mr[:], in0=ps[0:1, :], in1=br[:], op=ALU.add),
                     r=[ps, br], w=[mr])
                k.dma(modv[0:1, fs], mr[:], r=[mr], w=["mod_d"])
            wconv_step(1000)
        k.barrier()

    with contextlib.ExitStack() as st:
        m1 = k.sb(st, "m1", [96, 128], F32)
        m2 = k.sb(st, "m2", [96, 128], F32)
        g1 = k.sb(st, "g1l", [128, KC], F32)
        g2 = k.sb(st, "g2l", [128, KC], F32)
        pst = PB[2]
        mv = sc["mod_d"].rearrange("(c p) -> c p", p=128)
        k.dma(m1[:], mv[0:96, :], r=["mod_d"], w=[m1])
        k.dma(m2[:], mv[96:192, :], r=["mod_d"], w=[m2])
        k.dma(g1[:], ins["g1_l"][:, :], w=[g1])
        k.dma(g2[:], ins["g2_l"][:, :], w=[g2])
        k.op("pe", lambda e: e.transpose(out=pst[:, 0:96], in_=m1[:], identity=ident_f[0:96, 0:96]), r=[m1, ident_f], w=[pst])
        k.op("pe", lambda e: e.transpose(out=pst[:, 96:192], in_=m2[:], identity=ident_f[0:96, 0:96]), r=[m2, ident_f], w=[pst])
        k.op("dve", lambda e: e.tensor_copy(out=modT[:], in_=pst[:, 0:192]), r=[pst], w=[modT])
        k.op("dve", lambda e: e.scalar_tensor_tensor(out=A1[:], in0=modT[:, 32:64], scalar=1.0, in1=g1[:],
                                                     op0=ALU.add, op1=ALU.mult), r=[modT, g1], w=[A1])
        k.op("dve", lambda e: e.scalar_tensor_tensor(out=A2[:], in0=modT[:, 128:160], scalar=1.0, in1=g2[:],
                                                     op0=ALU.add, op1=ALU.mult), r=[modT, g2], w=[A2])
    k.barrier()
    B1 = modT[:, 0:32]
    B2 = modT[:, 96:128]

    def hT_keys():
        return [("hT", kk, tt) for kk in range(KC) for tt in range(4)]

    def norm_block(st_bufs, src, tb, Atab, Btab, hT):
        xt, xn, junk, ss, pT = st_bufs
        for tt in range(4):
            t0 = tb * 512 + tt * 128
            k.dma(xt[:], src[t0:t0 + 128, :], w=[xt])
            k.op("dve", lambda e: e.memset(ss[:], 0.0), w=[ss])
            k.op("act", lambda e: e.activation(out=junk[:], in_=xt[:], func=AF.Square, accum_out=ss[:, 0:1]),
                 r=[xt, ss], w=[junk, ss])
            k.op("dve", lambda e: e.tensor_scalar(out=ss[:, 1:2], in0=ss[:, 0:1], scalar1=1.0 / D, scalar2=EPS,
                                                  op0=ALU.mult, op1=ALU.add), r=[ss], w=[ss])
            k.op("act", lambda e: e.activation(out=ss[:, 3:4], in_=ss[:, 1:2], func=AF.Sqrt), r=[ss], w=[ss])
            k.op("dve", lambda e: e.reciprocal(out=ss[:, 2:3], in_=ss[:, 3:4]), r=[ss], w=[ss])
            k.op("dve", lambda e: e.tensor_scalar(out=xn[:], in0=xt[:], scalar1=ss[:, 2:3], scalar2=None,
                                                  op0=ALU.mult), r=[xt, ss], w=[xn])
            if dbg == -1:
                continue
            for kg in range(KC // 4):
                p = pT[kg % 2]
                for q4 in range(4):
                    kk = kg * 4 + q4
                    sl = slice(q4 * 128, q4 * 128 + 128)
                    k.op("pe", lambda e: e.transpose(out=p[:, sl], in_=xn[:, kk * 128:(kk + 1) * 128], identity=ident_b[:]),
                         r=[xn, ident_b], w=[p])
                for q4 in range(4):
                    kk = kg * 4 + q4
                    sl = slice(q4 * 128, q4 * 128 + 128)
                    k.op("dve", lambda e: e.tensor_scalar(out=hT[:, kk, tt * 128:(tt + 1) * 128], in0=p[:, sl],
                                                          scalar1=Atab[:, kk:kk + 1], scalar2=Btab[:, kk:kk + 1],
                                                          op0=ALU.mult, op1=ALU.add),
                         r=[p], w=[hT])

    if "inproj" in phases:
        with contextlib.ExitStack() as st:
            xt = k.sb(st, "xt", [128, D], F32)
            xn = k.sb(st, "xn", [128, D], BF16)
            junk = k.sb(st, "junk", [128, D], BF16)
            ss = k.sb(st, "ss", [128, 4], F32)
            pT = [PBb[0], PBb[1]]
            hT = k.sb(st, "hT", [128, KC, 512], BF16)
            W = [k.sb(st, "W%d" % i, [128, KC, 512], BF16) for i in range(2)]
            cosT = k.sb(st, "cosT", [128, T], F32)
            sinT = k.sb(st, "sinT", [128, T], F32)
            rotm = k.sb(st, "rotm", [128, 128], F32)
            qkg = k.sb(st, "qkg", [128, 4], F32)
            psA = [PB[2], PB[3]]
            ps2_ = [PB[4], PB[6]]
            ps3_ = [PB[5], PB[7]]
            raw_ = [k.sb(st, "raw%d" % i, [128, 512], F32) for i in range(2)]
            sq_ = [k.sb(st, "sq%d" % i, [128, 512], F32) for i in range(2)]
            rs_ = [k.sb(st, "rs%d" % i, [128, 512], F32) for i in range(2)]
            qn_ = [k.sb(st, "qn%d" % i, [128, 512], F32) for i in range(2)]
            t1_ = [k.sb(st, "t1%d" % i, [128, 512], F32) for i in range(2)]
            t2_ = [k.sb(st, "t2%d" % i, [128, 512], F32) for i in range(2)]
            oqr_ = [k.sb(st, "oqr%d" % i, [128, 512], BF16) for i in range(2)]
            ecnt = [0]
            ob = [k.sb(st, "ob%d" % i, [128, 512], BF16) for i in range(2)]
            of = [k.sb(st, "of%d" % i, [128, 512], F32) for i in range(2)]
            k.dma(cosT[:], ins["cosT"][:, :], w=[cosT])
            k.dma(sinT[:], ins["sinT"][:, :], w=[sinT])
            k.dma(rotm[:], ins["rotm"][:, :], w=[rotm])
            k.dma(qkg[:], ins["qkg_l"][:, :], w=[qkg])
            wv = sc["wi16_d"].rearrange("(k p) f -> p k f", p=128)
            cnt = [0]
            for tb in range(ntb):
                ts = slice(tb * 512, (tb + 1) * 512)
                if dbg != 0:
                    norm_block((xt, xn, junk, ss, pT), ins["x"], tb, A1, B1, hT)
                if dbg <= 1:
                    continue
                for ct in range(15):
                    if dbg == 2 and ct > 0:
                        continue
                    if dbg == 3 and ct not in (2,):
                        continue
                    if dbg == 4 and ct not in (11,):
                        continue
                    if dbg == 5 and ct not in (14,):
                        continue
                    if dbg == 6 and ct not in (8,):
                        continue
                    ncol = 512 if ct < 14 else 72
                    Wt = W[ct % 2]
                    for pc in range(4):
                        k.dma(Wt[:, pc * 8:(pc + 1) * 8, 0:ncol], wv[:, pc * 8:(pc + 1) * 8, ct * 512:ct * 512 + ncol], r=["wi16_d"], w=[Wt],
                              q=("sp" if pc % 2 == 0 else "act"))
                    if ct in (11, 13):
                        dst = sc["vs_d"] if ct == 11 else sc["vw_d"]
                        for tt in range(4):
                            ps = psA[cnt[0] % 2]
                            o = ob[cnt[0] % 2]
                            cnt[0] += 1
                            for kk in range(KC):
                                k.op("pe", lambda e: e.matmul(ps[:], lhsT=hT[:, kk, tt * 128:(tt + 1) * 128], rhs=Wt[:, kk, :],
                                                              start=(kk == 0), stop=(kk == KC - 1)), r=[hT, Wt], w=[ps])
                            k.op("act", lambda e: e.activation(out=o[:], in_=ps[:], func=AF.Identity), r=[ps], w=[o])
                            k.dma(dst[tb * 512 + tt * 128: tb * 512 + (tt + 1) * 128, :], o[:], r=[o], w=[dst])
                        continue
                    nsub = (ncol + 127) // 128
                    for sub in range(nsub):
                        mrows = min(128, ncol - sub * 128)
                        ps = psA[cnt[0] % 2]
                        o = ob[cnt[0] % 2]
                        o32 = of[cnt[0] % 2]
                        cnt[0] += 1
                        for kk in range(KC):
                            k.op("pe", lambda e: e.matmul(ps[0:mrows, :], lhsT=Wt[:, kk, sub * 128:sub * 128 + mrows], rhs=hT[:, kk, :],
                                                          start=(kk == 0), stop=(kk == KC - 1)), r=[hT, Wt], w=[ps])
                        fc = ct * 4 + sub
                        if ct < 2:
                            k.op("act", lambda e: e.activation(out=o32[:], in_=ps[:], func=AF.Identity), r=[ps], w=[o32])
                            k.dma(sc["pT_d"][fc * 128:(fc + 1) * 128, ts], o32[:], r=[o32], w=["pT_d"])
                        elif ct in (8, 9):
                            k.op("act", lambda e: e.activation(out=o[:], in_=ps[:], func=AF.Identity), r=[ps], w=[o])
                            k.dma(sc["kc_d"][ct - 8, sub, :, ts], o[:], r=[o], w=["kc_d"])
                        elif ct == 14:
                            k.op("act", lambda e: e.activation(out=o32[0:72, :], in_=ps[0:72, :], func=AF.Sigmoid), r=[ps], w=[o32])
                            k.dma(sc["gT_d"][:, ts], o32[0:72, :], r=[o32], w=["gT_d"])
                        else:
                            if ct < 8:
                                gcol = 0
                            elif ct == 10:
                                gcol = 2
                            else:
                                gcol = 3
                            ei_ = ecnt[0] % 2
                            ecnt[0] += 1
                            raw, sq, rs, qn, t1, t2, oqr = raw_[ei_], sq_[ei_], rs_[ei_], qn_[ei_], t1_[ei_], t2_[ei_], oqr_[ei_]
                            ps2, ps3 = ps2_[ei_], ps3_[ei_]
                            k.op("act", lambda e: e.activation(out=raw[:], in_=ps[:], func=AF.Identity), r=[ps], w=[raw])
                            k.op("act", lambda e: e.activation(out=sq[:], in_=ps[:], func=AF.Square), r=[ps], w=[sq])
                            k.op("pe", lambda e: e.matmul(ps2[:], lhsT=ones_f[:], rhs=sq[:], start=True, stop=True), r=[ones_f, sq], w=[ps2])
                            k.op("dve", lambda e: e.tensor_scalar(out=rs[:], in0=ps2[:], scalar1=1.0 / 128, scalar2=EPS,
                                                                  op0=ALU.mult, op1=ALU.add), r=[ps2], w=[rs])
                            k.op("act", lambda e: e.activation(out=sq[:], in_=rs[:], func=AF.Sqrt), r=[rs], w=[sq])
                            k.op("dve", lambda e: e.reciprocal(out=rs[:], in_=sq[:]), r=[sq], w=[rs])
                            k.op("dve", lambda e: e.scalar_tensor_tensor(out=qn[:], in0=raw[:], scalar=qkg[:, gcol:gcol + 1], in1=rs[:],
                                                                         op0=ALU.mult, op1=ALU.mult), r=[raw, qkg, rs], w=[qn])
                            if ct < 8:
                                h = fc - 8
                                k.op("act", lambda e: e.activation(out=o[:], in_=qn[:], func=AF.Identity), r=[qn], w=[o])
                                k.dma(sc["qn_d"][h, :, ts], o[:], r=[o], w=["qn_d"])
                            k.op("pe", lambda e: e.matmul(ps3[:], lhsT=rotm[:], rhs=qn[:], start=True, stop=True), r=[rotm, qn], w=[ps3])
                            k.op("dve", lambda e: e.tensor_tensor(out=t1[:], in0=qn[:], in1=cosT[:, ts], op=ALU.mult), r=[qn, cosT], w=[t1])
                            k.op("dve", lambda e: e.tensor_tensor(out=t2[:], in0=ps3[:], in1=sinT[:, ts], op=ALU.mult), r=[ps3, sinT], w=[t2])
                            k.op("dve", lambda e: e.tensor_tensor(out=t1[:], in0=t1[:], in1=t2[:], op=ALU.add), r=[t1, t2], w=[t1])
                            ob2 = oqr
                            k.op("act", lambda e: e.activation(out=ob2[:], in_=t1[:], func=AF.Identity), r=[t1], w=[ob2])
                            if ct < 8:
                                k.dma(sc["qr_d"][fc - 8, :, ts], ob2[:], r=[ob2], w=["qr_d"])
                            elif ct == 10:
                                k.dma(sc["ks_d"][sub, :, ts], ob2[:], r=[ob2], w=["ks_d"])
                            else:
                                k.dma(sc["kw_d"][sub, :, ts], ob2[:], r=[ob2], w=["kw_d"])
        k.barrier()


    if "pool" in phases:
        with contextlib.ExitStack() as st:
            pt = k.sb(st, "pl_pt", [128, T], F32)
            sa = k.sb(st, "pl_sa", [128, T], F32)
            sb_ = k.sb(st, "pl_sb", [128, T], F32)
            inv = k.sb(st, "pl_inv", [128, 4, T], F32)
            dT = k.sb(st, "pl_dT", [128, 2, T], BF16)
            wp = k.sb(st, "pl_wp", [128, 2, 256], F32)
            wpb = k.sb(st, "pl_wpb", [128, 2, 256], BF16)
            psl = k.sb(st, "pl_psl", [128, 8], F32)
            yo = [k.sb(st, "pl_yo%d" % i, [128, 512], BF16) for i in range(2)]
            k.dma(inv[:], ins["invcnt"][:, :, :], w=[inv])
            k.dma(psl[:], ins["pscale_l"][:, :], w=[psl])
            cnt = 0
            for gi in range(4):
                wwin = (2, 4, 8, 16)[gi]
                k.dma(wp[:], ins["w_pool"][gi].rearrange("(cc p) d -> p cc d", p=128), w=[wp])
                k.op("dve", lambda e: e.tensor_copy(out=wpb[:], in_=wp[:]), r=[wp], w=[wpb])
                for cc in range(2):
                    ch = gi * 2 + cc
                    k.dma(pt[:], sc["pT_d"][ch * 128:(ch + 1) * 128, :], r=["pT_d"], w=[pt])
                    cur = pt
                    bufs = [sa, sb_]
                    bi = 0
                    sh = 1
                    while sh < wwin:
                        nxt = bufs[bi]
                        bi ^= 1
                        k.op("dve", lambda e: e.tensor_tensor(out=nxt[:, sh:T], in0=cur[:, sh:T], in1=cur[:, 0:T - sh], op=ALU.add),
                             r=[cur], w=[nxt])
                        k.op("dve", lambda e: e.tensor_copy(out=nxt[:, 0:sh], in_=cur[:, 0:sh]), r=[cur], w=[nxt])
                        cur = nxt
                        sh *= 2
                    tmp = bufs[bi]
                    k.op("dve", lambda e: e.tensor_tensor(out=tmp[:], in0=cur[:], in1=inv[:, gi, :], op=ALU.mult), r=[cur, inv], w=[tmp])
                    k.op("dve", lambda e: e.tensor_tensor(out=dT[:, cc, :], in0=tmp[:], in1=pt[:], op=ALU.subtract), r=[tmp, pt], w=[dT])
                for dc in range(2):
                    for tb in range(4):
                        ps = PB[cnt % 2]
                        o = yo[cnt % 2]
                        cnt += 1
                        for cc in range(2):
                            k.op("pe", lambda e: e.matmul(ps[:], lhsT=wpb[:, cc, dc * 128:(dc + 1) * 128], rhs=dT[:, cc, tb * 512:(tb + 1) * 512],
                                                          start=(cc == 0), stop=(cc == 1)), r=[wpb, dT], w=[ps])
                        col = gi * 2 + dc
                        k.op("dve", lambda e: e.tensor_scalar(out=o[:], in0=ps[:], scalar1=psl[:, col:col + 1], scalar2=None, op0=ALU.mult),
                             r=[ps, psl], w=[o])
                        k.dma(sc["yT_d"][col * 128:(col + 1) * 128, tb * 512:(tb + 1) * 512], o[:], r=[o], w=["yT_d"])
        k.barrier()

    k.op("dve", lambda e: e.memset(kcmpT[:], 0.0), w=[kcmpT])
    k.op("dve", lambda e: e.memset(vcmp[:], 0.0), w=[vcmp])
    if "cmp" in phases:
        with contextlib.ExitStack() as st:
            src = k.sb(st, "cp_src", [128, T], BF16)
            w1s = [k.sb(st, "cp_w1s%d" % i, [128, 8, 256], F32) for i in range(2)]
            w1b = k.sb(st, "cp_w1b", [128, 32, 256], BF16)
            w2s = k.sb(st, "cp_w2s", [128, 2, 128], F32)
            w2b = k.sb(st, "cp_w2b", [128, 2, 128], BF16)
            pel = k.sb(st, "cp_pel", [128, 32], F32)
            peb = k.sb(st, "cp_peb", [128, 32], BF16)
            hidT = k.sb(st, "cp_hidT", [128, 2, 128], BF16)
            hpre = k.sb(st, "cp_hpre", [128, 128], F32)
            bias = k.sb(st, "cp_bias", [128, 2], F32)
            kf = k.sb(st, "cp_kf", [128, 128], F32)
            sq = k.sb(st, "cp_sq", [128, 128], F32)
            rs = k.sb(st, "cp_rs", [128, 128], F32)
            qkg = k.sb(st, "cp_qkg", [128, 4], F32)
            k.dma(qkg[:], ins["qkg_l"][:, :], w=[qkg])
            k.op("dve", lambda e: e.memset(hidT[:], 0.0), w=[hidT])
            for kv in range(2):
                w1v = ins["cmp_w1"][kv].rearrange("l d h -> d l h")
                for pc in range(4):
                    stg = w1s[pc % 2]
                    k.dma(stg[:], w1v[:, pc * 8:(pc + 1) * 8, :], w=[stg])
                    k.op("pool", lambda e: e.tensor_copy(out=w1b[:, pc * 8:(pc + 1) * 8, :], in_=stg[:]), r=[stg], w=[w1b])
                k.dma(w2s[:], ins["cmp_w2"][kv].rearrange("(hc p) d -> p hc d", p=128), w=[w2s])
                k.op("dve", lambda e: e.tensor_copy(out=w2b[:], in_=w2s[:]), r=[w2s], w=[w2b])
                k.dma(pel[:], ins["pe_l"][kv], w=[pel])
                k.op("dve", lambda e: e.tensor_copy(out=peb[:], in_=pel[:]), r=[pel], w=[peb])
                for hc in range(2):
                    ps = PB[4]
                    for l in range(32):
                        k.op("pe", lambda e: e.matmul(ps[:, 0:1], lhsT=w1b[:, l, hc * 128:(hc + 1) * 128], rhs=peb[:, l:l + 1],
                                                      start=(l == 0), stop=(l == 31)), r=[w1b, peb], w=[ps])
                    k.op("dve", lambda e: e.tensor_copy(out=bias[:, hc:hc + 1], in_=ps[:, 0:1]), r=[ps], w=[bias])
                for g in range(G):
                    k.dma(src[:], sc["kc_d"][kv, g, :, :], r=["kc_d"], w=[src])
                    for hc in range(2):
                        ps = PB[hc]
                        for l in range(32):
                            k.op("pe", lambda e: e.matmul(ps[:, 0:127], lhsT=w1b[:, l, hc * 128:(hc + 1) * 128],
                                                          rhs=src[:, l:l + 16 * 126 + 1:16],
                                                          start=(l == 0), stop=(l == 31)), r=[w1b, src], w=[ps])
                        k.op("dve", lambda e: e.tensor_scalar(out=hpre[:, 0:127], in0=ps[:, 0:127], scalar1=bias[:, hc:hc + 1], scalar2=None,
                                                              op0=ALU.add), r=[ps, bias], w=[hpre])
                        k.op("act", lambda e: e.activation(out=hidT[:, hc, 0:127], in_=hpre[:, 0:127], func=AF.Gelu), r=[hpre], w=[hidT])
                    if kv == 0:
                        ps = PB[2]
                        for hc in range(2):
                            k.op("pe", lambda e: e.matmul(ps[:, 0:127], lhsT=w2b[:, hc, :], rhs=hidT[:, hc, 0:127],
                                                          start=(hc == 0), stop=(hc == 1)), r=[w2b, hidT], w=[ps])
                        k.op("act", lambda e: e.activation(out=kf[:, 0:127], in_=ps[:, 0:127], func=AF.Identity), r=[ps], w=[kf])
                        k.op("act", lambda e: e.activation(out=sq[:, 0:127], in_=ps[:, 0:127], func=AF.Square), r=[ps], w=[sq])
                        k.op("pe", lambda e: e.matmul(PB[3][:, 0:127], lhsT=ones_f[:], rhs=sq[:, 0:127], start=True, stop=True),
                             r=[ones_f, sq], w=[PB[3]])
                        k.op("dve", lambda e: e.tensor_scalar(out=rs[:, 0:127], in0=PB[3][:, 0:127], scalar1=1.0 / 128, scalar2=EPS,
                                                              op0=ALU.mult, op1=ALU.add), r=[PB[3]], w=[rs])
                        k.op("act", lambda e: e.activation(out=sq[:, 0:127], in_=rs[:, 0:127], func=AF.Sqrt), r=[rs], w=[sq])
                        k.op("dve", lambda e: e.reciprocal(out=rs[:, 0:127], in_=sq[:, 0:127]), r=[sq], w=[rs])
                        k.op("dve", lambda e: e.scalar_tensor_tensor(out=kcmpT[:, g, 0:127], in0=kf[:, 0:127], scalar=qkg[:, 1:2],
                                                                     in1=rs[:, 0:127], op0=ALU.mult, op1=ALU.mult),
                             r=[kf, qkg, rs], w=[kcmpT])
                    else:
                        ps = PB[2]
                        for hc in range(2):
                            k.op("pe", lambda e: e.matmul(ps[0:127, 0:128], lhsT=hidT[:, hc, 0:127], rhs=w2b[:, hc, :],
                                                          start=(hc == 0), stop=(hc == 1)), r=[w2b, hidT], w=[ps])
                        k.op("act", lambda e: e.activation(out=vcmp[0:127, g, :], in_=ps[0:127, 0:128], func=AF.Identity), r=[ps], w=[vcmp])
        k.barrier()

    if "attn" in phases:
        with contextlib.ExitStack() as st:
            ksT = k.sb(st, "at_ksT", [128, T], BF16)
            kwT = k.sb(st, "at_kwT", [128, T], BF16)
            vs = k.sb(st, "at_vs", [128, 16, 128], BF16)
            vw = k.sb(st, "at_vw", [128, 16, 128], BF16)
            qn6 = k.sb(st, "at_qn6", [128, R, 512], BF16)
            qr6 = k.sb(st, "at_qr6", [128, R, 512], BF16)
            cmpm = k.sb(st, "at_cmpm", [128, T], F32)
            selmap_s = k.sb(st, "at_selmap", [128, NSEL], F32)
            tbl = k.sb(st, "at_tbl", [128, 16, NSEL], F32)
            eb = k.sb(st, "at_eb", [NSEL, 16, 128], F32)
            caus = k.sb(st, "at_caus", [128, 4, 512], F32)
            wmask = k.sb(st, "at_wmask", [128, 8, 512], F32)
            Pf = k.sb(st, "at_Pf", [128, 512], F32)
            Pn = k.sb(st, "at_Pn", [128, 512], F32)
            rden = k.sb(st, "at_rden", [128, 512], F32)
            Pb = [k.sb(st, "at_Pb%d" % i, [128, 512], BF16) for i in range(5)]
            score4 = k.sb(st, "at_score4", [128, 4, NSEL], F32)
            work = k.sb(st, "at_work", [128, NSEL], F32)
            m8 = k.sb(st, "at_m8", [128, 16], F32)
            selm = k.sb(st, "at_selm", [128, NSEL], F32)
            selT = k.sb(st, "at_selT", [NSEL, T], F32)
            M = k.sb(st, "at_M", [128, 16, 512], BF16)
            gb2 = [k.sb(st, "at_gb%d" % i, [128, 3, 512], F32) for i in range(2)]
            yacc = k.sb(st, "at_y", [128, 512], F32)
            wgt = k.sb(st, "at_wgt", [128, 512], F32)
            tmpo = k.sb(st, "at_tmpo", [128, 512], F32)
            yb = k.sb(st, "at_yb", [128, 512], BF16)
            k.dma(cmpm[:], ins["cmpmask"][:, :], w=[cmpm])
            k.dma(selmap_s[:], ins["selmap"][:, :], w=[selmap_s])
            k.dma(tbl[:], ins["tb"].rearrange("(tt p) j -> p tt j", p=128), w=[tbl])
            k.dma(eb[:], ins["eb"][:, :, :], w=[eb])
            k.dma(caus[:], ins["caus"][:, :, :], w=[caus])
            k.dma(wmask[:], ins["wmask"][:, :, :], w=[wmask])

            bcount = [0]

            def combine(b, O, Dn, gb):
                k.op("dve", lambda e: e.tensor_scalar(out=wgt[:], in0=Dn[:], scalar1=1e-30, scalar2=None, op0=ALU.max), r=[Dn], w=[wgt])
                k.op("dve", lambda e: e.reciprocal(out=wgt[:], in_=wgt[:]), r=[wgt], w=[wgt])
                k.op("dve", lambda e: e.tensor_tensor(out=wgt[:], in0=wgt[:], in1=gb[:, b, :], op=ALU.mult), r=[wgt, gb], w=[wgt])
                if b == 0:
                    k.op("dve", lambda e: e.tensor_tensor(out=yacc[:], in0=O[:], in1=wgt[:], op=ALU.mult), r=[O, wgt], w=[yacc])
                else:
                    k.op("dve", lambda e: e.tensor_tensor(out=tmpo[:], in0=O[:], in1=wgt[:], op=ALU.mult), r=[O, wgt], w=[tmpo])
                    k.op("pool", lambda e: e.tensor_tensor(out=yacc[:], in0=yacc[:], in1=tmpo[:], op=ALU.add), r=[yacc, tmpo], w=[yacc])

            for g in range(G):
                k.dma(ksT[:], sc["ks_d"][g, :, :], r=["ks_d"], w=[ksT])
                k.dma(kwT[:], sc["kw_d"][g, :, :], r=["kw_d"], w=[kwT])
                k.dma(vs[:], sc["vs_d"][:, g * 128:(g + 1) * 128].rearrange("(m p) d -> p m d", p=128), r=["vs_d"], w=[vs])
                k.dma(vw[:], sc["vw_d"][:, g * 128:(g + 1) * 128].rearrange("(m p) d -> p m d", p=128), r=["vw_d"], w=[vw])
                for c in range(4):
                    ts = slice(c * 512, (c + 1) * 512)
                    k.dma(qn6[:], sc["qn_d"][g * R:(g + 1) * R, :, ts].rearrange("r d t -> d r t"), r=["qn_d"], w=[qn6])
                    for r_ in range(R):
                        S = PB[r_ % 2]
                        k.op("pe", lambda e: e.matmul(S[:], lhsT=kcmpT[:, g, :], rhs=qn6[:, r_, :], start=True, stop=True), r=[kcmpT, qn6], w=[S])
                        k.op("act", lambda e: e.activation(out=Pf[:], in_=S[:], func=AF.Exp, scale=SCALE), r=[S], w=[Pf])
                        k.op("dve", lambda e: e.tensor_tensor(out=Pf[:], in0=Pf[:], in1=cmpm[:, ts], op=ALU.mult), r=[Pf, cmpm], w=[Pf])
                        k.op("pe", lambda e: e.matmul(PB[2][:], lhsT=ones_f[:], rhs=Pf[:], start=True, stop=True), r=[ones_f, Pf], w=[PB[2]])
                        k.op("dve", lambda e: e.tensor_scalar(out=rden[:], in0=PB[2][:], scalar1=1e-30, scalar2=None, op0=ALU.max), r=[PB[2]], w=[rden])
                        k.op("dve", lambda e: e.reciprocal(out=rden[:], in_=rden[:]), r=[rden], w=[rden])
                        k.op("dve", lambda e: e.tensor_tensor(out=Pn[:], in0=Pf[:], in1=rden[:], op=ALU.mult), r=[Pf, rden], w=[Pn])
                        for tt in range(4):
                            k.op("pe", lambda e: e.matmul(PB[3][:, tt * 32:(tt + 1) * 32], lhsT=Pn[:, tt * 128:(tt + 1) * 128], rhs=selmap_s[:, :],
                                                          start=(r_ == 0), stop=(r_ == R - 1)), r=[Pn, selmap_s], w=[PB[3]])
                    k.op("dve", lambda e: e.tensor_tensor(out=score4[:].rearrange("p a b -> p (a b)"), in0=PB[3][:, 0:128],
                                                          in1=tbl[:, c * 4:(c + 1) * 4, :].rearrange("p a b -> p (a b)"), op=ALU.add),
                         r=[PB[3], tbl], w=[score4])
                    for tt in range(4):
                        sc_ = score4[:, tt, :]
                        k.op("dve", lambda e: e.max(out=m8[:, 0:8], in_=sc_), r=[score4], w=[m8])
                        k.op("dve", lambda e: e.match_replace(out=work[:], in_to_replace=m8[:, 0:8], in_values=sc_, imm_value=-3e9), r=[score4, m8], w=[work])
                        k.op("dve", lambda e: e.max(out=m8[:, 8:16], in_=work[:]), r=[work], w=[m8])
                        k.op("dve", lambda e: e.tensor_scalar(out=selm[:], in0=sc_, scalar1=m8[:, 15:16], scalar2=None, op0=ALU.is_ge), r=[score4, m8], w=[selm])
                        k.op("pe", lambda e: e.transpose(out=PB[4][0:NSEL, tt * 128:(tt + 1) * 128], in_=selm[:], identity=ident_f[:]), r=[selm, ident_f], w=[PB[4]])
                    k.op("act", lambda e: e.activation(out=selT[:, ts], in_=PB[4][0:NSEL, :], func=AF.Identity), r=[PB[4]], w=[selT])
                for c in range(4):
                    ts = slice(c * 512, (c + 1) * 512)
                    nm = 4 * c + 4
                    for m in range(nm):
                        k.op("pe", lambda e: e.matmul(PB[7][:], lhsT=eb[:, m, :], rhs=selT[:, ts], start=True, stop=True), r=[eb, selT], w=[PB[7]])
                        if m >= 4 * c:
                            k.op("dve", lambda e: e.tensor_tensor(out=M[:, m, :], in0=PB[7][:], in1=caus[:, m - 4 * c, :], op=ALU.mult), r=[PB[7], caus], w=[("M", m)])
                        else:
                            k.op("act", lambda e: e.activation(out=M[:, m, :], in_=PB[7][:], func=AF.Identity), r=[PB[7]], w=[("M", m)])
                    k.dma(qn6[:], sc["qn_d"][g * R:(g + 1) * R, :, ts].rearrange("r d t -> d r t"), r=["qn_d"], w=[qn6])
                    k.dma(qr6[:], sc["qr_d"][g * R:(g + 1) * R, :, ts].rearrange("r d t -> d r t"), r=["qr_d"], w=[qr6])
                    jobs = []
                    for r_ in range(R):
                        h = g * R + r_
                        jobs.append(dict(kT=kcmpT[:, g, :], q=qn6[:, r_, :], mask=cmpm[:, ts], mk=cmpm, v=vcmp[:, g, :], br=0, h=h,
                                         first=True, last=True, kk=[kcmpT, qn6], vk=vcmp))
                        for m in range(nm):
                            jobs.append(dict(kT=ksT[:, m * 128:(m + 1) * 128], q=qr6[:, r_, :], mask=M[:, m, :], mk=("M", m), v=vs[:, m, :], br=1, h=h,
                                             first=(m == 0), last=(m == nm - 1), kk=[ksT, qr6], vk=vs))
                        m0 = max(0, 4 * c - 4)
                        for m in range(m0, nm):
                            jobs.append(dict(kT=kwT[:, m * 128:(m + 1) * 128], q=qr6[:, r_, :], mask=wmask[:, m - 4 * c + 4, :], mk=wmask, v=vw[:, m, :], br=2, h=h,
                                             first=(m == m0), last=(m == nm - 1), kk=[kwT, qr6], vk=vw))
                    Sb = [PB[0], PB[1], PB[2], PB[7]]
                    sets = [(PB[3], PB[4]), (PB[5], PB[6])]
                    LOOK = 3
                    nj = len(jobs)
                    for i in range(nj + LOOK):
                        if i < nj:
                            j = jobs[i]
                            S = Sb[i % 4]
                            if j["br"] == 0:
                                hp = j["h"] % 2
                                if (j["h"] * 4 + c) % 3 != 0:
                                    conv_step(1)
                                for b_ in range(3):
                                    k.dma(gb2[hp][:, b_, :], sc["gT_d"][j["h"] * 3 + b_:j["h"] * 3 + b_ + 1, ts].partition_broadcast(128), r=["gT_d"], w=[gb2[hp]])
                            k.op("pe", lambda e: e.matmul(S[:], lhsT=j["kT"], rhs=j["q"], start=True, stop=True), r=j["kk"], w=[S])
                        if i >= LOOK:
                            ii = i - LOOK
                            j = jobs[ii]
                            S = Sb[ii % 4]
                            P = Pb[ii % 5]
                            if j["first"]:
                                bcount[0] += 1
                            O, Dn = sets[bcount[0] % 2]
                            k.op("act", lambda e: e.activation(out=P[:], in_=S[:], func=AF.Exp, scale=SCALE), r=[S], w=[P])
                            k.op("pool" if j["br"] == 2 else "dve", lambda e: e.tensor_tensor(out=P[:], in0=P[:], in1=j["mask"], op=ALU.mult), r=[P, j["mk"]], w=[P])
                            k.op("pe", lambda e: e.matmul(O[:], lhsT=j["v"], rhs=P[:], start=j["first"], stop=j["last"]), r=[j["vk"], P], w=[O])
                            k.op("pe", lambda e: e.matmul(Dn[:], lhsT=ones_b[:], rhs=P[:], start=j["first"], stop=j["last"]), r=[ones_b, P], w=[Dn])
                            if j["last"]:
                                combine(j["br"], O, Dn, gb2[j["h"] % 2])
                                if j["br"] == 2:
                                    hh = j["h"]
                                    k.op("act", lambda e: e.activation(out=yb[:], in_=yacc[:], func=AF.Identity), r=[yacc], w=[yb])
                                    k.dma(sc["yT_d"][1024 + hh * 128:1024 + (hh + 1) * 128, ts], yb[:], r=[yb], w=["yT_d"])
        k.barrier()

    if "outproj" in phases:
        with contextlib.ExitStack() as st:
            yT = k.sb(st, "op_yT", [128, KC, 512], BF16)
            W = [k.sb(st, "op_W%d" % i, [128, KC, 512], BF16) for i in range(2)]
            g1b = k.sb(st, "op_g1b", [128, D], F32)
            xt2 = [k.sb(st, "op_xt%d" % i, [128, 512], F32) for i in range(2)]
            o2 = [k.sb(st, "op_o%d" % i, [128, 512], F32) for i in range(2)]
            modrow = sc["mod_d"].rearrange("(a f) -> a f", a=1)
            k.dma(g1b[:], modrow[0:1, 2 * D:3 * D].partition_broadcast(128), r=["mod_d"], w=[g1b])
            yv = sc["yT_d"].rearrange("(k p) t -> p k t", p=128)
            wv = sc["wo16_d"].rearrange("(k p) f -> p k f", p=128)
            cnt = 0
            for tb in range(4):
                ts = slice(tb * 512, (tb + 1) * 512)
                for pc in range(4):
                    k.dma(yT[:, pc * 8:(pc + 1) * 8, :], yv[:, pc * 8:(pc + 1) * 8, ts], r=["yT_d"], w=[yT])
                for fb in range(8):
                    fs = slice(fb * 512, (fb + 1) * 512)
                    Wt = W[fb % 2]
                    for pc in range(4):
                        k.dma(Wt[:, pc * 8:(pc + 1) * 8, :], wv[:, pc * 8:(pc + 1) * 8, fs], r=["wo16_d"], w=[Wt], q=("sp" if pc % 2 == 0 else "act"))
                    for tt in range(4):
                        ps = PB[cnt % 2]
                        xx = xt2[cnt % 2]
                        oo = o2[cnt % 2]
                        cnt += 1
                        rows = slice(tb * 512 + tt * 128, tb * 512 + (tt + 1) * 128)
                        k.dma(xx[:], ins["x"][rows, fs], w=[xx])
                        for kk in range(KC):
                            k.op("pe", lambda e: e.matmul(ps[:], lhsT=yT[:, kk, tt * 128:(tt + 1) * 128], rhs=Wt[:, kk, :],
                                                          start=(kk == 0), stop=(kk == KC - 1)), r=[yT, Wt], w=[ps])
                        k.op("dve", lambda e: e.tensor_tensor(out=oo[:], in0=ps[:], in1=g1b[:, fs], op=ALU.mult), r=[ps, g1b], w=[oo])
                        k.op("dve", lambda e: e.tensor_tensor(out=oo[:], in0=oo[:], in1=xx[:], op=ALU.add), r=[oo, xx], w=[oo])
                        k.dma(sc["x1_d"][rows, fs], oo[:], r=[oo], w=["x1_d"])
        k.barrier()

    if "peer" in phases:
        with contextlib.ExitStack() as st:
            xt = k.sb(st, "pr_xt", [128, D], F32)
            xn = k.sb(st, "pr_xn", [128, D], BF16)
            junk = k.sb(st, "pr_junk", [128, D], BF16)
            ss = k.sb(st, "pr_ss", [128, 4], F32)
            hT = k.sb(st, "pr_hT", [128, KC, 512], BF16)
            Wt2 = [k.sb(st, "pr_W%d" % i, [128, KC, 512], BF16) for i in range(2)]
            kraw = k.sb(st, "pr_kraw", [128, 16, 128], F32)
            keysT = k.sb(st, "pr_keysT", [128, 16, 128], F32)
            qT = k.sb(st, "pr_qT", [128, 16, 512], F32)
            S = k.sb(st, "pr_S", [128, 16, 128], F32)
            wk = k.sb(st, "pr_wk", [128, 256], F32)
            v2 = k.sb(st, "pr_v2", [128, 2, 16], F32)
            i2u = k.sb(st, "pr_i2u", [128, 2, 16], U32)
            i2f = k.sb(st, "pr_i2f", [128, 2, 16], F32)
            cand = k.sb(st, "pr_cand", [128, 16, 16], F32)
            cidx = k.sb(st, "pr_cidx", [128, 16, 16], F32)
            tops = k.sb(st, "pr_tops", [128, 16], F32)
            ef = k.sb(st, "pr_ef", [128, 128], F32)
            ei = k.sb(st, "pr_ei", [128, 128], I32)
            gt = k.sb(st, "pr_gt", [128, 128], F32)
            sm = k.sb(st, "pr_sm", [128, 4], F32)
            ex = k.sb(st, "pr_ex", [128, 16], F32)
            iot = k.sb(st, "pr_iota", [128, 256], F32)
            wk2 = k.sb(st, "pr_wk2", [128, 256], F32)
            posu = k.sb(st, "pr_posu", [128, 16], U32)
            posf = k.sb(st, "pr_posf", [128, 16], F32)
            k.dma(iot[:], ins["iota256"][:, :], w=[iot])
            k.dma(kraw[:], ins["peer_keys"].rearrange("a n d -> n a d"), w=[kraw])
            for hc in range(16):
                pk = PB[4 + (hc // 4) % 2]
                k.op("pe", lambda e: e.transpose(out=pk[:, (hc % 4) * 128:(hc % 4 + 1) * 128], in_=kraw[:, hc, :], identity=ident_f[:]),
                     r=[kraw, ident_f], w=[pk])
                if hc % 4 == 3:
                    k.op("act", lambda e: e.activation(out=keysT[:, hc - 3:hc + 1, :].rearrange("p a b -> p (a b)"), in_=pk[:], func=AF.Identity),
                         r=[pk], w=[keysT])
            wv = sc["wq16_d"].rearrange("(k p) f -> p k f", p=128)
            pT = [PBb[0], PBb[1]]
            cnt = 0
            for tb in range(4):
                norm_block((xt, xn, junk, ss, pT), sc["x1_d"], tb, A2, B2, hT)
                for wt_ in range(4):
                    Wt = Wt2[wt_ % 2]
                    for pc in range(4):
                        k.dma(Wt[:, pc * 8:(pc + 1) * 8, :], wv[:, pc * 8:(pc + 1) * 8, wt_ * 512:(wt_ + 1) * 512], r=["wq16_d"], w=[Wt], q=("sp" if pc % 2 == 0 else "act"))
                    for sub in range(4):
                        hc = wt_ * 4 + sub
                        ps = PB[2 + cnt % 2]
                        cnt += 1
                        for kk in range(KC):
                            k.op("pe", lambda e: e.matmul(ps[:], lhsT=Wt[:, kk, sub * 128:(sub + 1) * 128], rhs=hT[:, kk, :],
                                                          start=(kk == 0), stop=(kk == KC - 1)), r=[Wt, hT], w=[ps])
                        k.op("act", lambda e: e.activation(out=qT[:, hc, :], in_=ps[:], func=AF.Identity), r=[ps], w=[("qT", hc)])
                for tt in range(4):
                    rows = slice(tb * 512 + tt * 128, tb * 512 + (tt + 1) * 128)
                    for hc in range(16):
                        pk = PB[4 + (hc // 4) % 2]
                        k.op("pe", lambda e: e.matmul(pk[:, (hc % 4) * 128:(hc % 4 + 1) * 128], lhsT=qT[:, hc, tt * 128:(tt + 1) * 128], rhs=keysT[:, hc, :],
                                                      start=True, stop=True), r=[("qT", hc), keysT], w=[pk])
                        if hc % 4 == 3:
                            k.op("act", lambda e: e.activation(out=S[:, hc - 3:hc + 1, :].rearrange("p a b -> p (a b)"), in_=pk[:], func=AF.Identity),
                                 r=[pk], w=[("S", hc // 4)])
                    for h in range(8):
                        skey = ("S", h // 2)
                        for c2 in range(2):
                            sv = S[:, 2 * h + c2, :]
                            k.op("dve", lambda e: e.max(out=v2[:, c2, 0:8], in_=sv), r=[skey], w=[v2])
                            k.op("dve", lambda e: e.max_index(out=i2u[:, c2, 0:8], in_max=v2[:, c2, 0:8], in_values=sv), r=[skey, v2], w=[i2u])
                            k.op("dve", lambda e: e.match_replace(out=wk[:, 0:128], in_to_replace=v2[:, c2, 0:8], in_values=sv, imm_value=-1e30), r=[skey, v2], w=[wk])
                            k.op("dve", lambda e: e.max(out=v2[:, c2, 8:16], in_=wk[:, 0:128]), r=[wk], w=[v2])
                            k.op("dve", lambda e: e.max_index(out=i2u[:, c2, 8:16], in_max=v2[:, c2, 8:16], in_values=wk[:, 0:128]), r=[wk, v2], w=[i2u])
                        k.op("dve", lambda e: e.tensor_copy(out=i2f[:], in_=i2u[:]), r=[i2u], w=[i2f])
                        k.op("dve", lambda e: e.tensor_scalar(out=i2f[:, 0, :], in0=i2f[:, 0, :], scalar1=128.0, scalar2=None, op0=ALU.mult), r=[i2f], w=[i2f])
                        k.op("dve", lambda e: e.tensor_tensor(out=cand[:], in0=v2[:, 0, :].unsqueeze(2).to_broadcast([128, 16, 16]),
                                                              in1=v2[:, 1, :].unsqueeze(1).to_broadcast([128, 16, 16]), op=ALU.add), r=[v2], w=[cand])
                        k.op("dve", lambda e: e.tensor_tensor(out=cidx[:], in0=i2f[:, 0, :].unsqueeze(2).to_broadcast([128, 16, 16]),
                                                              in1=i2f[:, 1, :].unsqueeze(1).to_broadcast([128, 16, 16]), op=ALU.add), r=[i2f], w=[cidx])
                        cf = cand[:].rearrange("p a b -> p (a b)")
                        xf = cidx[:].rearrange("p a b -> p (a b)")
                        k.op("dve", lambda e: e.max(out=tops[:, 0:8], in_=cf), r=[cand], w=[tops])
                        k.op("dve", lambda e: e.match_replace(out=wk[:], in_to_replace=tops[:, 0:8], in_values=cf, imm_value=-1e30), r=[cand, tops], w=[wk])
                        k.op("dve", lambda e: e.max(out=tops[:, 8:16], in_=wk[:]), r=[wk], w=[tops])
                        k.op("dve", lambda e: e.max_index(out=posu[:, 0:8], in_max=tops[:, 0:8], in_values=cf), r=[cand, tops], w=[posu])
                        k.op("dve", lambda e: e.max_index(out=posu[:, 8:16], in_max=tops[:, 8:16], in_values=wk[:]), r=[wk, tops], w=[posu])
                        k.op("dve", lambda e: e.tensor_copy(out=posf[:], in_=posu[:]), r=[posu], w=[posf])
                        k.op("dve", lambda e: e.memset(ef[:, h * 16:(h + 1) * 16], 0.0), w=[("ef", h)])
                        for kk in range(16):
                            k.op("dve", lambda e: e.scalar_tensor_tensor(out=wk2[:], in0=iot[:], scalar=posf[:, kk:kk + 1], in1=xf,
                                                                         op0=ALU.is_equal, op1=ALU.mult, accum_out=ef[:, h * 16 + kk:h * 16 + kk + 1]),
                                 r=[iot, cidx, posf], w=[wk2, ("ef", h)])
                        k.op("dve", lambda e: e.tensor_scalar(out=ef[:, h * 16:(h + 1) * 16], in0=ef[:, h * 16:(h + 1) * 16], scalar1=16383.0, scalar2=0.0,
                                                              op0=ALU.min, op1=ALU.max), r=[("ef", h)], w=[("ef", h)])
                        k.op("dve", lambda e: e.tensor_scalar(out=sm[:, 0:1], in0=tops[:, 0:1], scalar1=-1.0, scalar2=None, op0=ALU.mult), r=[tops], w=[sm])
                        k.op("dve", lambda e: e.memset(sm[:, 1:2], 0.0), w=[sm])
                        k.op("act", lambda e: e.activation(out=ex[:], in_=tops[:], func=AF.Exp, bias=sm[:, 0:1], accum_out=sm[:, 1:2]), r=[tops, sm], w=[ex, sm])
                        k.op("dve", lambda e: e.reciprocal(out=sm[:, 2:3], in_=sm[:, 1:2]), r=[sm], w=[sm])
                        k.op("dve", lambda e: e.tensor_scalar(out=gt[:, h * 16:(h + 1) * 16], in0=ex[:], scalar1=sm[:, 2:3], scalar2=None, op0=ALU.mult),
                             r=[ex, sm], w=[("gt", h)])
                    k.op("dve", lambda e: e.tensor_copy(out=ei[:], in_=ef[:]), r=[("ef", h) for h in range(8)], w=[ei])
                    k.dma(sc["eidx_d"][rows, :], ei[:], r=[ei], w=["eidx_d"])
                    k.dma(sc["ef_d"][rows, :], ef[:], r=[("ef", h) for h in range(8)], w=["ef_d"])
                    k.dma(sc["gts_d"][rows, :], gt[:], r=[("gt", h) for h in range(8)], w=["gts_d"])
        k.barrier()

        conv_step(1000)
        with contextlib.ExitStack() as st:
            h2f = k.sb(st, "pe_h2f", [128, D], F32)
            h2b = k.sb(st, "pe_h2b", [128, D], BF16)
            A2b = k.sb(st, "pe_A2b", [128, D], F32)
            B2b = k.sb(st, "pe_B2b", [128, D], F32)
            Ug = [k.sb(st, "pe_Ug%d" % i, [128, D], BF16) for i in range(4)]
            Vg = [k.sb(st, "pe_Vg%d" % i, [128, D], BF16) for i in range(4)]
            x1c = [k.sb(st, "pe_x1c%d" % i, [128, 512], F32) for i in range(2)]
            g2c = [k.sb(st, "pe_g2c%d" % i, [128, 512], F32) for i in range(2)]
            junkb = k.sb(st, "pe_junk", [128, D], BF16)
            x1t = k.sb(st, "pe_x1t", [128, D], F32)
            Wall = k.sb(st, "pe_Wall", [128, 128 * 128], BF16)
            ei = k.sb(st, "pe_ei", [128, 128], I32)
            eft = k.sb(st, "pe_eft", [128, 128], F32)
            eiT2 = [k.sb(st, "pe_eiT%d" % i, [128, 128], I32) for i in range(2)]
            gt = k.sb(st, "pe_gt", [128, 128], F32)
            av = k.sb(st, "pe_a", [128, 128], F32)
            wv_ = k.sb(st, "pe_w", [128, 128], F32)
            wT = k.sb(st, "pe_wT", [128, 128], F32)
            ss = k.sb(st, "pe_ss", [128, 4], F32)
            modrow = sc["mod_d"].rearrange("(a f) -> a f", a=1)
            k.op("pool", lambda e: e.memset(Wall[:], 0.0), w=[Wall])
            k.dma(A2b[:], modrow[0:1, 4 * D:5 * D].partition_broadcast(128), r=["mod_d"], w=[A2b])
            k.dma(B2b[:], ins["g2_row"][0:1, :].partition_broadcast(128), w=[B2b])
            k.op("dve", lambda e: e.scalar_tensor_tensor(out=A2b[:], in0=A2b[:], scalar=1.0, in1=B2b[:], op0=ALU.add, op1=ALU.mult), r=[A2b, B2b], w=[A2b])
            k.dma(B2b[:], modrow[0:1, 3 * D:4 * D].partition_broadcast(128), r=["mod_d", A2b], w=[B2b])
            def prep(ti):
                rows = slice(ti * 128, (ti + 1) * 128)
                eT = eiT2[ti % 2]
                k.dma(x1t[:], sc["x1_d"][rows, :], r=["x1_d"], w=[x1t])
                k.dma(ei[:], sc["eidx_d"][rows, :], r=["eidx_d"], w=[ei])
                k.dma(eft[:], sc["ef_d"][rows, :], r=["ef_d"], w=[eft])
                k.dma(gt[:], sc["gts_d"][rows, :], r=["gts_d"], w=[gt])
                k.op("dve", lambda e: e.memset(ss[:], 0.0), w=[ss])
                k.op("act", lambda e: e.activation(out=junkb[:], in_=x1t[:], func=AF.Square, accum_out=ss[:, 0:1]), r=[x1t, ss], w=[junkb, ss])
                k.op("dve", lambda e: e.tensor_scalar(out=ss[:, 1:2], in0=ss[:, 0:1], scalar1=1.0 / D, scalar2=EPS, op0=ALU.mult, op1=ALU.add), r=[ss], w=[ss])
                k.op("act", lambda e: e.activation(out=ss[:, 3:4], in_=ss[:, 1:2], func=AF.Sqrt), r=[ss], w=[ss])
                k.op("dve", lambda e: e.reciprocal(out=ss[:, 2:3], in_=ss[:, 3:4]), r=[ss], w=[ss])
                k.op("dve", lambda e: e.scalar_tensor_tensor(out=h2f[:], in0=x1t[:], scalar=ss[:, 2:3], in1=A2b[:], op0=ALU.mult, op1=ALU.mult), r=[x1t, ss, A2b], w=[h2f])
                k.op("dve", lambda e: e.tensor_tensor(out=h2b[:], in0=h2f[:], in1=B2b[:], op=ALU.add), r=[h2f, B2b], w=[h2b])
                k.op("pe", lambda e: e.transpose(out=PB[0][:, 0:128], in_=eft[:], identity=ident_f[:]), r=[eft, ident_f], w=[PB[0]])
                k.op("dve", lambda e: e.tensor_copy(out=eT[:], in_=PB[0][:, 0:128]), r=[PB[0]], w=[eT])
                k.op("dve", lambda e: e.memset(av[:], 0.0), w=[av])

            def u_step(ti, s_):
                u = Ug[s_ % 4]
                k.gather(u[:], sc["u16_d"][:, :], ei[:, s_:s_ + 1], r=[ei, "u16_d"], w=[u])
                k.op("dve", lambda e: e.scalar_tensor_tensor(out=junkb[:], in0=u[:], scalar=1.0, in1=h2b[:], op0=ALU.mult, op1=ALU.mult,
                                                             accum_out=av[:, s_:s_ + 1]), r=[u, h2b], w=[junkb, ("av", s_)])

            def post(ti):
                k.op("act", lambda e: e.activation(out=wv_[:], in_=av[:], func=AF.Gelu), r=[av] + [("av", s_) for s_ in range(128)], w=[wv_])
                k.op("dve", lambda e: e.tensor_tensor(out=wv_[:], in0=wv_[:], in1=gt[:], op=ALU.mult), r=[wv_, gt], w=[wv_])
                k.op("pe", lambda e: e.transpose(out=PB[0][:, 0:128], in_=wv_[:], identity=ident_f[:]), r=[wv_, ident_f], w=[PB[0]])
                k.op("dve", lambda e: e.tensor_copy(out=Wall[:, 0:128 * 128:129], in_=PB[0][:, 0:128]), r=[PB[0]], w=[Wall])

            def v_step(ti, t_):
                u = Vg[t_ % 4]
                eT = eiT2[ti % 2]
                k.gather(u[:], sc["v16_d"][:, :], eT[:, t_:t_ + 1], r=[eT, "v16_d"], w=[u])
                for nb in range(8):
                    k.op("pe", lambda e: e.matmul(PB[nb][:], lhsT=Wall[:, t_ * 128:(t_ + 1) * 128], rhs=u[:, nb * 512:(nb + 1) * 512],
                                                  start=(t_ == 0), stop=(t_ == 127)), r=[Wall, u], w=[PB[nb]])

            def evac(ti):
                rows = slice(ti * 128, (ti + 1) * 128)
                for nb in range(8):
                    cs = slice(nb * 512, (nb + 1) * 512)
                    xc = x1c[nb % 2]
                    gc = g2c[nb % 2]
                    k.dma(xc[:], sc["x1_d"][rows, cs], r=["x1_d"], w=[xc])
                    k.dma(gc[:], modrow[0:1, 5 * D + nb * 512:5 * D + (nb + 1) * 512].partition_broadcast(128), r=["mod_d"], w=[gc])
                    k.op("dve", lambda e: e.tensor_tensor(out=h2f[:, cs], in0=PB[nb][:], in1=gc[:], op=ALU.mult), r=[PB[nb], gc, h2f], w=[("yo", nb)])
                    k.op("pool", lambda e: e.tensor_tensor(out=h2f[:, cs], in0=h2f[:, cs], in1=xc[:], op=ALU.add), r=[("yo", nb), xc], w=[("yo", nb)])
                k.dma(out[rows, :], h2f[:], r=[("yo", nb) for nb in range(8)], w=["out", h2f])

            prep(0)
            for s_ in range(128):
                u_step(0, s_)
            post(0)
            for ti in range(16):
                nxt = ti + 1 < 16
                if nxt:
                    prep(ti + 1)
                for s_ in range(128):
                    if nxt:
                        u_step(ti + 1, s_)
                    v_step(ti, s_)
                evac(ti)
                if nxt:
                    post(ti + 1)
        k.barrier()

    k.finish()
    es.close()
    return nc, k


def prep_core_inputs(inp, b, consts):
    f = lambda a: np.ascontiguousarray(a, dtype=np.float32)
    d = {}
    d["x"] = f(inp["x"][b])
    d["c_l"] = f(inp["c"][b].reshape(KC, 128).T)
    d["w_ada"] = f(inp["w_ada"][0])
    d["b_ada"] = f(inp["b_ada"][0].reshape(1, -1))
    d["g1_l"] = f(inp["norm1_g"][0].reshape(KC, 128).T)
    d["g2_l"] = f(inp["norm2_g"][0].reshape(KC, 128).T)
    d["g2_row"] = f(inp["norm2_g"][0].reshape(1, -1))
    d["w_in"] = f(inp["w_in"][0])
    d["w_out"] = f(inp["w_out"][0])
    d["w_pool"] = f(inp["w_pool"][0])
    d["pscale_l"] = f(inp["pool_scale"][0].reshape(8, 128).T)
    d["qkg_l"] = f(np.concatenate([inp["q_norm_g"][0][None, :], inp["k_norm_g"][0]], 0).T)
    d["pe_l"] = f(np.transpose(inp["cmp_pe"][0], (0, 2, 1)))
    d["cmp_w1"] = f(inp["cmp_w1"][0])
    d["cmp_w2"] = f(inp["cmp_w2"][0])
    d["w_pq"] = f(inp["w_pq"][0])
    d["peer_keys"] = f(inp["peer_keys"][0].reshape(16, 128, 128))
    d["peer_u"] = f(inp["peer_u"][0])
    d["peer_v"] = f(inp["peer_v"][0])
    d.update(consts)
    return d


def sim_deadlock(k):
    streams = {}
    for e in k.log:
        streams.setdefault(e[0], []).append(e)
    pc = {e: 0 for e in streams}
    sem = {}
    progress = True
    while progress:
        progress = False
        for e, lst in streams.items():
            while pc[e] < len(lst):
                _, kind, s, v = lst[pc[e]]
                if kind == "w":
                    if sem.get(s, 0) >= v:
                        pc[e] += 1
                        progress = True
                    else:
                        break
                else:
                    sem[s] = sem.get(s, 0) + v
                    pc[e] += 1
                    progress = True
    stuck = {e: (pc[e], len(l), l[pc[e]] if pc[e] < len(l) else None) for e, l in streams.items() if pc[e] < len(l)}
    return stuck, sem


_CACHE = {}


def kernel(**inputs):
    inp = {n: np.asarray(v) for n, v in inputs.items()}
    consts = make_consts()
    if "nc" not in _CACHE:
        _CACHE["nc"] = build()[0]
    nc = _CACHE["nc"]
    in_maps = [prep_core_inputs(inp, b, consts) for b in range(8)]
    res = run_bass_kernel_spmd(nc, in_maps, core_ids=list(range(8)))
    return np.stack([np.asarray(r["out"], dtype=np.float32) for r in res.results], axis=0)
```

```python
import contextlib
import numpy as np
import concourse.bass as bass
import concourse.mybir as mybir
from concourse.bass_utils import run_bass_kernel_spmd

F32 = mybir.dt.float32
BF16 = mybir.dt.bfloat16
I32 = mybir.dt.int32
U32 = mybir.dt.uint32
AF = mybir.ActivationFunctionType
ALU = mybir.AluOpType

T = 2048
D = 4096
KC = 32
NH = 24
G = 4
R = 6
NCMP = 127
NSEL = 32
INW = 7240
EPS = 1e-6
SCALE = 128 ** -0.5


class MK:
    NLANES = 12

    def __init__(self, nc):
        self.nc = nc
        self.es = contextlib.ExitStack()
        self.eng = {"pe": nc.tensor, "dve": nc.vector, "act": nc.scalar,
                    "pool": nc.gpsimd, "sp": nc.sync}
        self.sem = {}
        self.cnt = {}
        for n in self.eng:
            self.sem[n] = self.es.enter_context(nc.semaphore("sem_" + n))
            self.cnt[n] = 0
        self.lanes = []
        for i in range(self.NLANES):
            n = "lane%d" % i
            self.sem[n] = self.es.enter_context(nc.semaphore(n))
            self.cnt[n] = 0
            self.lanes.append(n)
        self.lane_rr = 0
        self.clanes = []
        for i in range(3):
            n = "clane%d" % i
            self.sem[n] = self.es.enter_context(nc.semaphore(n))
            self.cnt[n] = 0
            self.clanes.append(n)
        self.clane_rr = 0
        self.waited = {}
        self.last_w = {}
        self.readers = {}
        self.ninst = 0
        self.log = []

    @staticmethod
    def key(x):
        if isinstance(x, (str, tuple)):
            return x
        if hasattr(x, "tensor"):
            return x.tensor.name
        return x.name

    def _wait(self, engname, semname, val):
        if val <= 0:
            return
        k = (engname, semname)
        if self.waited.get(k, 0) >= val:
            return
        self.waited[k] = val
        self.eng[engname].wait_ge(self.sem[semname], val)
        self.ninst += 1
        self.log.append((engname, "w", semname, val))

    def _deps(self, engname, r, w):
        need = {}
        for x in r:
            lw = self.last_w.get(self.key(x))
            if lw:
                need[lw[0]] = max(need.get(lw[0], 0), lw[1])
        for x in w:
            k = self.key(x)
            lw = self.last_w.get(k)
            if lw:
                need[lw[0]] = max(need.get(lw[0], 0), lw[1])
            for (s, v) in self.readers.get(k, []):
                need[s] = max(need.get(s, 0), v)
        for s, v in need.items():
            if s == "pe" and engname == "pe":
                continue
            self._wait(engname, s, v)

    def _record(self, tok, r, w):
        for x in r:
            lst = self.readers.setdefault(self.key(x), [])
            lst[:] = [(s, v) for (s, v) in lst if s != tok[0]]
            lst.append(tok)
        for x in w:
            k = self.key(x)
            self.last_w[k] = tok
            self.readers[k] = []

    def op(self, engname, fn, r=(), w=()):
        self._deps(engname, r, w)
        inst = fn(self.eng[engname])
        self.cnt[engname] += 1
        inst.then_inc(self.sem[engname], 1)
        self.ninst += 1
        self.log.append((engname, "i", engname, 1))
        self._record((engname, self.cnt[engname]), r, w)
        return inst

    def _lane(self):
        lane = self.lanes[self.lane_rr]
        self.lane_rr = (self.lane_rr + 1) % self.NLANES
        return lane

    def dma(self, out, in_, r=(), w=(), q="sp", conv=False, **kw):
        if conv:
            lane = self.clanes[self.clane_rr]
            self.clane_rr = (self.clane_rr + 1) % len(self.clanes)
        else:
            lane = self._lane()
        self._deps(q, r, w)
        self._wait(q, lane, self.cnt[lane])
        inst = self.eng[q].dma_start(out=out, in_=in_, **kw)
        self.cnt[lane] += 16
        inst.then_inc(self.sem[lane], 16)
        self.ninst += 1
        self.log.append((q, "i", lane, 16))
        self._record((lane, self.cnt[lane]), r, w)
        return inst

    def gather(self, out, table, idx_ap, r=(), w=()):
        lane = self._lane()
        q = "pool"
        self._deps(q, r, w)
        self._wait(q, lane, self.cnt[lane])
        inst = self.nc.gpsimd.indirect_dma_start(
            out=out, out_offset=None, in_=table,
            in_offset=bass.IndirectOffsetOnAxis(ap=idx_ap, axis=0))
        self.cnt[lane] += 16
        inst.then_inc(self.sem[lane], 16)
        self.ninst += 1
        self.log.append((q, "i", lane, 16))
        self._record((lane, self.cnt[lane]), r, w)
        return inst

    def barrier(self):
        for e in self.eng:
            for s in self.sem:
                if s == e:
                    continue
                self._wait(e, s, self.cnt[s])
        self.last_w = {}
        self.readers = {}

    def finish(self, engname="sp"):
        for s in self.sem:
            self._wait(engname, s, self.cnt[s])

    def sb(self, stack, name, shape, dt):
        return stack.enter_context(self.nc.sbuf_tensor("s_" + name, shape, dt))

    def ps(self, stack, name, shape, dt=F32):
        return stack.enter_context(self.nc.psum_tensor("p_" + name, shape, dt))


def make_consts():
    c = {}
    c["ident_f"] = np.eye(128, dtype=np.float32)
    c["ones_f"] = np.ones((128, 128), np.float32)
    rot = np.zeros((128, 128), np.float32)
    for m in range(128):
        rot[(m + 64) % 128, m] = 1.0
    c["rotm"] = rot
    half = 64
    inv = (10000.0 ** (-np.arange(half, dtype=np.float32) / half)).astype(np.float32)
    pos = np.arange(T, dtype=np.float32)
    ang = (pos[:, None] * inv[None, :]).astype(np.float32)
    cos = np.cos(ang).astype(np.float32).T
    sin = np.sin(ang).astype(np.float32).T
    c["cosT"] = np.concatenate([cos, cos], 0).astype(np.float32)
    c["sinT"] = np.concatenate([-sin, sin], 0).astype(np.float32)
    n = np.arange(128)
    t = np.arange(T)
    cm = ((16 * n[:, None] + 31) <= t[None, :]) & (n[:, None] < NCMP)
    c["cmpmask"] = cm.astype(np.float32)
    cst = np.arange(NCMP)[:, None] * 16
    sst = np.arange(NSEL)[None, :] * 64
    ov = np.clip(np.minimum(cst + 32, sst + 64) - np.maximum(cst, sst), 0, None)
    sm = np.zeros((128, NSEL), np.float32)
    sm[:NCMP] = ov / 32.0
    c["selmap"] = sm
    j = np.arange(NSEL)
    cur = t // 64
    forced = (j[None, :] == 0) | (j[None, :] == cur[:, None]) | (j[None, :] == cur[:, None] - 1)
    valid = (j[None, :] * 64) <= t[:, None]
    c["tb"] = (np.where(forced, 1000.0, 0.0) + np.where(valid, 0.0, -1e9)).astype(np.float32)
    eb = np.zeros((NSEL, 16, 128), np.float32)
    for m in range(16):
        for kk in range(128):
            eb[2 * m + kk // 64, m, kk] = 1.0
    c["eb"] = eb
    kk = np.arange(128)[:, None]
    q = np.arange(512)[None, :]
    caus = np.zeros((128, 4, 512), np.float32)
    for d in range(4):
        caus[:, d, :] = ((q - kk) >= 128 * d)
    c["caus"] = caus
    wm = np.zeros((128, 8, 512), np.float32)
    for d in range(-4, 4):
        dist = q - kk - 128 * d
        wm[:, d + 4, :] = (dist >= 0) & (dist < 512)
    c["wmask"] = wm
    ic = np.zeros((128, 4, T), np.float32)
    for gi, w in enumerate((2, 4, 8, 16)):
        ic[:, gi, :] = 1.0 / np.minimum(t + 1, w).astype(np.float32)[None, :]
    c["invcnt"] = ic
    c["iota256"] = np.tile(np.arange(256, dtype=np.float32)[None, :], (128, 1))
    return c


CONST_SHAPES = {
    "ident_f": [128, 128], "ones_f": [128, 128], "rotm": [128, 128],
    "cosT": [128, T], "sinT": [128, T], "cmpmask": [128, T], "selmap": [128, NSEL],
    "tb": [T, NSEL], "eb": [NSEL, 16, 128], "caus": [128, 4, 512], "wmask": [128, 8, 512],
    "invcnt": [128, 4, T], "iota256": [128, 256],
}

INPUT_SHAPES = {
    "x": [T, D], "c_l": [128, KC], "w_ada": [D, 6 * D], "b_ada": [1, 6 * D],
    "g1_l": [128, KC], "g2_l": [128, KC], "g2_row": [1, D],
    "w_in": [D, INW], "w_out": [D, D], "w_pool": [4, 256, 256], "pscale_l": [128, 8],
    "qkg_l": [128, 4], "pe_l": [2, 128, 32], "cmp_w1": [2, 32, 128, 256], "cmp_w2": [2, 256, 128],
    "w_pq": [D, 2048], "peer_keys": [16, 128, 128], "peer_u": [16384, D], "peer_v": [16384, D],
}

SCRATCH = {
    "mod_d": ([6 * D], F32),
    "pT_d": ([1024, T], F32),
    "qn_d": ([NH, 128, T], BF16),
    "qr_d": ([NH, 128, T], BF16),
    "kc_d": ([2, G, 128, T], BF16),
    "ks_d": ([G, 128, T], BF16),
    "kw_d": ([G, 128, T], BF16),
    "vs_d": ([T, 512], BF16),
    "vw_d": ([T, 512], BF16),
    "gT_d": ([72, T], F32),
    "yT_d": ([D, T], BF16),
    "x1_d": ([T, D], F32),
    "eidx_d": ([T, 128], I32),
    "gts_d": ([T, 128], F32),
    "ef_d": ([T, 128], F32),
}
BIG_SCRATCH = {
    "u16_d": ([16384, D], BF16),
    "v16_d": ([16384, D], BF16),
    "wi16_d": ([D, INW], BF16),
    "wo16_d": ([D, D], BF16),
    "wq16_d": ([D, 2048], BF16),
}


def build(phases=("mod", "inproj", "pool", "cmp", "attn", "outproj", "peer"), debug=False,
          inputs_needed=None, ntb=4, dbg=99):
    nc = bass.Bass("TRN2", target_bir_lowering=False)
    k = MK(nc)
    es = k.es
    ins = {}
    for name, shp in list(INPUT_SHAPES.items()) + list(CONST_SHAPES.items()):
        if inputs_needed is not None and name not in inputs_needed:
            continue
        ins[name] = nc.dram_tensor(name, shp, F32, kind="ExternalInput").ap()
    out = nc.dram_tensor("out", [T, D], F32, kind="ExternalOutput").ap()
    sc = {}
    for name, (shp, dt) in SCRATCH.items():
        sc[name] = nc.dram_tensor(name, shp, dt, kind="ExternalOutput" if debug else "Internal").ap()
    for name, (shp, dt) in BIG_SCRATCH.items():
        sc[name] = nc.dram_tensor(name, shp, dt, kind="Internal").ap()
    conv_jobs = []
    if "peer" in phases:
        for i in range(32):
            rs_ = slice(i * 512, (i + 1) * 512)
            conv_jobs.append(("u16_d", "peer_u", rs_))
            conv_jobs.append(("v16_d", "peer_v", rs_))

    def conv_step(n=1):
        for _ in range(n):
            if conv_jobs:
                dn, sn, rs_ = conv_jobs.pop(0)
                k.dma(sc[dn][rs_, :], ins[sn][rs_, :], w=[dn], q="pool", conv=True)

    ident_f = k.sb(es, "ident_f_sb", [128, 128], F32)
    ident_b = k.sb(es, "ident_b_sb", [128, 128], BF16)
    ones_f = k.sb(es, "ones_f_sb", [128, 128], F32)
    ones_b = k.sb(es, "ones_b_sb", [128, 128], BF16)
    modT = k.sb(es, "modT", [128, 192], F32)
    A1 = k.sb(es, "A1", [128, KC], F32)
    A2 = k.sb(es, "A2", [128, KC], F32)
    kcmpT = k.sb(es, "kcmpT", [128, G, 128], BF16)
    vcmp = k.sb(es, "vcmp", [128, G, 128], BF16)
    PB = [k.ps(es, "bank%d" % i, [128, 512]) for i in range(8)]
    PBb = [b.bitcast(BF16) for b in PB]
    k.dma(ident_f[:], ins["ident_f"][:, :], w=[ident_f])
    k.dma(ones_f[:], ins["ones_f"][:, :], w=[ones_f])
    k.op("dve", lambda e: e.tensor_copy(out=ident_b[:], in_=ident_f[:]), r=[ident_f], w=[ident_b])
    k.op("dve", lambda e: e.tensor_copy(out=ones_b[:], in_=ones_f[:]), r=[ones_f], w=[ones_b])

    wconv_jobs = []
    for dn, sn, nrow, step in (("wi16_d", "w_in", D, 256), ("wo16_d", "w_out", D, 512), ("wq16_d", "w_pq", D, 1024)):
        if sn in ins:
            for r0 in range(0, nrow, step):
                wconv_jobs.append((dn, sn, r0, step))

    def wconv_step(n=1):
        for _ in range(n):
            if wconv_jobs:
                dn, sn, r0, step = wconv_jobs.pop(0)
                k.dma(sc[dn][r0:r0 + step, :], ins[sn][r0:r0 + step, :], w=[dn], q="pool", conv=True)

    if "mod" not in phases:
        wconv_step(1000)

    if "mod" in phases:
        with contextlib.ExitStack() as st:
            cl = k.sb(st, "cl", [128, KC], F32)
            scl = k.sb(st, "scl", [128, KC], BF16)
            wst = [k.sb(st, "wada_s%d" % i, [128, 16, 512], F32) for i in range(3)]
            wbb = [k.sb(st, "wada_b%d" % i, [128, 16, 512], BF16) for i in range(3)]
            brow = [k.sb(st, "brow%d" % i, [1, 512], F32) for i in range(2)]
            mrow = [k.sb(st, "mrow%d" % i, [1, 512], F32) for i in range(2)]
            psm = [PB[0], PB[1]]
            k.dma(cl[:], ins["c_l"][:, :], w=[cl])
            k.op("act", lambda e: e.activation(out=scl[:], in_=cl[:], func=AF.Silu), r=[cl], w=[scl])
            wv = ins["w_ada"].rearrange("(k p) f -> p k f", p=128)
            modv = sc["mod_d"].rearrange("(a f) -> a f", a=1)
            nh = 0
            pend = None
            for fb in range(48):
                ps = psm[fb % 2]
                br = brow[fb % 2]
                mr = mrow[fb % 2]
                fs = slice(fb * 512, (fb + 1) * 512)
                wconv_step(1)
                k.dma(br[:], ins["b_ada"][0:1, fs], w=[br])
                halves = []
                for hf in range(2):
                    ws_ = wst[nh % 3]
                    w_ = wbb[nh % 3]
                    ce = ("dve", "act", "pool")[nh % 3]
                    nh += 1
                    k.dma(ws_[:], wv[:, hf * 16:(hf + 1) * 16, fs], w=[ws_], q="sp")
                    halves.append((ws_, w_, ce))
                if pend is not None:
                    k.dma(pend[0], pend[1][:], r=[pend[1]], w=["mod_d"], q="sp")
                for hf in range(2):
                    ws_, w_, ce = halves[hf]
                    if ce == "act":
                        k.op("act", lambda e: e.activation(out=w_[:], in_=ws_[:], func=AF.Identity), r=[ws_], w=[w_])
                    else:
                        k.op(ce, lambda e: e.tensor_copy(out=w_[:], in_=ws_[:]), r=[ws_], w=[w_])
                    for kk in range(16):
                        kg = hf * 16 + kk
                        k.op("pe", lambda e: e.matmul(ps[0:1, :], lhsT=scl[:, kg:kg + 1], rhs=w_[:, kk, :],
                                                      start=(kg == 0), stop=(kg == 31)),
                             r=[scl, w_], w=[ps])
                k.op("dve", lambda e: e.tensor_tensor(out=mr[:], in0=ps[0:1, :], in1=br[:], op=ALU.add),
                     r=[ps, br], w=[mr])
                pend = (modv[0:1, fs], mr)
            k.dma(pend[0], pend[1][:], r=[pend[1]], w=["mod_d"], q="sp")
            wconv_step(1000)
        k.barrier()

    with contextlib.ExitStack() as st:
        m1 = k.sb(st, "m1", [96, 128], F32)
        m2 = k.sb(st, "m2", [96, 128], F32)
        g1 = k.sb(st, "g1l", [128, KC], F32)
        g2 = k.sb(st, "g2l", [128, KC], F32)
        pst = PB[2]
        mv = sc["mod_d"].rearrange("(c p) -> c p", p=128)
        k.dma(m1[:], mv[0:96, :], r=["mod_d"], w=[m1])
        k.dma(m2[:], mv[96:192, :], r=["mod_d"], w=[m2])
        k.dma(g1[:], ins["g1_l"][:, :], w=[g1])
        k.dma(g2[:], ins["g2_l"][:, :], w=[g2])
        k.op("pe", lambda e: e.transpose(out=pst[:, 0:96], in_=m1[:], identity=ident_f[0:96, 0:96]), r=[m1, ident_f], w=[pst])
        k.op("pe", lambda e: e.transpose(out=pst[:, 96:192], in_=m2[:], identity=ident_f[0:96, 0:96]), r=[m2, ident_f], w=[pst])
        k.op("dve", lambda e: e.tensor_copy(out=modT[:], in_=pst[:, 0:192]), r=[pst], w=[modT])
        k.op("dve", lambda e: e.scalar_tensor_tensor(out=A1[:], in0=modT[:, 32:64], scalar=1.0, in1=g1[:],
                                                     op0=ALU.add, op1=ALU.mult), r=[modT, g1], w=[A1])
        k.op("dve", lambda e: e.scalar_tensor_tensor(out=A2[:], in0=modT[:, 128:160], scalar=1.0, in1=g2[:],
                                                     op0=ALU.add, op1=ALU.mult), r=[modT, g2], w=[A2])
    k.barrier()
    B1 = modT[:, 0:32]
    B2 = modT[:, 96:128]

    def hT_keys():
        return [("hT", kk, tt) for kk in range(KC) for tt in range(4)]

    def norm_block(st_bufs, src, tb, Atab, Btab, hT):
        xt, xn, junk, ss, pT = st_bufs
        for tt in range(4):
            t0 = tb * 512 + tt * 128
            k.dma(xt[:], src[t0:t0 + 128, :], w=[xt])
            k.op("dve", lambda e: e.memset(ss[:], 0.0), w=[ss])
            k.op("act", lambda e: e.activation(out=junk[:], in_=xt[:], func=AF.Square, accum_out=ss[:, 0:1]),
                 r=[xt, ss], w=[junk, ss])
            k.op("dve", lambda e: e.tensor_scalar(out=ss[:, 1:2], in0=ss[:, 0:1], scalar1=1.0 / D, scalar2=EPS,
                                                  op0=ALU.mult, op1=ALU.add), r=[ss], w=[ss])
            k.op("act", lambda e: e.activation(out=ss[:, 3:4], in_=ss[:, 1:2], func=AF.Sqrt), r=[ss], w=[ss])
            k.op("dve", lambda e: e.reciprocal(out=ss[:, 2:3], in_=ss[:, 3:4]), r=[ss], w=[ss])
            k.op("dve", lambda e: e.tensor_scalar(out=xn[:], in0=xt[:], scalar1=ss[:, 2:3], scalar2=None,
                                                  op0=ALU.mult), r=[xt, ss], w=[xn])
            if dbg == -1:
                continue
            for kg in range(KC // 4):
                p = pT[kg % 2]
                for q4 in range(4):
                    kk = kg * 4 + q4
                    sl = slice(q4 * 128, q4 * 128 + 128)
                    k.op("pe", lambda e: e.transpose(out=p[:, sl], in_=xn[:, kk * 128:(kk + 1) * 128], identity=ident_b[:]),
                         r=[xn, ident_b], w=[p])
                for q4 in range(4):
                    kk = kg * 4 + q4
                    sl = slice(q4 * 128, q4 * 128 + 128)
                    k.op("dve", lambda e: e.tensor_scalar(out=hT[:, kk, tt * 128:(tt + 1) * 128], in0=p[:, sl],
                                                          scalar1=Atab[:, kk:kk + 1], scalar2=Btab[:, kk:kk + 1],
                                                          op0=ALU.mult, op1=ALU.add),
                         r=[p], w=[hT])

    if "inproj" in phases:
        with contextlib.ExitStack() as st:
            xt = k.sb(st, "xt", [128, D], F32)
            xn = k.sb(st, "xn", [128, D], BF16)
            junk = k.sb(st, "junk", [128, D], BF16)
            ss = k.sb(st, "ss", [128, 4], F32)
            pT = [PBb[0], PBb[1]]
            hT = k.sb(st, "hT", [128, KC, 512], BF16)
            W = [k.sb(st, "W%d" % i, [128, KC, 512], BF16) for i in range(2)]
            cosT = k.sb(st, "cosT", [128, T], F32)
            sinT = k.sb(st, "sinT", [128, T], F32)
            rotm = k.sb(st, "rotm", [128, 128], F32)
            qkg = k.sb(st, "qkg", [128, 4], F32)
            psA = [PB[2], PB[3]]
            ps2_ = [PB[4], PB[6]]
            ps3_ = [PB[5], PB[7]]
            raw_ = [k.sb(st, "raw%d" % i, [128, 512], F32) for i in range(2)]
            sq_ = [k.sb(st, "sq%d" % i, [128, 512], F32) for i in range(2)]
            rs_ = [k.sb(st, "rs%d" % i, [128, 512], F32) for i in range(2)]
            qn_ = [k.sb(st, "qn%d" % i, [128, 512], F32) for i in range(2)]
            t1_ = [k.sb(st, "t1%d" % i, [128, 512], F32) for i in range(2)]
            t2_ = [k.sb(st, "t2%d" % i, [128, 512], F32) for i in range(2)]
            oqr_ = [k.sb(st, "oqr%d" % i, [128, 512], BF16) for i in range(2)]
            ecnt = [0]
            ob = [k.sb(st, "ob%d" % i, [128, 512], BF16) for i in range(2)]
            of = [k.sb(st, "of%d" % i, [128, 512], F32) for i in range(2)]
            k.dma(cosT[:], ins["cosT"][:, :], w=[cosT])
            k.dma(sinT[:], ins["sinT"][:, :], w=[sinT])
            k.dma(rotm[:], ins["rotm"][:, :], w=[rotm])
            k.dma(qkg[:], ins["qkg_l"][:, :], w=[qkg])
            wv = sc["wi16_d"].rearrange("(k p) f -> p k f", p=128)
            cnt = [0]
            for tb in range(ntb):
                ts = slice(tb * 512, (tb + 1) * 512)
                if dbg != 0:
                    norm_block((xt, xn, junk, ss, pT), ins["x"], tb, A1, B1, hT)
                if dbg <= 1:
                    continue
                for ct in range(15):
                    if dbg == 2 and ct > 0:
                        continue
                    if dbg == 3 and ct not in (2,):
                        continue
                    if dbg == 4 and ct not in (11,):
                        continue
                    if dbg == 5 and ct not in (14,):
                        continue
                    if dbg == 6 and ct not in (8,):
                        continue
                    ncol = 512 if ct < 14 else 72
                    Wt = W[ct % 2]
                    for pc in range(4):
                        k.dma(Wt[:, pc * 8:(pc + 1) * 8, 0:ncol], wv[:, pc * 8:(pc + 1) * 8, ct * 512:ct * 512 + ncol], r=["wi16_d"], w=[Wt],
                              q="sp")
                    if ct in (11, 13):
                        dst = sc["vs_d"] if ct == 11 else sc["vw_d"]
                        for tt in range(4):
                            ps = psA[cnt[0] % 2]
                            o = ob[cnt[0] % 2]
                            cnt[0] += 1
                            for kk in range(KC):
                                k.op("pe", lambda e: e.matmul(ps[:], lhsT=hT[:, kk, tt * 128:(tt + 1) * 128], rhs=Wt[:, kk, :],
                                                              start=(kk == 0), stop=(kk == KC - 1)), r=[hT, Wt], w=[ps])
                            k.op("act", lambda e: e.activation(out=o[:], in_=ps[:], func=AF.Identity), r=[ps], w=[o])
                            k.dma(dst[tb * 512 + tt * 128: tb * 512 + (tt + 1) * 128, :], o[:], r=[o], w=[dst], q="pool")
                        continue
                    nsub = (ncol + 127) // 128
                    for sub in range(nsub):
                        mrows = min(128, ncol - sub * 128)
                        ps = psA[cnt[0] % 2]
                        o = ob[cnt[0] % 2]
                        o32 = of[cnt[0] % 2]
                        cnt[0] += 1
                        for kk in range(KC):
                            k.op("pe", lambda e: e.matmul(ps[0:mrows, :], lhsT=Wt[:, kk, sub * 128:sub * 128 + mrows], rhs=hT[:, kk, :],
                                                          start=(kk == 0), stop=(kk == KC - 1)), r=[hT, Wt], w=[ps])
                        fc = ct * 4 + sub
                        if ct < 2:
                            k.op("act", lambda e: e.activation(out=o32[:], in_=ps[:], func=AF.Identity), r=[ps], w=[o32])
                            k.dma(sc["pT_d"][fc * 128:(fc + 1) * 128, ts], o32[:], r=[o32], w=["pT_d"], q="pool")
                        elif ct in (8, 9):
                            k.op("act", lambda e: e.activation(out=o[:], in_=ps[:], func=AF.Identity), r=[ps], w=[o])
                            k.dma(sc["kc_d"][ct - 8, sub, :, ts], o[:], r=[o], w=["kc_d"], q="pool")
                        elif ct == 14:
                            k.op("act", lambda e: e.activation(out=o32[0:72, :], in_=ps[0:72, :], func=AF.Sigmoid), r=[ps], w=[o32])
                            k.dma(sc["gT_d"][:, ts], o32[0:72, :], r=[o32], w=["gT_d"], q="pool")
                        else:
                            if ct < 8:
                                gcol = 0
                            elif ct == 10:
                                gcol = 2
                            else:
                                gcol = 3
                            ei_ = ecnt[0] % 2
                            ecnt[0] += 1
                            raw, sq, rs, qn, t1, t2, oqr = raw_[ei_], sq_[ei_], rs_[ei_], qn_[ei_], t1_[ei_], t2_[ei_], oqr_[ei_]
                            ps2, ps3 = ps2_[ei_], ps3_[ei_]
                            k.op("act", lambda e: e.activation(out=raw[:], in_=ps[:], func=AF.Identity), r=[ps], w=[raw])
                            k.op("act", lambda e: e.activation(out=sq[:], in_=ps[:], func=AF.Square), r=[ps], w=[sq])
                            k.op("pe", lambda e: e.matmul(ps2[:], lhsT=ones_f[:], rhs=sq[:], start=True, stop=True), r=[ones_f, sq], w=[ps2])
                            k.op("dve", lambda e: e.tensor_scalar(out=rs[:], in0=ps2[:], scalar1=1.0 / 128, scalar2=EPS,
                                                                  op0=ALU.mult, op1=ALU.add), r=[ps2], w=[rs])
                            k.op("act", lambda e: e.activation(out=sq[:], in_=rs[:], func=AF.Sqrt), r=[rs], w=[sq])
                            k.op("dve", lambda e: e.reciprocal(out=rs[:], in_=sq[:]), r=[sq], w=[rs])
                            k.op("dve", lambda e: e.scalar_tensor_tensor(out=qn[:], in0=raw[:], scalar=qkg[:, gcol:gcol + 1], in1=rs[:],
                                                                         op0=ALU.mult, op1=ALU.mult), r=[raw, qkg, rs], w=[qn])
                            if ct < 8:
                                h = fc - 8
                                k.op("act", lambda e: e.activation(out=o[:], in_=qn[:], func=AF.Identity), r=[qn], w=[o])
                                k.dma(sc["qn_d"][h, :, ts], o[:], r=[o], w=["qn_d"], q="pool")
                            k.op("pe", lambda e: e.matmul(ps3[:], lhsT=rotm[:], rhs=qn[:], start=True, stop=True), r=[rotm, qn], w=[ps3])
                            k.op("dve", lambda e: e.tensor_tensor(out=t1[:], in0=qn[:], in1=cosT[:, ts], op=ALU.mult), r=[qn, cosT], w=[t1])
                            k.op("dve", lambda e: e.tensor_tensor(out=t2[:], in0=ps3[:], in1=sinT[:, ts], op=ALU.mult), r=[ps3, sinT], w=[t2])
                            k.op("dve", lambda e: e.tensor_tensor(out=t1[:], in0=t1[:], in1=t2[:], op=ALU.add), r=[t1, t2], w=[t1])
                            ob2 = oqr
                            k.op("act", lambda e: e.activation(out=ob2[:], in_=t1[:], func=AF.Identity), r=[t1], w=[ob2])
                            if ct < 8:
                                k.dma(sc["qr_d"][fc - 8, :, ts], ob2[:], r=[ob2], w=["qr_d"], q="pool")
                            elif ct == 10:
                                k.dma(sc["ks_d"][sub, :, ts], ob2[:], r=[ob2], w=["ks_d"], q="pool")
                            else:
                                k.dma(sc["kw_d"][sub, :, ts], ob2[:], r=[ob2], w=["kw_d"], q="pool")
        k.barrier()


    if "pool" in phases:
        with contextlib.ExitStack() as st:
            pt = k.sb(st, "pl_pt", [128, T], F32)
            sa = k.sb(st, "pl_sa", [128, T], F32)
            sb_ = k.sb(st, "pl_sb", [128, T], F32)
            inv = k.sb(st, "pl_inv", [128, 4, T], F32)
            dT = k.sb(st, "pl_dT", [128, 2, T], BF16)
            wp = k.sb(st, "pl_wp", [128, 2, 256], F32)
            wpb = k.sb(st, "pl_wpb", [128, 2, 256], BF16)
            psl = k.sb(st, "pl_psl", [128, 8], F32)
            yo = [k.sb(st, "pl_yo%d" % i, [128, 512], BF16) for i in range(2)]
            k.dma(inv[:], ins["invcnt"][:, :, :], w=[inv])
            k.dma(psl[:], ins["pscale_l"][:, :], w=[psl])
            cnt = 0
            for gi in range(4):
                wwin = (2, 4, 8, 16)[gi]
                k.dma(wp[:], ins["w_pool"][gi].rearrange("(cc p) d -> p cc d", p=128), w=[wp])
                k.op("dve", lambda e: e.tensor_copy(out=wpb[:], in_=wp[:]), r=[wp], w=[wpb])
                for cc in range(2):
                    ch = gi * 2 + cc
                    k.dma(pt[:], sc["pT_d"][ch * 128:(ch + 1) * 128, :], r=["pT_d"], w=[pt])
                    cur = pt
                    bufs = [sa, sb_]
                    bi = 0
                    sh = 1
                    while sh < wwin:
                        nxt = bufs[bi]
                        bi ^= 1
                        k.op("dve", lambda e: e.tensor_tensor(out=nxt[:, sh:T], in0=cur[:, sh:T], in1=cur[:, 0:T - sh], op=ALU.add),
                             r=[cur], w=[nxt])
                        k.op("dve", lambda e: e.tensor_copy(out=nxt[:, 0:sh], in_=cur[:, 0:sh]), r=[cur], w=[nxt])
                        cur = nxt
                        sh *= 2
                    tmp = bufs[bi]
                    k.op("dve", lambda e: e.tensor_tensor(out=tmp[:], in0=cur[:], in1=inv[:, gi, :], op=ALU.mult), r=[cur, inv], w=[tmp])
                    k.op("dve", lambda e: e.tensor_tensor(out=dT[:, cc, :], in0=tmp[:], in1=pt[:], op=ALU.subtract), r=[tmp, pt], w=[dT])
                for dc in range(2):
                    for tb in range(4):
                        ps = PB[cnt % 2]
                        o = yo[cnt % 2]
                        cnt += 1
                        for cc in range(2):
                            k.op("pe", lambda e: e.matmul(ps[:], lhsT=wpb[:, cc, dc * 128:(dc + 1) * 128], rhs=dT[:, cc, tb * 512:(tb + 1) * 512],
                                                          start=(cc == 0), stop=(cc == 1)), r=[wpb, dT], w=[ps])
                        col = gi * 2 + dc
                        k.op("dve", lambda e: e.tensor_scalar(out=o[:], in0=ps[:], scalar1=psl[:, col:col + 1], scalar2=None, op0=ALU.mult),
                             r=[ps, psl], w=[o])
                        k.dma(sc["yT_d"][col * 128:(col + 1) * 128, tb * 512:(tb + 1) * 512], o[:], r=[o], w=["yT_d"])
        k.barrier()

    k.op("dve", lambda e: e.memset(kcmpT[:], 0.0), w=[kcmpT])
    k.op("dve", lambda e: e.memset(vcmp[:], 0.0), w=[vcmp])
    if "cmp" in phases:
        with contextlib.ExitStack() as st:
            src = k.sb(st, "cp_src", [128, T], BF16)
            w1s = [k.sb(st, "cp_w1s%d" % i, [128, 8, 256], F32) for i in range(2)]
            w1b = k.sb(st, "cp_w1b", [128, 32, 256], BF16)
            w2s = k.sb(st, "cp_w2s", [128, 2, 128], F32)
            w2b = k.sb(st, "cp_w2b", [128, 2, 128], BF16)
            pel = k.sb(st, "cp_pel", [128, 32], F32)
            peb = k.sb(st, "cp_peb", [128, 32], BF16)
            hidT = k.sb(st, "cp_hidT", [128, 2, 128], BF16)
            hpre = k.sb(st, "cp_hpre", [128, 128], F32)
            bias = k.sb(st, "cp_bias", [128, 2], F32)
            kf = k.sb(st, "cp_kf", [128, 128], F32)
            sq = k.sb(st, "cp_sq", [128, 128], F32)
            rs = k.sb(st, "cp_rs", [128, 128], F32)
            qkg = k.sb(st, "cp_qkg", [128, 4], F32)
            k.dma(qkg[:], ins["qkg_l"][:, :], w=[qkg])
            k.op("dve", lambda e: e.memset(hidT[:], 0.0), w=[hidT])
            for kv in range(2):
                w1v = ins["cmp_w1"][kv].rearrange("l d h -> d l h")
                for pc in range(4):
                    stg = w1s[pc % 2]
                    k.dma(stg[:], w1v[:, pc * 8:(pc + 1) * 8, :], w=[stg])
                    k.op("pool", lambda e: e.tensor_copy(out=w1b[:, pc * 8:(pc + 1) * 8, :], in_=stg[:]), r=[stg], w=[w1b])
                k.dma(w2s[:], ins["cmp_w2"][kv].rearrange("(hc p) d -> p hc d", p=128), w=[w2s])
                k.op("dve", lambda e: e.tensor_copy(out=w2b[:], in_=w2s[:]), r=[w2s], w=[w2b])
                k.dma(pel[:], ins["pe_l"][kv], w=[pel])
                k.op("dve", lambda e: e.tensor_copy(out=peb[:], in_=pel[:]), r=[pel], w=[peb])
                for hc in range(2):
                    ps = PB[4]
                    for l in range(32):
                        k.op("pe", lambda e: e.matmul(ps[:, 0:1], lhsT=w1b[:, l, hc * 128:(hc + 1) * 128], rhs=peb[:, l:l + 1],
                                                      start=(l == 0), stop=(l == 31)), r=[w1b, peb], w=[ps])
                    k.op("dve", lambda e: e.tensor_copy(out=bias[:, hc:hc + 1], in_=ps[:, 0:1]), r=[ps], w=[bias])
                for g in range(G):
                    k.dma(src[:], sc["kc_d"][kv, g, :, :], r=["kc_d"], w=[src])
                    for hc in range(2):
                        ps = PB[hc]
                        for l in range(32):
                            k.op("pe", lambda e: e.matmul(ps[:, 0:127], lhsT=w1b[:, l, hc * 128:(hc + 1) * 128],
                                                          rhs=src[:, l:l + 16 * 126 + 1:16],
                                                          start=(l == 0), stop=(l == 31)), r=[w1b, src], w=[ps])
                        k.op("dve", lambda e: e.tensor_scalar(out=hpre[:, 0:127], in0=ps[:, 0:127], scalar1=bias[:, hc:hc + 1], scalar2=None,
                                                              op0=ALU.add), r=[ps, bias], w=[hpre])
                        k.op("act", lambda e: e.activation(out=hidT[:, hc, 0:127], in_=hpre[:, 0:127], func=AF.Gelu), r=[hpre], w=[hidT])
                    if kv == 0:
                        ps = PB[2]
                        for hc in range(2):
                            k.op("pe", lambda e: e.matmul(ps[:, 0:127], lhsT=w2b[:, hc, :], rhs=hidT[:, hc, 0:127],
                                                          start=(hc == 0), stop=(hc == 1)), r=[w2b, hidT], w=[ps])
                        k.op("act", lambda e: e.activation(out=kf[:, 0:127], in_=ps[:, 0:127], func=AF.Identity), r=[ps], w=[kf])
                        k.op("act", lambda e: e.activation(out=sq[:, 0:127], in_=ps[:, 0:127], func=AF.Square), r=[ps], w=[sq])
                        k.op("pe", lambda e: e.matmul(PB[3][:, 0:127], lhsT=ones_f[:], rhs=sq[:, 0:127], start=True, stop=True),
                             r=[ones_f, sq], w=[PB[3]])
                        k.op("dve", lambda e: e.tensor_scalar(out=rs[:, 0:127], in0=PB[3][:, 0:127], scalar1=1.0 / 128, scalar2=EPS,
                                                              op0=ALU.mult, op1=ALU.add), r=[PB[3]], w=[rs])
                        k.op("act", lambda e: e.activation(out=sq[:, 0:127], in_=rs[:, 0:127], func=AF.Sqrt), r=[rs], w=[sq])
                        k.op("dve", lambda e: e.reciprocal(out=rs[:, 0:127], in_=sq[:, 0:127]), r=[sq], w=[rs])
                        k.op("dve", lambda e: e.scalar_tensor_tensor(out=kcmpT[:, g, 0:127], in0=kf[:, 0:127], scalar=qkg[:, 1:2],
                                                                     in1=rs[:, 0:127], op0=ALU.mult, op1=ALU.mult),
                             r=[kf, qkg, rs], w=[kcmpT])
                    else:
                        ps = PB[2]
                        for hc in range(2):
                            k.op("pe", lambda e: e.matmul(ps[0:127, 0:128], lhsT=hidT[:, hc, 0:127], rhs=w2b[:, hc, :],
                                                          start=(hc == 0), stop=(hc == 1)), r=[w2b, hidT], w=[ps])
                        k.op("act", lambda e: e.activation(out=vcmp[0:127, g, :], in_=ps[0:127, 0:128], func=AF.Identity), r=[ps], w=[vcmp])
        k.barrier()

    if "attn" in phases:
        with contextlib.ExitStack() as st:
            ksT = k.sb(st, "at_ksT", [128, T], BF16)
            kwT = k.sb(st, "at_kwT", [128, T], BF16)
            vs = k.sb(st, "at_vs", [128, 16, 128], BF16)
            vw = k.sb(st, "at_vw", [128, 16, 128], BF16)
            qn6 = k.sb(st, "at_qn6", [128, R, 512], BF16)
            qr6 = k.sb(st, "at_qr6", [128, R, 512], BF16)
            cmpm = k.sb(st, "at_cmpm", [128, T], F32)
            selmap_s = k.sb(st, "at_selmap", [128, NSEL], F32)
            tbl = k.sb(st, "at_tbl", [128, 16, NSEL], F32)
            eb = k.sb(st, "at_eb", [NSEL, 16, 128], F32)
            caus = k.sb(st, "at_caus", [128, 4, 512], F32)
            wmask = k.sb(st, "at_wmask", [128, 8, 512], F32)
            Pf = k.sb(st, "at_Pf", [128, 512], F32)
            Pn = k.sb(st, "at_Pn", [128, 512], F32)
            rden = k.sb(st, "at_rden", [128, 512], F32)
            Pb = [k.sb(st, "at_Pb%d" % i, [128, 512], BF16) for i in range(5)]
            score4 = k.sb(st, "at_score4", [128, 4, NSEL], F32)
            work = k.sb(st, "at_work", [128, NSEL], F32)
            m8 = k.sb(st, "at_m8", [128, 16], F32)
            selm = k.sb(st, "at_selm", [128, NSEL], F32)
            selT = k.sb(st, "at_selT", [NSEL, T], F32)
            M = k.sb(st, "at_M", [128, 16, 512], BF16)
            gb2 = [k.sb(st, "at_gb%d" % i, [128, 3, 512], F32) for i in range(2)]
            yacc = k.sb(st, "at_y", [128, 512], F32)
            wgt = k.sb(st, "at_wgt", [128, 512], F32)
            tmpo = k.sb(st, "at_tmpo", [128, 512], F32)
            yb = k.sb(st, "at_yb", [128, 512], BF16)
            k.dma(cmpm[:], ins["cmpmask"][:, :], w=[cmpm])
            k.dma(selmap_s[:], ins["selmap"][:, :], w=[selmap_s])
            k.dma(tbl[:], ins["tb"].rearrange("(tt p) j -> p tt j", p=128), w=[tbl])
            k.dma(eb[:], ins["eb"][:, :, :], w=[eb])
            k.dma(caus[:], ins["caus"][:, :, :], w=[caus])
            k.dma(wmask[:], ins["wmask"][:, :, :], w=[wmask])

            bcount = [0]

            def combine(b, O, Dn, gb):
                k.op("dve", lambda e: e.tensor_scalar(out=wgt[:], in0=Dn[:], scalar1=1e-30, scalar2=None, op0=ALU.max), r=[Dn], w=[wgt])
                k.op("dve", lambda e: e.reciprocal(out=wgt[:], in_=wgt[:]), r=[wgt], w=[wgt])
                k.op("dve", lambda e: e.tensor_tensor(out=wgt[:], in0=wgt[:], in1=gb[:, b, :], op=ALU.mult), r=[wgt, gb], w=[wgt])
                if b == 0:
                    k.op("dve", lambda e: e.tensor_tensor(out=yacc[:], in0=O[:], in1=wgt[:], op=ALU.mult), r=[O, wgt], w=[yacc])
                else:
                    k.op("dve", lambda e: e.tensor_tensor(out=tmpo[:], in0=O[:], in1=wgt[:], op=ALU.mult), r=[O, wgt], w=[tmpo])
                    k.op("pool", lambda e: e.tensor_tensor(out=yacc[:], in0=yacc[:], in1=tmpo[:], op=ALU.add), r=[yacc, tmpo], w=[yacc])

            for g in range(G):
                k.dma(ksT[:], sc["ks_d"][g, :, :], r=["ks_d"], w=[ksT])
                k.dma(kwT[:], sc["kw_d"][g, :, :], r=["kw_d"], w=[kwT])
                k.dma(vs[:], sc["vs_d"][:, g * 128:(g + 1) * 128].rearrange("(m p) d -> p m d", p=128), r=["vs_d"], w=[vs])
                k.dma(vw[:], sc["vw_d"][:, g * 128:(g + 1) * 128].rearrange("(m p) d -> p m d", p=128), r=["vw_d"], w=[vw])
                for c in range(4):
                    ts = slice(c * 512, (c + 1) * 512)
                    k.dma(qn6[:], sc["qn_d"][g * R:(g + 1) * R, :, ts].rearrange("r d t -> d r t"), r=["qn_d"], w=[qn6])
                    for r_ in range(R):
                        S = PB[r_ % 2]
                        k.op("pe", lambda e: e.matmul(S[:], lhsT=kcmpT[:, g, :], rhs=qn6[:, r_, :], start=True, stop=True), r=[kcmpT, qn6], w=[S])
                        k.op("act", lambda e: e.activation(out=Pf[:], in_=S[:], func=AF.Exp, scale=SCALE), r=[S], w=[Pf])
                        k.op("dve", lambda e: e.tensor_tensor(out=Pf[:], in0=Pf[:], in1=cmpm[:, ts], op=ALU.mult), r=[Pf, cmpm], w=[Pf])
                        k.op("pe", lambda e: e.matmul(PB[2][:], lhsT=ones_f[:], rhs=Pf[:], start=True, stop=True), r=[ones_f, Pf], w=[PB[2]])
                        k.op("dve", lambda e: e.tensor_scalar(out=rden[:], in0=PB[2][:], scalar1=1e-30, scalar2=None, op0=ALU.max), r=[PB[2]], w=[rden])
                        k.op("dve", lambda e: e.reciprocal(out=rden[:], in_=rden[:]), r=[rden], w=[rden])
                        k.op("dve", lambda e: e.tensor_tensor(out=Pn[:], in0=Pf[:], in1=rden[:], op=ALU.mult), r=[Pf, rden], w=[Pn])
                        for tt in range(4):
                            k.op("pe", lambda e: e.matmul(PB[3][:, tt * 32:(tt + 1) * 32], lhsT=Pn[:, tt * 128:(tt + 1) * 128], rhs=selmap_s[:, :],
                                                          start=(r_ == 0), stop=(r_ == R - 1)), r=[Pn, selmap_s], w=[PB[3]])
                    k.op("dve", lambda e: e.tensor_tensor(out=score4[:].rearrange("p a b -> p (a b)"), in0=PB[3][:, 0:128],
                                                          in1=tbl[:, c * 4:(c + 1) * 4, :].rearrange("p a b -> p (a b)"), op=ALU.add),
                         r=[PB[3], tbl], w=[score4])
                    for tt in range(4):
                        sc_ = score4[:, tt, :]
                        k.op("dve", lambda e: e.max(out=m8[:, 0:8], in_=sc_), r=[score4], w=[m8])
                        k.op("dve", lambda e: e.match_replace(out=work[:], in_to_replace=m8[:, 0:8], in_values=sc_, imm_value=-3e9), r=[score4, m8], w=[work])
                        k.op("dve", lambda e: e.max(out=m8[:, 8:16], in_=work[:]), r=[work], w=[m8])
                        k.op("dve", lambda e: e.tensor_scalar(out=selm[:], in0=sc_, scalar1=m8[:, 15:16], scalar2=None, op0=ALU.is_ge), r=[score4, m8], w=[selm])
                        k.op("pe", lambda e: e.transpose(out=PB[4][0:NSEL, tt * 128:(tt + 1) * 128], in_=selm[:], identity=ident_f[:]), r=[selm, ident_f], w=[PB[4]])
                    k.op("act", lambda e: e.activation(out=selT[:, ts], in_=PB[4][0:NSEL, :], func=AF.Identity), r=[PB[4]], w=[selT])
                for c in range(4):
                    ts = slice(c * 512, (c + 1) * 512)
                    nm = 4 * c + 4
                    for m in range(nm):
                        k.op("pe", lambda e: e.matmul(PB[7][:], lhsT=eb[:, m, :], rhs=selT[:, ts], start=True, stop=True), r=[eb, selT], w=[PB[7]])
                        if m >= 4 * c:
                            k.op("dve", lambda e: e.tensor_tensor(out=M[:, m, :], in0=PB[7][:], in1=caus[:, m - 4 * c, :], op=ALU.mult), r=[PB[7], caus], w=[("M", m)])
                        else:
                            k.op("act", lambda e: e.activation(out=M[:, m, :], in_=PB[7][:], func=AF.Identity), r=[PB[7]], w=[("M", m)])
                    k.dma(qn6[:], sc["qn_d"][g * R:(g + 1) * R, :, ts].rearrange("r d t -> d r t"), r=["qn_d"], w=[qn6])
                    k.dma(qr6[:], sc["qr_d"][g * R:(g + 1) * R, :, ts].rearrange("r d t -> d r t"), r=["qr_d"], w=[qr6])
                    jobs = []
                    for r_ in range(R):
                        h = g * R + r_
                        jobs.append(dict(kT=kcmpT[:, g, :], q=qn6[:, r_, :], mask=cmpm[:, ts], mk=cmpm, v=vcmp[:, g, :], br=0, h=h,
                                         first=True, last=True, kk=[kcmpT, qn6], vk=vcmp))
                        for m in range(nm):
                            jobs.append(dict(kT=ksT[:, m * 128:(m + 1) * 128], q=qr6[:, r_, :], mask=M[:, m, :], mk=("M", m), v=vs[:, m, :], br=1, h=h,
                                             first=(m == 0), last=(m == nm - 1), kk=[ksT, qr6], vk=vs))
                        m0 = max(0, 4 * c - 4)
                        for m in range(m0, nm):
                            jobs.append(dict(kT=kwT[:, m * 128:(m + 1) * 128], q=qr6[:, r_, :], mask=wmask[:, m - 4 * c + 4, :], mk=wmask, v=vw[:, m, :], br=2, h=h,
                                             first=(m == m0), last=(m == nm - 1), kk=[kwT, qr6], vk=vw))
                    Sb = [PB[0], PB[1], PB[2], PB[7]]
                    sets = [(PB[3], PB[4]), (PB[5], PB[6])]
                    LOOK = 3
                    nj = len(jobs)
                    for i in range(nj + LOOK):
                        if i < nj:
                            j = jobs[i]
                            S = Sb[i % 4]
                            if j["br"] == 0:
                                hp = j["h"] % 2
                                if (j["h"] * 4 + c) % 3 != 0:
                                    conv_step(1)
                                for b_ in range(3):
                                    k.dma(gb2[hp][:, b_, :], sc["gT_d"][j["h"] * 3 + b_:j["h"] * 3 + b_ + 1, ts].partition_broadcast(128), r=["gT_d"], w=[gb2[hp]])
                            k.op("pe", lambda e: e.matmul(S[:], lhsT=j["kT"], rhs=j["q"], start=True, stop=True), r=j["kk"], w=[S])
                        if i >= LOOK:
                            ii = i - LOOK
                            j = jobs[ii]
                            S = Sb[ii % 4]
                            P = Pb[ii % 5]
                            if j["first"]:
                                bcount[0] += 1
                            O, Dn = sets[bcount[0] % 2]
                            k.op("act", lambda e: e.activation(out=P[:], in_=S[:], func=AF.Exp, scale=SCALE), r=[S], w=[P])
                            k.op("pool" if j["br"] == 2 else "dve", lambda e: e.tensor_tensor(out=P[:], in0=P[:], in1=j["mask"], op=ALU.mult), r=[P, j["mk"]], w=[P])
                            k.op("pe", lambda e: e.matmul(O[:], lhsT=j["v"], rhs=P[:], start=j["first"], stop=j["last"]), r=[j["vk"], P], w=[O])
                            k.op("pe", lambda e: e.matmul(Dn[:], lhsT=ones_b[:], rhs=P[:], start=j["first"], stop=j["last"]), r=[ones_b, P], w=[Dn])
                            if j["last"]:
                                combine(j["br"], O, Dn, gb2[j["h"] % 2])
                                if j["br"] == 2:
                                    hh = j["h"]
                                    k.op("act", lambda e: e.activation(out=yb[:], in_=yacc[:], func=AF.Identity), r=[yacc], w=[yb])
                                    k.dma(sc["yT_d"][1024 + hh * 128:1024 + (hh + 1) * 128, ts], yb[:], r=[yb], w=["yT_d"])
        k.barrier()

    if "outproj" in phases:
        with contextlib.ExitStack() as st:
            yT = k.sb(st, "op_yT", [128, KC, 512], BF16)
            W = [k.sb(st, "op_W%d" % i, [128, KC, 512], BF16) for i in range(2)]
            g1b = k.sb(st, "op_g1b", [128, D], F32)
            xt2 = [k.sb(st, "op_xt%d" % i, [128, 512], F32) for i in range(2)]
            o2 = [k.sb(st, "op_o%d" % i, [128, 512], F32) for i in range(2)]
            modrow = sc["mod_d"].rearrange("(a f) -> a f", a=1)
            k.dma(g1b[:], modrow[0:1, 2 * D:3 * D].partition_broadcast(128), r=["mod_d"], w=[g1b])
            yv = sc["yT_d"].rearrange("(k p) t -> p k t", p=128)
            wv = sc["wo16_d"].rearrange("(k p) f -> p k f", p=128)
            cnt = 0
            for tb in range(4):
                ts = slice(tb * 512, (tb + 1) * 512)
                for pc in range(4):
                    k.dma(yT[:, pc * 8:(pc + 1) * 8, :], yv[:, pc * 8:(pc + 1) * 8, ts], r=["yT_d"], w=[yT])
                for fb in range(8):
                    fs = slice(fb * 512, (fb + 1) * 512)
                    Wt = W[fb % 2]
                    for pc in range(4):
                        k.dma(Wt[:, pc * 8:(pc + 1) * 8, :], wv[:, pc * 8:(pc + 1) * 8, fs], r=["wo16_d"], w=[Wt], q="sp")
                    for tt in range(4):
                        ps = PB[cnt % 2]
                        xx = xt2[cnt % 2]
                        oo = o2[cnt % 2]
                        cnt += 1
                        rows = slice(tb * 512 + tt * 128, tb * 512 + (tt + 1) * 128)
                        k.dma(xx[:], ins["x"][rows, fs], w=[xx])
                        for kk in range(KC):
                            k.op("pe", lambda e: e.matmul(ps[:], lhsT=yT[:, kk, tt * 128:(tt + 1) * 128], rhs=Wt[:, kk, :],
                                                          start=(kk == 0), stop=(kk == KC - 1)), r=[yT, Wt], w=[ps])
                        k.op("dve", lambda e: e.tensor_tensor(out=oo[:], in0=ps[:], in1=g1b[:, fs], op=ALU.mult), r=[ps, g1b], w=[oo])
                        k.op("dve", lambda e: e.tensor_tensor(out=oo[:], in0=oo[:], in1=xx[:], op=ALU.add), r=[oo, xx], w=[oo])
                        k.dma(sc["x1_d"][rows, fs], oo[:], r=[oo], w=["x1_d"], q="pool")
        k.barrier()

    if "peer" in phases:
        with contextlib.ExitStack() as st:
            xt = k.sb(st, "pr_xt", [128, D], F32)
            xn = k.sb(st, "pr_xn", [128, D], BF16)
            junk = k.sb(st, "pr_junk", [128, D], BF16)
            ss = k.sb(st, "pr_ss", [128, 4], F32)
            hT = k.sb(st, "pr_hT", [128, KC, 512], BF16)
            Wt2 = [k.sb(st, "pr_W%d" % i, [128, KC, 512], BF16) for i in range(2)]
            kraw = k.sb(st, "pr_kraw", [128, 16, 128], F32)
            keysT = k.sb(st, "pr_keysT", [128, 16, 128], F32)
            qT = k.sb(st, "pr_qT", [128, 16, 512], F32)
            S = k.sb(st, "pr_S", [128, 16, 128], F32)
            wk = k.sb(st, "pr_wk", [128, 256], F32)
            v2 = k.sb(st, "pr_v2", [128, 2, 16], F32)
            i2u = k.sb(st, "pr_i2u", [128, 2, 16], U32)
            i2f = k.sb(st, "pr_i2f", [128, 2, 16], F32)
            cand = k.sb(st, "pr_cand", [128, 16, 16], F32)
            cidx = k.sb(st, "pr_cidx", [128, 16, 16], F32)
            tops = k.sb(st, "pr_tops", [128, 16], F32)
            ef = k.sb(st, "pr_ef", [128, 128], F32)
            ei = k.sb(st, "pr_ei", [128, 128], I32)
            gt = k.sb(st, "pr_gt", [128, 128], F32)
            sm = k.sb(st, "pr_sm", [128, 4], F32)
            ex = k.sb(st, "pr_ex", [128, 16], F32)
            iot = k.sb(st, "pr_iota", [128, 256], F32)
            wk2 = k.sb(st, "pr_wk2", [128, 256], F32)
            posu = k.sb(st, "pr_posu", [128, 16], U32)
            posf = k.sb(st, "pr_posf", [128, 16], F32)
            k.dma(iot[:], ins["iota256"][:, :], w=[iot])
            k.dma(kraw[:], ins["peer_keys"].rearrange("a n d -> n a d"), w=[kraw])
            for hc in range(16):
                pk = PB[4 + (hc // 4) % 2]
                k.op("pe", lambda e: e.transpose(out=pk[:, (hc % 4) * 128:(hc % 4 + 1) * 128], in_=kraw[:, hc, :], identity=ident_f[:]),
                     r=[kraw, ident_f], w=[pk])
                if hc % 4 == 3:
                    k.op("act", lambda e: e.activation(out=keysT[:, hc - 3:hc + 1, :].rearrange("p a b -> p (a b)"), in_=pk[:], func=AF.Identity),
                         r=[pk], w=[keysT])
            wv = sc["wq16_d"].rearrange("(k p) f -> p k f", p=128)
            pT = [PBb[0], PBb[1]]
            cnt = 0
            for tb in range(4):
                norm_block((xt, xn, junk, ss, pT), sc["x1_d"], tb, A2, B2, hT)
                for wt_ in range(4):
                    Wt = Wt2[wt_ % 2]
                    for pc in range(4):
                        k.dma(Wt[:, pc * 8:(pc + 1) * 8, :], wv[:, pc * 8:(pc + 1) * 8, wt_ * 512:(wt_ + 1) * 512], r=["wq16_d"], w=[Wt], q="sp")
                    for sub in range(4):
                        hc = wt_ * 4 + sub
                        ps = PB[2 + cnt % 2]
                        cnt += 1
                        for kk in range(KC):
                            k.op("pe", lambda e: e.matmul(ps[:], lhsT=Wt[:, kk, sub * 128:(sub + 1) * 128], rhs=hT[:, kk, :],
                                                          start=(kk == 0), stop=(kk == KC - 1)), r=[Wt, hT], w=[ps])
                        k.op("act", lambda e: e.activation(out=qT[:, hc, :], in_=ps[:], func=AF.Identity), r=[ps], w=[("qT", hc)])
                for tt in range(4):
                    rows = slice(tb * 512 + tt * 128, tb * 512 + (tt + 1) * 128)
                    for hc in range(16):
                        pk = PB[4 + (hc // 4) % 2]
                        k.op("pe", lambda e: e.matmul(pk[:, (hc % 4) * 128:(hc % 4 + 1) * 128], lhsT=qT[:, hc, tt * 128:(tt + 1) * 128], rhs=keysT[:, hc, :],
                                                      start=True, stop=True), r=[("qT", hc), keysT], w=[pk])
                        if hc % 4 == 3:
                            k.op("act", lambda e: e.activation(out=S[:, hc - 3:hc + 1, :].rearrange("p a b -> p (a b)"), in_=pk[:], func=AF.Identity),
                                 r=[pk], w=[("S", hc // 4)])
                    for h in range(8):
                        skey = ("S", h // 2)
                        for c2 in range(2):
                            sv = S[:, 2 * h + c2, :]
                            k.op("dve", lambda e: e.max(out=v2[:, c2, 0:8], in_=sv), r=[skey], w=[v2])
                            k.op("dve", lambda e: e.max_index(out=i2u[:, c2, 0:8], in_max=v2[:, c2, 0:8], in_values=sv), r=[skey, v2], w=[i2u])
                            k.op("dve", lambda e: e.match_replace(out=wk[:, 0:128], in_to_replace=v2[:, c2, 0:8], in_values=sv, imm_value=-1e30), r=[skey, v2], w=[wk])
                            k.op("dve", lambda e: e.max(out=v2[:, c2, 8:16], in_=wk[:, 0:128]), r=[wk], w=[v2])
                            k.op("dve", lambda e: e.max_index(out=i2u[:, c2, 8:16], in_max=v2[:, c2, 8:16], in_values=wk[:, 0:128]), r=[wk, v2], w=[i2u])
                        k.op("dve", lambda e: e.tensor_copy(out=i2f[:], in_=i2u[:]), r=[i2u], w=[i2f])
                        k.op("dve", lambda e: e.tensor_scalar(out=i2f[:, 0, :], in0=i2f[:, 0, :], scalar1=128.0, scalar2=None, op0=ALU.mult), r=[i2f], w=[i2f])
                        k.op("dve", lambda e: e.tensor_tensor(out=cand[:], in0=v2[:, 0, :].unsqueeze(2).to_broadcast([128, 16, 16]),
                                                              in1=v2[:, 1, :].unsqueeze(1).to_broadcast([128, 16, 16]), op=ALU.add), r=[v2], w=[cand])
                        k.op("dve", lambda e: e.tensor_tensor(out=cidx[:], in0=i2f[:, 0, :].unsqueeze(2).to_broadcast([128, 16, 16]),
                                                              in1=i2f[:, 1, :].unsqueeze(1).to_broadcast([128, 16, 16]), op=ALU.add), r=[i2f], w=[cidx])
                        cf = cand[:].rearrange("p a b -> p (a b)")
                        xf = cidx[:].rearrange("p a b -> p (a b)")
                        k.op("dve", lambda e: e.max(out=tops[:, 0:8], in_=cf), r=[cand], w=[tops])
                        k.op("dve", lambda e: e.match_replace(out=wk[:], in_to_replace=tops[:, 0:8], in_values=cf, imm_value=-1e30), r=[cand, tops], w=[wk])
                        k.op("dve", lambda e: e.max(out=tops[:, 8:16], in_=wk[:]), r=[wk], w=[tops])
                        k.op("dve", lambda e: e.max_index(out=posu[:, 0:8], in_max=tops[:, 0:8], in_values=cf), r=[cand, tops], w=[posu])
                        k.op("dve", lambda e: e.max_index(out=posu[:, 8:16], in_max=tops[:, 8:16], in_values=wk[:]), r=[wk, tops], w=[posu])
                        k.op("dve", lambda e: e.tensor_copy(out=posf[:], in_=posu[:]), r=[posu], w=[posf])
                        k.op("dve", lambda e: e.memset(ef[:, h * 16:(h + 1) * 16], 0.0), w=[("ef", h)])
                        for kk in range(16):
                            k.op("dve", lambda e: e.scalar_tensor_tensor(out=wk2[:], in0=iot[:], scalar=posf[:, kk:kk + 1], in1=xf,
                                                                         op0=ALU.is_equal, op1=ALU.mult, accum_out=ef[:, h * 16 + kk:h * 16 + kk + 1]),
                                 r=[iot, cidx, posf], w=[wk2, ("ef", h)])
                        k.op("dve", lambda e: e.tensor_scalar(out=ef[:, h * 16:(h + 1) * 16], in0=ef[:, h * 16:(h + 1) * 16], scalar1=16383.0, scalar2=0.0,
                                                              op0=ALU.min, op1=ALU.max), r=[("ef", h)], w=[("ef", h)])
                        k.op("dve", lambda e: e.tensor_scalar(out=sm[:, 0:1], in0=tops[:, 0:1], scalar1=-1.0, scalar2=None, op0=ALU.mult), r=[tops], w=[sm])
                        k.op("dve", lambda e: e.memset(sm[:, 1:2], 0.0), w=[sm])
                        k.op("act", lambda e: e.activation(out=ex[:], in_=tops[:], func=AF.Exp, bias=sm[:, 0:1], accum_out=sm[:, 1:2]), r=[tops, sm], w=[ex, sm])
                        k.op("dve", lambda e: e.reciprocal(out=sm[:, 2:3], in_=sm[:, 1:2]), r=[sm], w=[sm])
                        k.op("dve", lambda e: e.tensor_scalar(out=gt[:, h * 16:(h + 1) * 16], in0=ex[:], scalar1=sm[:, 2:3], scalar2=None, op0=ALU.mult),
                             r=[ex, sm], w=[("gt", h)])
                    k.op("dve", lambda e: e.tensor_copy(out=ei[:], in_=ef[:]), r=[("ef", h) for h in range(8)], w=[ei])
                    k.dma(sc["eidx_d"][rows, :], ei[:], r=[ei], w=["eidx_d"], q="pool")
                    k.dma(sc["ef_d"][rows, :], ef[:], r=[("ef", h) for h in range(8)], w=["ef_d"], q="pool")
                    k.dma(sc["gts_d"][rows, :], gt[:], r=[("gt", h) for h in range(8)], w=["gts_d"], q="pool")
        k.barrier()

        conv_step(1000)
        with contextlib.ExitStack() as st:
            h2f = k.sb(st, "pe_h2f", [128, D], F32)
            h2b = k.sb(st, "pe_h2b", [128, D], BF16)
            A2b = k.sb(st, "pe_A2b", [128, D], F32)
            B2b = k.sb(st, "pe_B2b", [128, D], F32)
            Ug = [k.sb(st, "pe_Ug%d" % i, [128, D], BF16) for i in range(4)]
            Vg = [k.sb(st, "pe_Vg%d" % i, [128, D], BF16) for i in range(4)]
            x1c = [k.sb(st, "pe_x1c%d" % i, [128, 512], F32) for i in range(2)]
            g2c = [k.sb(st, "pe_g2c%d" % i, [128, 512], F32) for i in range(2)]
            junkb = k.sb(st, "pe_junk", [128, D], BF16)
            x1t = k.sb(st, "pe_x1t", [128, D], F32)
            Wall = k.sb(st, "pe_Wall", [128, 128 * 128], BF16)
            ei = k.sb(st, "pe_ei", [128, 128], I32)
            eft = k.sb(st, "pe_eft", [128, 128], F32)
            eiT2 = [k.sb(st, "pe_eiT%d" % i, [128, 128], I32) for i in range(2)]
            gt = k.sb(st, "pe_gt", [128, 128], F32)
            av = k.sb(st, "pe_a", [128, 128], F32)
            wv_ = k.sb(st, "pe_w", [128, 128], F32)
            wT = k.sb(st, "pe_wT", [128, 128], F32)
            ss = k.sb(st, "pe_ss", [128, 4], F32)
            modrow = sc["mod_d"].rearrange("(a f) -> a f", a=1)
            k.op("pool", lambda e: e.memset(Wall[:], 0.0), w=[Wall])
            k.dma(A2b[:], modrow[0:1, 4 * D:5 * D].partition_broadcast(128), r=["mod_d"], w=[A2b])
            k.dma(B2b[:], ins["g2_row"][0:1, :].partition_broadcast(128), w=[B2b])
            k.op("dve", lambda e: e.scalar_tensor_tensor(out=A2b[:], in0=A2b[:], scalar=1.0, in1=B2b[:], op0=ALU.add, op1=ALU.mult), r=[A2b, B2b], w=[A2b])
            k.dma(B2b[:], modrow[0:1, 3 * D:4 * D].partition_broadcast(128), r=["mod_d", A2b], w=[B2b])
            def prep(ti):
                rows = slice(ti * 128, (ti + 1) * 128)
                eT = eiT2[ti % 2]
                k.dma(x1t[:], sc["x1_d"][rows, :], r=["x1_d"], w=[x1t])
                k.dma(ei[:], sc["eidx_d"][rows, :], r=["eidx_d"], w=[ei])
                k.dma(eft[:], sc["ef_d"][rows, :], r=["ef_d"], w=[eft])
                k.dma(gt[:], sc["gts_d"][rows, :], r=["gts_d"], w=[gt])
                k.op("dve", lambda e: e.memset(ss[:], 0.0), w=[ss])
                k.op("act", lambda e: e.activation(out=junkb[:], in_=x1t[:], func=AF.Square, accum_out=ss[:, 0:1]), r=[x1t, ss], w=[junkb, ss])
                k.op("dve", lambda e: e.tensor_scalar(out=ss[:, 1:2], in0=ss[:, 0:1], scalar1=1.0 / D, scalar2=EPS, op0=ALU.mult, op1=ALU.add), r=[ss], w=[ss])
                k.op("act", lambda e: e.activation(out=ss[:, 3:4], in_=ss[:, 1:2], func=AF.Sqrt), r=[ss], w=[ss])
                k.op("dve", lambda e: e.reciprocal(out=ss[:, 2:3], in_=ss[:, 3:4]), r=[ss], w=[ss])
                k.op("dve", lambda e: e.scalar_tensor_tensor(out=h2f[:], in0=x1t[:], scalar=ss[:, 2:3], in1=A2b[:], op0=ALU.mult, op1=ALU.mult), r=[x1t, ss, A2b], w=[h2f])
                k.op("dve", lambda e: e.tensor_tensor(out=h2b[:], in0=h2f[:], in1=B2b[:], op=ALU.add), r=[h2f, B2b], w=[h2b])
                k.op("pe", lambda e: e.transpose(out=PB[0][:, 0:128], in_=eft[:], identity=ident_f[:]), r=[eft, ident_f], w=[PB[0]])
                k.op("dve", lambda e: e.tensor_copy(out=eT[:], in_=PB[0][:, 0:128]), r=[PB[0]], w=[eT])
                k.op("dve", lambda e: e.memset(av[:], 0.0), w=[av])

            def u_step(ti, s_):
                u = Ug[s_ % 4]
                k.gather(u[:], sc["u16_d"][:, :], ei[:, s_:s_ + 1], r=[ei, "u16_d"], w=[u])
                k.op("dve", lambda e: e.scalar_tensor_tensor(out=junkb[:], in0=u[:], scalar=1.0, in1=h2b[:], op0=ALU.mult, op1=ALU.mult,
                                                             accum_out=av[:, s_:s_ + 1]), r=[u, h2b], w=[junkb, ("av", s_)])

            def post(ti):
                k.op("act", lambda e: e.activation(out=wv_[:], in_=av[:], func=AF.Gelu), r=[av] + [("av", s_) for s_ in range(128)], w=[wv_])
                k.op("dve", lambda e: e.tensor_tensor(out=wv_[:], in0=wv_[:], in1=gt[:], op=ALU.mult), r=[wv_, gt], w=[wv_])
                k.op("pe", lambda e: e.transpose(out=PB[0][:, 0:128], in_=wv_[:], identity=ident_f[:]), r=[wv_, ident_f], w=[PB[0]])
                k.op("dve", lambda e: e.tensor_copy(out=Wall[:, 0:128 * 128:129], in_=PB[0][:, 0:128]), r=[PB[0]], w=[Wall])

            def v_step(ti, t_):
                u = Vg[t_ % 4]
                eT = eiT2[ti % 2]
                k.gather(u[:], sc["v16_d"][:, :], eT[:, t_:t_ + 1], r=[eT, "v16_d"], w=[u])
                for nb in range(8):
                    k.op("pe", lambda e: e.matmul(PB[nb][:], lhsT=Wall[:, t_ * 128:(t_ + 1) * 128], rhs=u[:, nb * 512:(nb + 1) * 512],
                                                  start=(t_ == 0), stop=(t_ == 127)), r=[Wall, u], w=[PB[nb]])

            def evac(ti):
                rows = slice(ti * 128, (ti + 1) * 128)
                for nb in range(8):
                    cs = slice(nb * 512, (nb + 1) * 512)
                    xc = x1c[nb % 2]
                    gc = g2c[nb % 2]
                    k.dma(xc[:], sc["x1_d"][rows, cs], r=["x1_d"], w=[xc])
                    k.dma(gc[:], modrow[0:1, 5 * D + nb * 512:5 * D + (nb + 1) * 512].partition_broadcast(128), r=["mod_d"], w=[gc])
                    k.op("dve", lambda e: e.tensor_tensor(out=h2f[:, cs], in0=PB[nb][:], in1=gc[:], op=ALU.mult), r=[PB[nb], gc, h2f], w=[("yo", nb)])
                    k.op("pool", lambda e: e.tensor_tensor(out=h2f[:, cs], in0=h2f[:, cs], in1=xc[:], op=ALU.add), r=[("yo", nb), xc], w=[("yo", nb)])
                k.dma(out[rows, :], h2f[:], r=[("yo", nb) for nb in range(8)], w=["out", h2f])

            prep(0)
            for s_ in range(128):
                u_step(0, s_)
            post(0)
            for ti in range(16):
                nxt = ti + 1 < 16
                if nxt:
                    prep(ti + 1)
                for s_ in range(128):
                    if nxt:
                        u_step(ti + 1, s_)
                    v_step(ti, s_)
                evac(ti)
                if nxt:
                    post(ti + 1)
        k.barrier()

    k.finish()
    es.close()
    return nc, k


def prep_core_inputs(inp, b, consts):
    f = lambda a: np.ascontiguousarray(a, dtype=np.float32)
    d = {}
    d["x"] = f(inp["x"][b])
    d["c_l"] = f(inp["c"][b].reshape(KC, 128).T)
    d["w_ada"] = f(inp["w_ada"][0])
    d["b_ada"] = f(inp["b_ada"][0].reshape(1, -1))
    d["g1_l"] = f(inp["norm1_g"][0].reshape(KC, 128).T)
    d["g2_l"] = f(inp["norm2_g"][0].reshape(KC, 128).T)
    d["g2_row"] = f(inp["norm2_g"][0].reshape(1, -1))
    d["w_in"] = f(inp["w_in"][0])
    d["w_out"] = f(inp["w_out"][0])
    d["w_pool"] = f(inp["w_pool"][0])
    d["pscale_l"] = f(inp["pool_scale"][0].reshape(8, 128).T)
    d["qkg_l"] = f(np.concatenate([inp["q_norm_g"][0][None, :], inp["k_norm_g"][0]], 0).T)
    d["pe_l"] = f(np.transpose(inp["cmp_pe"][0], (0, 2, 1)))
    d["cmp_w1"] = f(inp["cmp_w1"][0])
    d["cmp_w2"] = f(inp["cmp_w2"][0])
    d["w_pq"] = f(inp["w_pq"][0])
    d["peer_keys"] = f(inp["peer_keys"][0].reshape(16, 128, 128))
    d["peer_u"] = f(inp["peer_u"][0])
    d["peer_v"] = f(inp["peer_v"][0])
    d.update(consts)
    return d


def sim_deadlock(k):
    streams = {}
    for e in k.log:
        streams.setdefault(e[0], []).append(e)
    pc = {e: 0 for e in streams}
    sem = {}
    progress = True
    while progress:
        progress = False
        for e, lst in streams.items():
            while pc[e] < len(lst):
                _, kind, s, v = lst[pc[e]]
                if kind == "w":
                    if sem.get(s, 0) >= v:
                        pc[e] += 1
                        progress = True
                    else:
                        break
                else:
                    sem[s] = sem.get(s, 0) + v
                    pc[e] += 1
                    progress = True
    stuck = {e: (pc[e], len(l), l[pc[e]] if pc[e] < len(l) else None) for e, l in streams.items() if pc[e] < len(l)}
    return stuck, sem


_CACHE = {}


def kernel(**inputs):
    inp = {n: np.asarray(v) for n, v in inputs.items()}
    consts = make_consts()
    if "nc" not in _CACHE:
        _CACHE["nc"] = build()[0]
    nc = _CACHE["nc"]
    in_maps = [prep_core_inputs(inp, b, consts) for b in range(8)]
    res = run_bass_kernel_spmd(nc, in_maps, core_ids=list(range(8)))
    return np.stack([np.asarray(r["out"], dtype=np.float32) for r in res.results], axis=0)
```

```python
import contextlib
import numpy as np
import concourse.bass as bass
import concourse.mybir as mybir
from concourse.bass_utils import run_bass_kernel_spmd

F32 = mybir.dt.float32
BF16 = mybir.dt.bfloat16
I32 = mybir.dt.int32
U32 = mybir.dt.uint32
AF = mybir.ActivationFunctionType
ALU = mybir.AluOpType

T = 2048
D = 4096
KC = 32
NH = 24
G = 4
R = 6
NCMP = 127
NSEL = 32
INW = 7240
EPS = 1e-6
SCALE = 128 ** -0.5


class MK:
    NLANES = 12

    def __init__(self, nc):
        self.nc = nc
        self.es = contextlib.ExitStack()
        self.eng = {"pe": nc.tensor, "dve": nc.vector, "act": nc.scalar,
                    "pool": nc.gpsimd, "sp": nc.sync}
        self.sem = {}
        self.cnt = {}
        for n in self.eng:
            self.sem[n] = self.es.enter_context(nc.semaphore("sem_" + n))
            self.cnt[n] = 0
        self.lanes = []
        for i in range(self.NLANES):
            n = "lane%d" % i
            self.sem[n] = self.es.enter_context(nc.semaphore(n))
            self.cnt[n] = 0
            self.lanes.append(n)
        self.lane_rr = 0
        self.clanes = []
        for i in range(2):
            n = "clane%d" % i
            self.sem[n] = self.es.enter_context(nc.semaphore(n))
            self.cnt[n] = 0
            self.clanes.append(n)
        self.clane_rr = 0
        self.inflight = []
        self.waited = {}
        self.last_w = {}
        self.readers = {}
        self.ninst = 0
        self.log = []

    @staticmethod
    def key(x):
        if isinstance(x, (str, tuple)):
            return x
        if hasattr(x, "tensor"):
            return x.tensor.name
        return x.name

    def _wait(self, engname, semname, val):
        if val <= 0:
            return
        k = (engname, semname)
        if self.waited.get(k, 0) >= val:
            return
        self.waited[k] = val
        self.eng[engname].wait_ge(self.sem[semname], val)
        self.ninst += 1
        self.log.append((engname, "w", semname, val))

    def _deps(self, engname, r, w):
        need = {}
        for x in r:
            lw = self.last_w.get(self.key(x))
            if lw:
                need[lw[0]] = max(need.get(lw[0], 0), lw[1])
        for x in w:
            k = self.key(x)
            lw = self.last_w.get(k)
            if lw:
                need[lw[0]] = max(need.get(lw[0], 0), lw[1])
            for (s, v) in self.readers.get(k, []):
                need[s] = max(need.get(s, 0), v)
        for s, v in need.items():
            if s == "pe" and engname == "pe":
                continue
            self._wait(engname, s, v)

    def _record(self, tok, r, w):
        for x in r:
            lst = self.readers.setdefault(self.key(x), [])
            lst[:] = [(s, v) for (s, v) in lst if s != tok[0]]
            lst.append(tok)
        for x in w:
            k = self.key(x)
            self.last_w[k] = tok
            self.readers[k] = []

    def op(self, engname, fn, r=(), w=()):
        self._deps(engname, r, w)
        inst = fn(self.eng[engname])
        self.cnt[engname] += 1
        inst.then_inc(self.sem[engname], 1)
        self.ninst += 1
        self.log.append((engname, "i", engname, 1))
        self._record((engname, self.cnt[engname]), r, w)
        return inst

    def _lane(self):
        lane = self.lanes[self.lane_rr]
        self.lane_rr = (self.lane_rr + 1) % self.NLANES
        return lane

    DESC_BUDGET = 2600

    def dma(self, out, in_, r=(), w=(), q="sp", conv=False, nd=128, **kw):
        if conv:
            lane = self.clanes[self.clane_rr]
            self.clane_rr = (self.clane_rr + 1) % len(self.clanes)
        else:
            lane = self._lane()
            while self.inflight and sum(x[2] for x in self.inflight) + nd > self.DESC_BUDGET:
                ol, oc, _ = self.inflight.pop(0)
                self._wait(q, ol, oc)
        self._deps(q, r, w)
        self._wait(q, lane, self.cnt[lane])
        inst = self.eng[q].dma_start(out=out, in_=in_, **kw)
        self.cnt[lane] += 16
        inst.then_inc(self.sem[lane], 16)
        self.ninst += 1
        self.log.append((q, "i", lane, 16))
        if not conv:
            self.inflight.append((lane, self.cnt[lane], nd))
        self._record((lane, self.cnt[lane]), r, w)
        return inst

    def gather(self, out, table, idx_ap, r=(), w=()):
        lane = self._lane()
        q = "pool"
        self._deps(q, r, w)
        self._wait(q, lane, self.cnt[lane])
        inst = self.nc.gpsimd.indirect_dma_start(
            out=out, out_offset=None, in_=table,
            in_offset=bass.IndirectOffsetOnAxis(ap=idx_ap, axis=0))
        self.cnt[lane] += 16
        inst.then_inc(self.sem[lane], 16)
        self.ninst += 1
        self.log.append((q, "i", lane, 16))
        self._record((lane, self.cnt[lane]), r, w)
        return inst

    def barrier(self):
        for e in self.eng:
            for s in self.sem:
                if s == e:
                    continue
                self._wait(e, s, self.cnt[s])
        self.last_w = {}
        self.readers = {}

    def finish(self, engname="sp"):
        for s in self.sem:
            self._wait(engname, s, self.cnt[s])

    def sb(self, stack, name, shape, dt):
        return stack.enter_context(self.nc.sbuf_tensor("s_" + name, shape, dt))

    def ps(self, stack, name, shape, dt=F32):
        return stack.enter_context(self.nc.psum_tensor("p_" + name, shape, dt))


def make_consts():
    c = {}
    c["ident_f"] = np.eye(128, dtype=np.float32)
    c["ones_f"] = np.ones((128, 128), np.float32)
    rot = np.zeros((128, 128), np.float32)
    for m in range(128):
        rot[(m + 64) % 128, m] = 1.0
    c["rotm"] = rot
    half = 64
    inv = (10000.0 ** (-np.arange(half, dtype=np.float32) / half)).astype(np.float32)
    pos = np.arange(T, dtype=np.float32)
    ang = (pos[:, None] * inv[None, :]).astype(np.float32)
    cos = np.cos(ang).astype(np.float32).T
    sin = np.sin(ang).astype(np.float32).T
    c["cosT"] = np.concatenate([cos, cos], 0).astype(np.float32)
    c["sinT"] = np.concatenate([-sin, sin], 0).astype(np.float32)
    n = np.arange(128)
    t = np.arange(T)
    cm = ((16 * n[:, None] + 31) <= t[None, :]) & (n[:, None] < NCMP)
    c["cmpmask"] = cm.astype(np.float32)
    cst = np.arange(NCMP)[:, None] * 16
    sst = np.arange(NSEL)[None, :] * 64
    ov = np.clip(np.minimum(cst + 32, sst + 64) - np.maximum(cst, sst), 0, None)
    sm = np.zeros((128, NSEL), np.float32)
    sm[:NCMP] = ov / 32.0
    c["selmap"] = sm
    j = np.arange(NSEL)
    cur = t // 64
    forced = (j[None, :] == 0) | (j[None, :] == cur[:, None]) | (j[None, :] == cur[:, None] - 1)
    valid = (j[None, :] * 64) <= t[:, None]
    c["tb"] = (np.where(forced, 1000.0, 0.0) + np.where(valid, 0.0, -1e9)).astype(np.float32)
    eb = np.zeros((NSEL, 16, 128), np.float32)
    for m in range(16):
        for kk in range(128):
            eb[2 * m + kk // 64, m, kk] = 1.0
    c["eb"] = eb
    kk = np.arange(128)[:, None]
    q = np.arange(512)[None, :]
    caus = np.zeros((128, 4, 512), np.float32)
    for d in range(4):
        caus[:, d, :] = ((q - kk) >= 128 * d)
    c["caus"] = caus
    wm = np.zeros((128, 8, 512), np.float32)
    for d in range(-4, 4):
        dist = q - kk - 128 * d
        wm[:, d + 4, :] = (dist >= 0) & (dist < 512)
    c["wmask"] = wm
    ic = np.zeros((128, 4, T), np.float32)
    for gi, w in enumerate((2, 4, 8, 16)):
        ic[:, gi, :] = 1.0 / np.minimum(t + 1, w).astype(np.float32)[None, :]
    c["invcnt"] = ic
    c["iota256"] = np.tile(np.arange(256, dtype=np.float32)[None, :], (128, 1))
    return c


CONST_SHAPES = {
    "ident_f": [128, 128], "ones_f": [128, 128], "rotm": [128, 128],
    "cosT": [128, T], "sinT": [128, T], "cmpmask": [128, T], "selmap": [128, NSEL],
    "tb": [T, NSEL], "eb": [NSEL, 16, 128], "caus": [128, 4, 512], "wmask": [128, 8, 512],
    "invcnt": [128, 4, T], "iota256": [128, 256],
}

INPUT_SHAPES = {
    "x": [T, D], "c_l": [128, KC], "w_ada": [D, 6 * D], "b_ada": [1, 6 * D],
    "g1_l": [128, KC], "g2_l": [128, KC], "g2_row": [1, D],
    "w_in": [D, INW], "w_out": [D, D], "w_pool": [4, 256, 256], "pscale_l": [128, 8],
    "qkg_l": [128, 4], "pe_l": [2, 128, 32], "cmp_w1": [2, 32, 128, 256], "cmp_w2": [2, 256, 128],
    "w_pq": [D, 2048], "peer_keys": [16, 128, 128], "peer_u": [16384, D], "peer_v": [16384, D],
}

SCRATCH = {
    "mod_d": ([6 * D], F32),
    "pT_d": ([1024, T], F32),
    "qn_d": ([NH, 128, T], BF16),
    "qr_d": ([NH, 128, T], BF16),
    "kc_d": ([2, G, 128, T], BF16),
    "ks_d": ([G, 128, T], BF16),
    "kw_d": ([G, 128, T], BF16),
    "vs_d": ([T, 512], BF16),
    "vw_d": ([T, 512], BF16),
    "gT_d": ([72, T], F32),
    "yT_d": ([D, T], BF16),
    "x1_d": ([T, D], F32),
    "eidx_d": ([T, 128], I32),
    "gts_d": ([T, 128], F32),
    "ef_d": ([T, 128], F32),
}
BIG_SCRATCH = {
    "u16_d": ([16384, D], BF16),
    "v16_d": ([16384, D], BF16),
    "wi16_d": ([D, INW], BF16),
    "wo16_d": ([D, D], BF16),
    "wq16_d": ([D, 2048], BF16),
}


def build(phases=("mod", "inproj", "pool", "cmp", "attn", "outproj", "peer"), debug=False,
          inputs_needed=None, ntb=4, dbg=99):
    nc = bass.Bass("TRN2", target_bir_lowering=False)
    k = MK(nc)
    es = k.es
    ins = {}
    for name, shp in list(INPUT_SHAPES.items()) + list(CONST_SHAPES.items()):
        if inputs_needed is not None and name not in inputs_needed:
            continue
        ins[name] = nc.dram_tensor(name, shp, F32, kind="ExternalInput").ap()
    out = nc.dram_tensor("out", [T, D], F32, kind="ExternalOutput").ap()
    sc = {}
    for name, (shp, dt) in SCRATCH.items():
        sc[name] = nc.dram_tensor(name, shp, dt, kind="ExternalOutput" if debug else "Internal").ap()
    for name, (shp, dt) in BIG_SCRATCH.items():
        sc[name] = nc.dram_tensor(name, shp, dt, kind="Internal").ap()
    conv_jobs = []
    if "peer" in phases:
        for i in range(64):
            rs_ = slice(i * 256, (i + 1) * 256)
            conv_jobs.append(("u16_d", "peer_u", rs_))
            conv_jobs.append(("v16_d", "peer_v", rs_))

    def conv_step(n=1):
        for _ in range(n):
            if conv_jobs:
                dn, sn, rs_ = conv_jobs.pop(0)
                k.dma(sc[dn][rs_, :], ins[sn][rs_, :], w=[dn], q="pool", conv=True)

    ident_f = k.sb(es, "ident_f_sb", [128, 128], F32)
    ident_b = k.sb(es, "ident_b_sb", [128, 128], BF16)
    ones_f = k.sb(es, "ones_f_sb", [128, 128], F32)
    ones_b = k.sb(es, "ones_b_sb", [128, 128], BF16)
    modT = k.sb(es, "modT", [128, 192], F32)
    A1 = k.sb(es, "A1", [128, KC], F32)
    A2 = k.sb(es, "A2", [128, KC], F32)
    kcmpT = k.sb(es, "kcmpT", [128, G, 128], BF16)
    vcmp = k.sb(es, "vcmp", [128, G, 128], BF16)
    PB = [k.ps(es, "bank%d" % i, [128, 512]) for i in range(8)]
    PBb = [b.bitcast(BF16) for b in PB]
    k.dma(ident_f[:], ins["ident_f"][:, :], w=[ident_f])
    k.dma(ones_f[:], ins["ones_f"][:, :], w=[ones_f])
    k.op("dve", lambda e: e.tensor_copy(out=ident_b[:], in_=ident_f[:]), r=[ident_f], w=[ident_b])
    k.op("dve", lambda e: e.tensor_copy(out=ones_b[:], in_=ones_f[:]), r=[ones_f], w=[ones_b])

    wconv_jobs = []
    for dn, sn, nrow, step in (("wi16_d", "w_in", D, 128),):
        if sn in ins:
            for r0 in range(0, nrow, step):
                wconv_jobs.append((dn, sn, r0, step))
    late = []
    for dn, sn, nrow, step in (("wo16_d", "w_out", D, 256), ("wq16_d", "w_pq", D, 512)):
        if sn in ins:
            for r0 in range(0, nrow, step):
                late.append((dn, sn, slice(r0, r0 + step)))
    conv_jobs[:0] = late
    if "attn" not in phases:
        conv_step(1000)

    def wconv_step(n=1):
        for _ in range(n):
            if wconv_jobs:
                dn, sn, r0, step = wconv_jobs.pop(0)
                k.dma(sc[dn][r0:r0 + step, :], ins[sn][r0:r0 + step, :], w=[dn], q="pool", conv=True)

    if "mod" not in phases:
        wconv_step(1000)

    if "mod" in phases:
        with contextlib.ExitStack() as st:
            cl = k.sb(st, "cl", [128, KC], F32)
            scl = k.sb(st, "scl", [128, KC], BF16)
            wst = [k.sb(st, "wada_s%d" % i, [128, 16, 512], F32) for i in range(3)]
            wbb = [k.sb(st, "wada_b%d" % i, [128, 16, 512], BF16) for i in range(3)]
            brow = [k.sb(st, "brow%d" % i, [1, 512], F32) for i in range(2)]
            mrow = [k.sb(st, "mrow%d" % i, [1, 512], F32) for i in range(2)]
            psm = [PB[0], PB[1]]
            k.dma(cl[:], ins["c_l"][:, :], w=[cl])
            k.op("act", lambda e: e.activation(out=scl[:], in_=cl[:], func=AF.Silu), r=[cl], w=[scl])
            wv = ins["w_ada"].rearrange("(k p) f -> p k f", p=128)
            modv = sc["mod_d"].rearrange("(a f) -> a f", a=1)
            nh = 0
            pend = None
            for fb in range(48):
                ps = psm[fb % 2]
                br = brow[fb % 2]
                mr = mrow[fb % 2]
                fs = slice(fb * 512, (fb + 1) * 512)
                wconv_step(1)
                k.dma(br[:], ins["b_ada"][0:1, fs], w=[br])
                halves = []
                for hf in range(2):
                    ws_ = wst[nh % 3]
                    w_ = wbb[nh % 3]
                    ce = ("dve", "act", "pool")[nh % 3]
                    nh += 1
                    k.dma(ws_[:], wv[:, hf * 16:(hf + 1) * 16, fs], w=[ws_], q="sp", nd=1024)
                    halves.append((ws_, w_, ce))
                if pend is not None:
                    k.dma(pend[0], pend[1][:], r=[pend[1]], w=["mod_d"], q="sp")
                for hf in range(2):
                    ws_, w_, ce = halves[hf]
                    if ce == "act":
                        k.op("act", lambda e: e.activation(out=w_[:], in_=ws_[:], func=AF.Identity), r=[ws_], w=[w_])
                    else:
                        k.op(ce, lambda e: e.tensor_copy(out=w_[:], in_=ws_[:]), r=[ws_], w=[w_])
                    for kk in range(16):
                        kg = hf * 16 + kk
                        k.op("pe", lambda e: e.matmul(ps[0:1, :], lhsT=scl[:, kg:kg + 1], rhs=w_[:, kk, :],
                                                      start=(kg == 0), stop=(kg == 31)),
                             r=[scl, w_], w=[ps])
                k.op("dve", lambda e: e.tensor_tensor(out=mr[:], in0=ps[0:1, :], in1=br[:], op=ALU.add),
                     r=[ps, br], w=[mr])
                pend = (modv[0:1, fs], mr)
            k.dma(pend[0], pend[1][:], r=[pend[1]], w=["mod_d"], q="sp")
            wconv_step(1000)
        k.barrier()

    with contextlib.ExitStack() as st:
        m1 = k.sb(st, "m1", [96, 128], F32)
        m2 = k.sb(st, "m2", [96, 128], F32)
        g1 = k.sb(st, "g1l", [128, KC], F32)
        g2 = k.sb(st, "g2l", [128, KC], F32)
        pst = PB[2]
        mv = sc["mod_d"].rearrange("(c p) -> c p", p=128)
        k.dma(m1[:], mv[0:96, :], r=["mod_d"], w=[m1])
        k.dma(m2[:], mv[96:192, :], r=["mod_d"], w=[m2])
        k.dma(g1[:], ins["g1_l"][:, :], w=[g1])
        k.dma(g2[:], ins["g2_l"][:, :], w=[g2])
        k.op("pe", lambda e: e.transpose(out=pst[:, 0:96], in_=m1[:], identity=ident_f[0:96, 0:96]), r=[m1, ident_f], w=[pst])
        k.op("pe", lambda e: e.transpose(out=pst[:, 96:192], in_=m2[:], identity=ident_f[0:96, 0:96]), r=[m2, ident_f], w=[pst])
        k.op("dve", lambda e: e.tensor_copy(out=modT[:], in_=pst[:, 0:192]), r=[pst], w=[modT])
        k.op("dve", lambda e: e.scalar_tensor_tensor(out=A1[:], in0=modT[:, 32:64], scalar=1.0, in1=g1[:],
                                                     op0=ALU.add, op1=ALU.mult), r=[modT, g1], w=[A1])
        k.op("dve", lambda e: e.scalar_tensor_tensor(out=A2[:], in0=modT[:, 128:160], scalar=1.0, in1=g2[:],
                                                     op0=ALU.add, op1=ALU.mult), r=[modT, g2], w=[A2])
    k.barrier()
    B1 = modT[:, 0:32]
    B2 = modT[:, 96:128]

    def hT_keys():
        return [("hT", kk, tt) for kk in range(KC) for tt in range(4)]

    def norm_block(st_bufs, src, tb, Atab, Btab, hT):
        xt, xn, junk, ss, pT = st_bufs
        for tt in range(4):
            t0 = tb * 512 + tt * 128
            k.dma(xt[:], src[t0:t0 + 128, :], w=[xt])
            k.op("dve", lambda e: e.memset(ss[:], 0.0), w=[ss])
            k.op("act", lambda e: e.activation(out=junk[:], in_=xt[:], func=AF.Square, accum_out=ss[:, 0:1]),
                 r=[xt, ss], w=[junk, ss])
            k.op("dve", lambda e: e.tensor_scalar(out=ss[:, 1:2], in0=ss[:, 0:1], scalar1=1.0 / D, scalar2=EPS,
                                                  op0=ALU.mult, op1=ALU.add), r=[ss], w=[ss])
            k.op("act", lambda e: e.activation(out=ss[:, 3:4], in_=ss[:, 1:2], func=AF.Sqrt), r=[ss], w=[ss])
            k.op("dve", lambda e: e.reciprocal(out=ss[:, 2:3], in_=ss[:, 3:4]), r=[ss], w=[ss])
            k.op("dve", lambda e: e.tensor_scalar(out=xn[:], in0=xt[:], scalar1=ss[:, 2:3], scalar2=None,
                                                  op0=ALU.mult), r=[xt, ss], w=[xn])
            if dbg == -1:
                continue
            for kg in range(KC // 4):
                p = pT[kg % 2]
                for q4 in range(4):
                    kk = kg * 4 + q4
                    sl = slice(q4 * 128, q4 * 128 + 128)
                    k.op("pe", lambda e: e.transpose(out=p[:, sl], in_=xn[:, kk * 128:(kk + 1) * 128], identity=ident_b[:]),
                         r=[xn, ident_b], w=[p])
                for q4 in range(4):
                    kk = kg * 4 + q4
                    sl = slice(q4 * 128, q4 * 128 + 128)
                    k.op("dve", lambda e: e.tensor_scalar(out=hT[:, kk, tt * 128:(tt + 1) * 128], in0=p[:, sl],
                                                          scalar1=Atab[:, kk:kk + 1], scalar2=Btab[:, kk:kk + 1],
                                                          op0=ALU.mult, op1=ALU.add),
                         r=[p], w=[hT])

    if "inproj" in phases:
        with contextlib.ExitStack() as st:
            xt = k.sb(st, "xt", [128, D], F32)
            xn = k.sb(st, "xn", [128, D], BF16)
            junk = k.sb(st, "junk", [128, D], BF16)
            ss = k.sb(st, "ss", [128, 4], F32)
            pT = [PBb[0], PBb[1]]
            hT = k.sb(st, "hT", [128, KC, 512], BF16)
            W = [k.sb(st, "W%d" % i, [128, KC, 512], BF16) for i in range(2)]
            cosT = k.sb(st, "cosT", [128, T], F32)
            sinT = k.sb(st, "sinT", [128, T], F32)
            rotm = k.sb(st, "rotm", [128, 128], F32)
            qkg = k.sb(st, "qkg", [128, 4], F32)
            psA = [PB[2], PB[3]]
            ps2_ = [PB[4], PB[6]]
            ps3_ = [PB[5], PB[7]]
            raw_ = [k.sb(st, "raw%d" % i, [128, 512], F32) for i in range(2)]
            sq_ = [k.sb(st, "sq%d" % i, [128, 512], F32) for i in range(2)]
            rs_ = [k.sb(st, "rs%d" % i, [128, 512], F32) for i in range(2)]
            qn_ = [k.sb(st, "qn%d" % i, [128, 512], F32) for i in range(2)]
            t1_ = [k.sb(st, "t1%d" % i, [128, 512], F32) for i in range(2)]
            t2_ = [k.sb(st, "t2%d" % i, [128, 512], F32) for i in range(2)]
            oqr_ = [k.sb(st, "oqr%d" % i, [128, 512], BF16) for i in range(2)]
            ecnt = [0]
            ob = [k.sb(st, "ob%d" % i, [128, 512], BF16) for i in range(2)]
            of = [k.sb(st, "of%d" % i, [128, 512], F32) for i in range(2)]
            k.dma(cosT[:], ins["cosT"][:, :], w=[cosT])
            k.dma(sinT[:], ins["sinT"][:, :], w=[sinT])
            k.dma(rotm[:], ins["rotm"][:, :], w=[rotm])
            k.dma(qkg[:], ins["qkg_l"][:, :], w=[qkg])
            wv = sc["wi16_d"].rearrange("(k p) f -> p k f", p=128)
            cnt = [0]

            def load_W(gti):
                ct_ = gti % 15
                ncol_ = 512 if ct_ < 14 else 72
                Wd = W[gti % 2]
                for pc in range(4):
                    k.dma(Wd[:, pc * 8:(pc + 1) * 8, 0:ncol_], wv[:, pc * 8:(pc + 1) * 8, ct_ * 512:ct_ * 512 + ncol_], r=["wi16_d"], w=[Wd], q="sp", nd=1024)

            for tb in range(ntb):
                ts = slice(tb * 512, (tb + 1) * 512)
                if dbg != 0:
                    norm_block((xt, xn, junk, ss, pT), ins["x"], tb, A1, B1, hT)
                if dbg <= 1:
                    continue
                for ct in range(15):
                    ncol = 512 if ct < 14 else 72
                    gti = tb * 15 + ct
                    Wt = W[gti % 2]
                    if gti == 0:
                        load_W(0)
                    if gti + 1 < ntb * 15:
                        load_W(gti + 1)
                    if ct in (11, 13):
                        dst = sc["vs_d"] if ct == 11 else sc["vw_d"]
                        for tt in range(4):
                            ps = psA[cnt[0] % 2]
                            o = ob[cnt[0] % 2]
                            cnt[0] += 1
                            for kk in range(KC):
                                k.op("pe", lambda e: e.matmul(ps[:], lhsT=hT[:, kk, tt * 128:(tt + 1) * 128], rhs=Wt[:, kk, :],
                                                              start=(kk == 0), stop=(kk == KC - 1)), r=[hT, Wt], w=[ps])
                            k.op("act", lambda e: e.activation(out=o[:], in_=ps[:], func=AF.Identity), r=[ps], w=[o])
                            k.dma(dst[tb * 512 + tt * 128: tb * 512 + (tt + 1) * 128, :], o[:], r=[o], w=[dst])
                        continue
                    nsub = (ncol + 127) // 128
                    for sub in range(nsub):
                        mrows = min(128, ncol - sub * 128)
                        ps = psA[cnt[0] % 2]
                        o = ob[cnt[0] % 2]
                        o32 = of[cnt[0] % 2]
                        cnt[0] += 1
                        for kk in range(KC):
                            k.op("pe", lambda e: e.matmul(ps[0:mrows, :], lhsT=Wt[:, kk, sub * 128:sub * 128 + mrows], rhs=hT[:, kk, :],
                                                          start=(kk == 0), stop=(kk == KC - 1)), r=[hT, Wt], w=[ps])
                        fc = ct * 4 + sub
                        if ct < 2:
                            k.op("act", lambda e: e.activation(out=o32[:], in_=ps[:], func=AF.Identity), r=[ps], w=[o32])
                            k.dma(sc["pT_d"][fc * 128:(fc + 1) * 128, ts], o32[:], r=[o32], w=["pT_d"])
                        elif ct in (8, 9):
                            k.op("act", lambda e: e.activation(out=o[:], in_=ps[:], func=AF.Identity), r=[ps], w=[o])
                            k.dma(sc["kc_d"][ct - 8, sub, :, ts], o[:], r=[o], w=["kc_d"])
                        elif ct == 14:
                            k.op("act", lambda e: e.activation(out=o32[0:72, :], in_=ps[0:72, :], func=AF.Sigmoid), r=[ps], w=[o32])
                            k.dma(sc["gT_d"][:, ts], o32[0:72, :], r=[o32], w=["gT_d"])
                        else:
                            if ct < 8:
                                gcol = 0
                            elif ct == 10:
                                gcol = 2
                            else:
                                gcol = 3
                            ei_ = ecnt[0] % 2
                            ecnt[0] += 1
                            raw, sq, rs, qn, t1, t2, oqr = raw_[ei_], sq_[ei_], rs_[ei_], qn_[ei_], t1_[ei_], t2_[ei_], oqr_[ei_]
                            ps2, ps3 = ps2_[ei_], ps3_[ei_]
                            k.op("act", lambda e: e.activation(out=raw[:], in_=ps[:], func=AF.Identity), r=[ps], w=[raw])
                            k.op("act", lambda e: e.activation(out=sq[:], in_=ps[:], func=AF.Square), r=[ps], w=[sq])
                            k.op("pe", lambda e: e.matmul(ps2[:], lhsT=ones_f[:], rhs=sq[:], start=True, stop=True), r=[ones_f, sq], w=[ps2])
                            k.op("dve", lambda e: e.tensor_scalar(out=rs[:], in0=ps2[:], scalar1=1.0 / 128, scalar2=EPS,
                                                                  op0=ALU.mult, op1=ALU.add), r=[ps2], w=[rs])
                            k.op("act", lambda e: e.activation(out=sq[:], in_=rs[:], func=AF.Sqrt), r=[rs], w=[sq])
                            k.op("dve", lambda e: e.reciprocal(out=rs[:], in_=sq[:]), r=[sq], w=[rs])
                            k.op("dve", lambda e: e.scalar_tensor_tensor(out=qn[:], in0=raw[:], scalar=qkg[:, gcol:gcol + 1], in1=rs[:],
                                                                         op0=ALU.mult, op1=ALU.mult), r=[raw, qkg, rs], w=[qn])
                            if ct < 8:
                                h = fc - 8
                                k.op("act", lambda e: e.activation(out=o[:], in_=qn[:], func=AF.Identity), r=[qn], w=[o])
                                k.dma(sc["qn_d"][h, :, ts], o[:], r=[o], w=["qn_d"])
                            k.op("pe", lambda e: e.matmul(ps3[:], lhsT=rotm[:], rhs=qn[:], start=True, stop=True), r=[rotm, qn], w=[ps3])
                            k.op("dve", lambda e: e.tensor_tensor(out=t1[:], in0=qn[:], in1=cosT[:, ts], op=ALU.mult), r=[qn, cosT], w=[t1])
                            k.op("dve", lambda e: e.tensor_tensor(out=t2[:], in0=ps3[:], in1=sinT[:, ts], op=ALU.mult), r=[ps3, sinT], w=[t2])
                            k.op("dve", lambda e: e.tensor_tensor(out=t1[:], in0=t1[:], in1=t2[:], op=ALU.add), r=[t1, t2], w=[t1])
                            ob2 = oqr
                            k.op("act", lambda e: e.activation(out=ob2[:], in_=t1[:], func=AF.Identity), r=[t1], w=[ob2])
                            if ct < 8:
                                k.dma(sc["qr_d"][fc - 8, :, ts], ob2[:], r=[ob2], w=["qr_d"])
                            elif ct == 10:
                                k.dma(sc["ks_d"][sub, :, ts], ob2[:], r=[ob2], w=["ks_d"])
                            else:
                                k.dma(sc["kw_d"][sub, :, ts], ob2[:], r=[ob2], w=["kw_d"])
        k.barrier()


    if "pool" in phases:
        with contextlib.ExitStack() as st:
            pt = k.sb(st, "pl_pt", [128, T], F32)
            sa = k.sb(st, "pl_sa", [128, T], F32)
            sb_ = k.sb(st, "pl_sb", [128, T], F32)
            inv = k.sb(st, "pl_inv", [128, 4, T], F32)
            dT = k.sb(st, "pl_dT", [128, 2, T], BF16)
            wp = k.sb(st, "pl_wp", [128, 2, 256], F32)
            wpb = k.sb(st, "pl_wpb", [128, 2, 256], BF16)
            psl = k.sb(st, "pl_psl", [128, 8], F32)
            yo = [k.sb(st, "pl_yo%d" % i, [128, 512], BF16) for i in range(2)]
            k.dma(inv[:], ins["invcnt"][:, :, :], w=[inv])
            k.dma(psl[:], ins["pscale_l"][:, :], w=[psl])
            cnt = 0
            for gi in range(4):
                wwin = (2, 4, 8, 16)[gi]
                k.dma(wp[:], ins["w_pool"][gi].rearrange("(cc p) d -> p cc d", p=128), w=[wp])
                k.op("dve", lambda e: e.tensor_copy(out=wpb[:], in_=wp[:]), r=[wp], w=[wpb])
                for cc in range(2):
                    ch = gi * 2 + cc
                    k.dma(pt[:], sc["pT_d"][ch * 128:(ch + 1) * 128, :], r=["pT_d"], w=[pt])
                    cur = pt
                    bufs = [sa, sb_]
                    bi = 0
                    sh = 1
                    while sh < wwin:
                        nxt = bufs[bi]
                        bi ^= 1
                        k.op("dve", lambda e: e.tensor_tensor(out=nxt[:, sh:T], in0=cur[:, sh:T], in1=cur[:, 0:T - sh], op=ALU.add),
                             r=[cur], w=[nxt])
                        k.op("dve", lambda e: e.tensor_copy(out=nxt[:, 0:sh], in_=cur[:, 0:sh]), r=[cur], w=[nxt])
                        cur = nxt
                        sh *= 2
                    tmp = bufs[bi]
                    k.op("dve", lambda e: e.tensor_tensor(out=tmp[:], in0=cur[:], in1=inv[:, gi, :], op=ALU.mult), r=[cur, inv], w=[tmp])
                    k.op("dve", lambda e: e.tensor_tensor(out=dT[:, cc, :], in0=tmp[:], in1=pt[:], op=ALU.subtract), r=[tmp, pt], w=[dT])
                for dc in range(2):
                    for tb in range(4):
                        ps = PB[cnt % 2]
                        o = yo[cnt % 2]
                        cnt += 1
                        for cc in range(2):
                            k.op("pe", lambda e: e.matmul(ps[:], lhsT=wpb[:, cc, dc * 128:(dc + 1) * 128], rhs=dT[:, cc, tb * 512:(tb + 1) * 512],
                                                          start=(cc == 0), stop=(cc == 1)), r=[wpb, dT], w=[ps])
                        col = gi * 2 + dc
                        k.op("dve", lambda e: e.tensor_scalar(out=o[:], in0=ps[:], scalar1=psl[:, col:col + 1], scalar2=None, op0=ALU.mult),
                             r=[ps, psl], w=[o])
                        k.dma(sc["yT_d"][col * 128:(col + 1) * 128, tb * 512:(tb + 1) * 512], o[:], r=[o], w=["yT_d"])
        k.barrier()

    k.op("dve", lambda e: e.memset(kcmpT[:], 0.0), w=[kcmpT])
    k.op("dve", lambda e: e.memset(vcmp[:], 0.0), w=[vcmp])
    if "cmp" in phases:
        with contextlib.ExitStack() as st:
            src = k.sb(st, "cp_src", [128, T], BF16)
            w1s = [k.sb(st, "cp_w1s%d" % i, [128, 8, 256], F32) for i in range(2)]
            w1b = k.sb(st, "cp_w1b", [128, 32, 256], BF16)
            w2s = k.sb(st, "cp_w2s", [128, 2, 128], F32)
            w2b = k.sb(st, "cp_w2b", [128, 2, 128], BF16)
            pel = k.sb(st, "cp_pel", [128, 32], F32)
            peb = k.sb(st, "cp_peb", [128, 32], BF16)
            hidT = k.sb(st, "cp_hidT", [128, 2, 128], BF16)
            hpre = k.sb(st, "cp_hpre", [128, 128], F32)
            bias = k.sb(st, "cp_bias", [128, 2], F32)
            kf = k.sb(st, "cp_kf", [128, 128], F32)
            sq = k.sb(st, "cp_sq", [128, 128], F32)
            rs = k.sb(st, "cp_rs", [128, 128], F32)
            qkg = k.sb(st, "cp_qkg", [128, 4], F32)
            k.dma(qkg[:], ins["qkg_l"][:, :], w=[qkg])
            k.op("dve", lambda e: e.memset(hidT[:], 0.0), w=[hidT])
            for kv in range(2):
                w1v = ins["cmp_w1"][kv].rearrange("l d h -> d l h")
                for pc in range(4):
                    stg = w1s[pc % 2]
                    k.dma(stg[:], w1v[:, pc * 8:(pc + 1) * 8, :], w=[stg])
                    k.op("pool", lambda e: e.tensor_copy(out=w1b[:, pc * 8:(pc + 1) * 8, :], in_=stg[:]), r=[stg], w=[w1b])
                k.dma(w2s[:], ins["cmp_w2"][kv].rearrange("(hc p) d -> p hc d", p=128), w=[w2s])
                k.op("dve", lambda e: e.tensor_copy(out=w2b[:], in_=w2s[:]), r=[w2s], w=[w2b])
                k.dma(pel[:], ins["pe_l"][kv], w=[pel])
                k.op("dve", lambda e: e.tensor_copy(out=peb[:], in_=pel[:]), r=[pel], w=[peb])
                for hc in range(2):
                    ps = PB[4]
                    for l in range(32):
                        k.op("pe", lambda e: e.matmul(ps[:, 0:1], lhsT=w1b[:, l, hc * 128:(hc + 1) * 128], rhs=peb[:, l:l + 1],
                                                      start=(l == 0), stop=(l == 31)), r=[w1b, peb], w=[ps])
                    k.op("dve", lambda e: e.tensor_copy(out=bias[:, hc:hc + 1], in_=ps[:, 0:1]), r=[ps], w=[bias])
                for g in range(G):
                    k.dma(src[:], sc["kc_d"][kv, g, :, :], r=["kc_d"], w=[src])
                    for hc in range(2):
                        ps = PB[hc]
                        for l in range(32):
                            k.op("pe", lambda e: e.matmul(ps[:, 0:127], lhsT=w1b[:, l, hc * 128:(hc + 1) * 128],
                                                          rhs=src[:, l:l + 16 * 126 + 1:16],
                                                          start=(l == 0), stop=(l == 31)), r=[w1b, src], w=[ps])
                        k.op("dve", lambda e: e.tensor_scalar(out=hpre[:, 0:127], in0=ps[:, 0:127], scalar1=bias[:, hc:hc + 1], scalar2=None,
                                                              op0=ALU.add), r=[ps, bias], w=[hpre])
                        k.op("act", lambda e: e.activation(out=hidT[:, hc, 0:127], in_=hpre[:, 0:127], func=AF.Gelu), r=[hpre], w=[hidT])
                    if kv == 0:
                        ps = PB[2]
                        for hc in range(2):
                            k.op("pe", lambda e: e.matmul(ps[:, 0:127], lhsT=w2b[:, hc, :], rhs=hidT[:, hc, 0:127],
                                                          start=(hc == 0), stop=(hc == 1)), r=[w2b, hidT], w=[ps])
                        k.op("act", lambda e: e.activation(out=kf[:, 0:127], in_=ps[:, 0:127], func=AF.Identity), r=[ps], w=[kf])
                        k.op("act", lambda e: e.activation(out=sq[:, 0:127], in_=ps[:, 0:127], func=AF.Square), r=[ps], w=[sq])
                        k.op("pe", lambda e: e.matmul(PB[3][:, 0:127], lhsT=ones_f[:], rhs=sq[:, 0:127], start=True, stop=True),
                             r=[ones_f, sq], w=[PB[3]])
                        k.op("dve", lambda e: e.tensor_scalar(out=rs[:, 0:127], in0=PB[3][:, 0:127], scalar1=1.0 / 128, scalar2=EPS,
                                                              op0=ALU.mult, op1=ALU.add), r=[PB[3]], w=[rs])
                        k.op("act", lambda e: e.activation(out=sq[:, 0:127], in_=rs[:, 0:127], func=AF.Sqrt), r=[rs], w=[sq])
                        k.op("dve", lambda e: e.reciprocal(out=rs[:, 0:127], in_=sq[:, 0:127]), r=[sq], w=[rs])
                        k.op("dve", lambda e: e.scalar_tensor_tensor(out=kcmpT[:, g, 0:127], in0=kf[:, 0:127], scalar=qkg[:, 1:2],
                                                                     in1=rs[:, 0:127], op0=ALU.mult, op1=ALU.mult),
                             r=[kf, qkg, rs], w=[kcmpT])
                    else:
                        ps = PB[2]
                        for hc in range(2):
                            k.op("pe", lambda e: e.matmul(ps[0:127, 0:128], lhsT=hidT[:, hc, 0:127], rhs=w2b[:, hc, :],
                                                          start=(hc == 0), stop=(hc == 1)), r=[w2b, hidT], w=[ps])
                        k.op("act", lambda e: e.activation(out=vcmp[0:127, g, :], in_=ps[0:127, 0:128], func=AF.Identity), r=[ps], w=[vcmp])
        k.barrier()

    if "attn" in phases:
        with contextlib.ExitStack() as st:
            ksT = k.sb(st, "at_ksT", [128, T], BF16)
            kwT = k.sb(st, "at_kwT", [128, T], BF16)
            vs = k.sb(st, "at_vs", [128, 16, 128], BF16)
            vw = k.sb(st, "at_vw", [128, 16, 128], BF16)
            qn6 = k.sb(st, "at_qn6", [128, R, 512], BF16)
            qr6 = k.sb(st, "at_qr6", [128, R, 512], BF16)
            cmpm = k.sb(st, "at_cmpm", [128, T], F32)
            selmap_s = k.sb(st, "at_selmap", [128, NSEL], F32)
            tbl = k.sb(st, "at_tbl", [128, 16, NSEL], F32)
            eb = k.sb(st, "at_eb", [NSEL, 16, 128], F32)
            caus = k.sb(st, "at_caus", [128, 4, 512], F32)
            wmask = k.sb(st, "at_wmask", [128, 8, 512], F32)
            Pf2 = [k.sb(st, "at_Pf%d" % i, [128, 512], F32) for i in range(2)]
            Pn2 = [k.sb(st, "at_Pn%d" % i, [128, 512], F32) for i in range(2)]
            rden2 = [k.sb(st, "at_rden%d" % i, [128, 512], F32) for i in range(2)]
            Pb = [k.sb(st, "at_Pb%d" % i, [128, 512], BF16) for i in range(5)]
            score4 = k.sb(st, "at_score4", [128, 4, NSEL], F32)
            work = k.sb(st, "at_work", [128, NSEL], F32)
            m8 = k.sb(st, "at_m8", [128, 16], F32)
            selm = k.sb(st, "at_selm", [128, NSEL], F32)
            selT = k.sb(st, "at_selT", [NSEL, T], F32)
            M = k.sb(st, "at_M", [128, 16, 512], BF16)
            gb2 = [k.sb(st, "at_gb%d" % i, [128, 3, 512], F32) for i in range(2)]
            yacc = k.sb(st, "at_y", [128, 512], F32)
            wgt = k.sb(st, "at_wgt", [128, 512], F32)
            tmpo = k.sb(st, "at_tmpo", [128, 512], F32)
            yb = k.sb(st, "at_yb", [128, 512], BF16)
            k.dma(cmpm[:], ins["cmpmask"][:, :], w=[cmpm])
            k.dma(selmap_s[:], ins["selmap"][:, :], w=[selmap_s])
            k.dma(tbl[:], ins["tb"].rearrange("(tt p) j -> p tt j", p=128), w=[tbl], nd=2048)
            k.dma(eb[:], ins["eb"][:, :, :], w=[eb])
            k.dma(caus[:], ins["caus"][:, :, :], w=[caus])
            k.dma(wmask[:], ins["wmask"][:, :, :], w=[wmask])

            bcount = [0]

            def combine(b, O, Dn, gb):
                k.op("dve", lambda e: e.tensor_scalar(out=wgt[:], in0=Dn[:], scalar1=1e-30, scalar2=None, op0=ALU.max), r=[Dn], w=[wgt])
                k.op("dve", lambda e: e.reciprocal(out=wgt[:], in_=wgt[:]), r=[wgt], w=[wgt])
                k.op("dve", lambda e: e.tensor_tensor(out=wgt[:], in0=wgt[:], in1=gb[:, b, :], op=ALU.mult), r=[wgt, gb], w=[wgt])
                if b == 0:
                    k.op("dve", lambda e: e.tensor_tensor(out=yacc[:], in0=O[:], in1=wgt[:], op=ALU.mult), r=[O, wgt], w=[yacc])
                else:
                    k.op("dve", lambda e: e.tensor_tensor(out=tmpo[:], in0=O[:], in1=wgt[:], op=ALU.mult), r=[O, wgt], w=[tmpo])
                    k.op("pool", lambda e: e.tensor_tensor(out=yacc[:], in0=yacc[:], in1=tmpo[:], op=ALU.add), r=[yacc, tmpo], w=[yacc])

            for g in range(G):
                k.dma(ksT[:], sc["ks_d"][g, :, :], r=["ks_d"], w=[ksT])
                k.dma(kwT[:], sc["kw_d"][g, :, :], r=["kw_d"], w=[kwT])
                k.dma(vs[:], sc["vs_d"][:, g * 128:(g + 1) * 128].rearrange("(m p) d -> p m d", p=128), r=["vs_d"], w=[vs], nd=2048)
                k.dma(vw[:], sc["vw_d"][:, g * 128:(g + 1) * 128].rearrange("(m p) d -> p m d", p=128), r=["vw_d"], w=[vw], nd=2048)
                for c in range(4):
                    ts = slice(c * 512, (c + 1) * 512)
                    k.dma(qn6[:], sc["qn_d"][g * R:(g + 1) * R, :, ts].rearrange("r d t -> d r t"), r=["qn_d"], w=[qn6], nd=768)
                    for r_ in range(R):
                        S = PB[r_ % 2]
                        Dq = (PB[2], PB[5])[r_ % 2]
                        Pf = Pf2[r_ % 2]
                        Pn = Pn2[r_ % 2]
                        rden = rden2[r_ % 2]
                        k.op("pe", lambda e: e.matmul(S[:], lhsT=kcmpT[:, g, :], rhs=qn6[:, r_, :], start=True, stop=True), r=[kcmpT, qn6], w=[S])
                        k.op("act", lambda e: e.activation(out=Pf[:], in_=S[:], func=AF.Exp, scale=SCALE), r=[S], w=[Pf])
                        k.op("pool", lambda e: e.tensor_tensor(out=Pf[:], in0=Pf[:], in1=cmpm[:, ts], op=ALU.mult), r=[Pf, cmpm], w=[Pf])
                        k.op("pe", lambda e: e.matmul(Dq[:], lhsT=ones_f[:], rhs=Pf[:], start=True, stop=True), r=[ones_f, Pf], w=[Dq])
                        k.op("dve", lambda e: e.tensor_scalar(out=rden[:], in0=Dq[:], scalar1=1e-30, scalar2=None, op0=ALU.max), r=[Dq], w=[rden])
                        k.op("dve", lambda e: e.reciprocal(out=rden[:], in_=rden[:]), r=[rden], w=[rden])
                        k.op("dve", lambda e: e.tensor_tensor(out=Pn[:], in0=Pf[:], in1=rden[:], op=ALU.mult), r=[Pf, rden], w=[Pn])
                        for tt in range(4):
                            k.op("pe", lambda e: e.matmul(PB[3][:, tt * 32:(tt + 1) * 32], lhsT=Pn[:, tt * 128:(tt + 1) * 128], rhs=selmap_s[:, :],
                                                          start=(r_ == 0), stop=(r_ == R - 1)), r=[Pn, selmap_s], w=[PB[3]])
                    k.op("dve", lambda e: e.tensor_tensor(out=score4[:].rearrange("p a b -> p (a b)"), in0=PB[3][:, 0:128],
                                                          in1=tbl[:, c * 4:(c + 1) * 4, :].rearrange("p a b -> p (a b)"), op=ALU.add),
                         r=[PB[3], tbl], w=[score4])
                    for tt in range(4):
                        sc_ = score4[:, tt, :]
                        k.op("dve", lambda e: e.max(out=m8[:, 0:8], in_=sc_), r=[score4], w=[m8])
                        k.op("dve", lambda e: e.match_replace(out=work[:], in_to_replace=m8[:, 0:8], in_values=sc_, imm_value=-3e9), r=[score4, m8], w=[work])
                        k.op("dve", lambda e: e.max(out=m8[:, 8:16], in_=work[:]), r=[work], w=[m8])
                        k.op("dve", lambda e: e.tensor_scalar(out=selm[:], in0=sc_, scalar1=m8[:, 15:16], scalar2=None, op0=ALU.is_ge), r=[score4, m8], w=[selm])
                        k.op("pe", lambda e: e.transpose(out=PB[4][0:NSEL, tt * 128:(tt + 1) * 128], in_=selm[:], identity=ident_f[:]), r=[selm, ident_f], w=[PB[4]])
                    k.op("act", lambda e: e.activation(out=selT[:, ts], in_=PB[4][0:NSEL, :], func=AF.Identity), r=[PB[4]], w=[selT])
                for c in range(4):
                    ts = slice(c * 512, (c + 1) * 512)
                    nm = 4 * c + 4
                    for m in range(nm):
                        k.op("pe", lambda e: e.matmul(PB[7][:], lhsT=eb[:, m, :], rhs=selT[:, ts], start=True, stop=True), r=[eb, selT], w=[PB[7]])
                        if m >= 4 * c:
                            k.op("dve", lambda e: e.tensor_tensor(out=M[:, m, :], in0=PB[7][:], in1=caus[:, m - 4 * c, :], op=ALU.mult), r=[PB[7], caus], w=[("M", m)])
                        else:
                            k.op("act", lambda e: e.activation(out=M[:, m, :], in_=PB[7][:], func=AF.Identity), r=[PB[7]], w=[("M", m)])
                    k.dma(qn6[:], sc["qn_d"][g * R:(g + 1) * R, :, ts].rearrange("r d t -> d r t"), r=["qn_d"], w=[qn6], nd=768)
                    k.dma(qr6[:], sc["qr_d"][g * R:(g + 1) * R, :, ts].rearrange("r d t -> d r t"), r=["qr_d"], w=[qr6], nd=768)
                    jobs = []
                    for r_ in range(R):
                        h = g * R + r_
                        jobs.append(dict(kT=kcmpT[:, g, :], q=qn6[:, r_, :], mask=cmpm[:, ts], mk=cmpm, v=vcmp[:, g, :], br=0, h=h,
                                         first=True, last=True, kk=[kcmpT, qn6], vk=vcmp))
                        for m in range(nm):
                            jobs.append(dict(kT=ksT[:, m * 128:(m + 1) * 128], q=qr6[:, r_, :], mask=M[:, m, :], mk=("M", m), v=vs[:, m, :], br=1, h=h,
                                             first=(m == 0), last=(m == nm - 1), kk=[ksT, qr6], vk=vs))
                        m0 = max(0, 4 * c - 4)
                        for m in range(m0, nm):
                            jobs.append(dict(kT=kwT[:, m * 128:(m + 1) * 128], q=qr6[:, r_, :], mask=wmask[:, m - 4 * c + 4, :], mk=wmask, v=vw[:, m, :], br=2, h=h,
                                             first=(m == m0), last=(m == nm - 1), kk=[kwT, qr6], vk=vw))
                    Sb = [PB[0], PB[1], PB[2], PB[7]]
                    sets = [(PB[3], PB[4]), (PB[5], PB[6])]
                    LOOK = 3
                    nj = len(jobs)
                    for i in range(nj + LOOK):
                        if i < nj:
                            j = jobs[i]
                            S = Sb[i % 4]
                            if j["br"] == 0:
                                hp = j["h"] % 2
                                conv_step(2)
                                for b_ in range(3):
                                    k.dma(gb2[hp][:, b_, :], sc["gT_d"][j["h"] * 3 + b_:j["h"] * 3 + b_ + 1, ts].partition_broadcast(128), r=["gT_d"], w=[gb2[hp]])
                            k.op("pe", lambda e: e.matmul(S[:], lhsT=j["kT"], rhs=j["q"], start=True, stop=True), r=j["kk"], w=[S])
                        if i >= LOOK:
                            ii = i - LOOK
                            j = jobs[ii]
                            S = Sb[ii % 4]
                            P = Pb[ii % 5]
                            if j["first"]:
                                bcount[0] += 1
                            O, Dn = sets[bcount[0] % 2]
                            k.op("act", lambda e: e.activation(out=P[:], in_=S[:], func=AF.Exp, scale=SCALE), r=[S], w=[P])
                            k.op("pool" if j["br"] == 2 else "dve", lambda e: e.tensor_tensor(out=P[:], in0=P[:], in1=j["mask"], op=ALU.mult), r=[P, j["mk"]], w=[P])
                            k.op("pe", lambda e: e.matmul(O[:], lhsT=j["v"], rhs=P[:], start=j["first"], stop=j["last"]), r=[j["vk"], P], w=[O])
                            k.op("pe", lambda e: e.matmul(Dn[:], lhsT=ones_b[:], rhs=P[:], start=j["first"], stop=j["last"]), r=[ones_b, P], w=[Dn])
                            if j["last"]:
                                combine(j["br"], O, Dn, gb2[j["h"] % 2])
                                if j["br"] == 2:
                                    hh = j["h"]
                                    k.op("act", lambda e: e.activation(out=yb[:], in_=yacc[:], func=AF.Identity), r=[yacc], w=[yb])
                                    k.dma(sc["yT_d"][1024 + hh * 128:1024 + (hh + 1) * 128, ts], yb[:], r=[yb], w=["yT_d"])
        k.barrier()

    if "outproj" in phases:
        while conv_jobs and conv_jobs[0][0] in ("wo16_d", "wq16_d"):
            conv_step(1)
        k.barrier()
        with contextlib.ExitStack() as st:
            yT = k.sb(st, "op_yT", [128, KC, 512], BF16)
            W = [k.sb(st, "op_W%d" % i, [128, KC, 512], BF16) for i in range(2)]
            g1b = k.sb(st, "op_g1b", [128, D], F32)
            xt2 = [k.sb(st, "op_xt%d" % i, [128, 512], F32) for i in range(2)]
            o2 = [k.sb(st, "op_o%d" % i, [128, 512], F32) for i in range(2)]
            modrow = sc["mod_d"].rearrange("(a f) -> a f", a=1)
            k.dma(g1b[:], modrow[0:1, 2 * D:3 * D].partition_broadcast(128), r=["mod_d"], w=[g1b])
            yv = sc["yT_d"].rearrange("(k p) t -> p k t", p=128)
            wv = sc["wo16_d"].rearrange("(k p) f -> p k f", p=128)
            cnt = 0

            def load_Wo(gti):
                fb_ = gti % 8
                Wd = W[gti % 2]
                for pc in range(4):
                    k.dma(Wd[:, pc * 8:(pc + 1) * 8, :], wv[:, pc * 8:(pc + 1) * 8, fb_ * 512:(fb_ + 1) * 512], r=["wo16_d"], w=[Wd], q="sp", nd=1024)

            for tb in range(4):
                ts = slice(tb * 512, (tb + 1) * 512)
                for pc in range(4):
                    k.dma(yT[:, pc * 8:(pc + 1) * 8, :], yv[:, pc * 8:(pc + 1) * 8, ts], r=["yT_d"], w=[yT], nd=1024)
                for fb in range(8):
                    fs = slice(fb * 512, (fb + 1) * 512)
                    gti = tb * 8 + fb
                    Wt = W[gti % 2]
                    if gti == 0:
                        load_Wo(0)
                    if gti + 1 < 32:
                        load_Wo(gti + 1)
                    for tt in range(4):
                        ps = PB[cnt % 2]
                        xx = xt2[cnt % 2]
                        oo = o2[cnt % 2]
                        cnt += 1
                        rows = slice(tb * 512 + tt * 128, tb * 512 + (tt + 1) * 128)
                        k.dma(xx[:], ins["x"][rows, fs], w=[xx])
                        for kk in range(KC):
                            k.op("pe", lambda e: e.matmul(ps[:], lhsT=yT[:, kk, tt * 128:(tt + 1) * 128], rhs=Wt[:, kk, :],
                                                          start=(kk == 0), stop=(kk == KC - 1)), r=[yT, Wt], w=[ps])
                        k.op("dve", lambda e: e.tensor_tensor(out=oo[:], in0=ps[:], in1=g1b[:, fs], op=ALU.mult), r=[ps, g1b], w=[oo])
                        k.op("dve", lambda e: e.tensor_tensor(out=oo[:], in0=oo[:], in1=xx[:], op=ALU.add), r=[oo, xx], w=[oo])
                        k.dma(sc["x1_d"][rows, fs], oo[:], r=[oo], w=["x1_d"])
        k.barrier()

    if "peer" in phases:
        with contextlib.ExitStack() as st:
            xt = k.sb(st, "pr_xt", [128, D], F32)
            xn = k.sb(st, "pr_xn", [128, D], BF16)
            junk = k.sb(st, "pr_junk", [128, D], BF16)
            ss = k.sb(st, "pr_ss", [128, 4], F32)
            hT = k.sb(st, "pr_hT", [128, KC, 512], BF16)
            Wt2 = [k.sb(st, "pr_W%d" % i, [128, KC, 512], BF16) for i in range(2)]
            kraw = k.sb(st, "pr_kraw", [128, 16, 128], F32)
            keysT = k.sb(st, "pr_keysT", [128, 16, 128], F32)
            qT = k.sb(st, "pr_qT", [128, 16, 512], F32)
            S = k.sb(st, "pr_S", [128, 16, 128], F32)
            wk = k.sb(st, "pr_wk", [128, 256], F32)
            v2 = k.sb(st, "pr_v2", [128, 2, 16], F32)
            i2u = k.sb(st, "pr_i2u", [128, 2, 16], U32)
            i2f = k.sb(st, "pr_i2f", [128, 2, 16], F32)
            cand = k.sb(st, "pr_cand", [128, 16, 16], F32)
            cidx = k.sb(st, "pr_cidx", [128, 16, 16], F32)
            tops = k.sb(st, "pr_tops", [128, 16], F32)
            ef = k.sb(st, "pr_ef", [128, 128], F32)
            ei = k.sb(st, "pr_ei", [128, 128], I32)
            gt = k.sb(st, "pr_gt", [128, 128], F32)
            sm = k.sb(st, "pr_sm", [128, 4], F32)
            ex = k.sb(st, "pr_ex", [128, 16], F32)
            iot = k.sb(st, "pr_iota", [128, 256], F32)
            wk2 = k.sb(st, "pr_wk2", [128, 256], F32)
            posu = k.sb(st, "pr_posu", [128, 16], U32)
            posf = k.sb(st, "pr_posf", [128, 16], F32)
            k.dma(iot[:], ins["iota256"][:, :], w=[iot])
            k.dma(kraw[:], ins["peer_keys"].rearrange("a n d -> n a d"), w=[kraw])
            for hc in range(16):
                pk = PB[4 + (hc // 4) % 2]
                k.op("pe", lambda e: e.transpose(out=pk[:, (hc % 4) * 128:(hc % 4 + 1) * 128], in_=kraw[:, hc, :], identity=ident_f[:]),
                     r=[kraw, ident_f], w=[pk])
                if hc % 4 == 3:
                    k.op("act", lambda e: e.activation(out=keysT[:, hc - 3:hc + 1, :].rearrange("p a b -> p (a b)"), in_=pk[:], func=AF.Identity),
                         r=[pk], w=[keysT])
            wv = sc["wq16_d"].rearrange("(k p) f -> p k f", p=128)
            pT = [PBb[0], PBb[1]]
            cnt = 0

            def load_Wq(gti):
                w_i = gti % 4
                Wd = Wt2[gti % 2]
                for pc in range(4):
                    k.dma(Wd[:, pc * 8:(pc + 1) * 8, :], wv[:, pc * 8:(pc + 1) * 8, w_i * 512:(w_i + 1) * 512], r=["wq16_d"], w=[Wd], q="sp", nd=1024)

            for tb in range(4):
                norm_block((xt, xn, junk, ss, pT), sc["x1_d"], tb, A2, B2, hT)
                for wt_ in range(4):
                    gti = tb * 4 + wt_
                    Wt = Wt2[gti % 2]
                    if gti == 0:
                        load_Wq(0)
                    if gti + 1 < 16:
                        load_Wq(gti + 1)
                    for sub in range(4):
                        hc = wt_ * 4 + sub
                        ps = PB[2 + cnt % 2]
                        cnt += 1
                        for kk in range(KC):
                            k.op("pe", lambda e: e.matmul(ps[:], lhsT=Wt[:, kk, sub * 128:(sub + 1) * 128], rhs=hT[:, kk, :],
                                                          start=(kk == 0), stop=(kk == KC - 1)), r=[Wt, hT], w=[ps])
                        k.op("act", lambda e: e.activation(out=qT[:, hc, :], in_=ps[:], func=AF.Identity), r=[ps], w=[("qT", hc)])
                for tt in range(4):
                    rows = slice(tb * 512 + tt * 128, tb * 512 + (tt + 1) * 128)
                    for hc in range(16):
                        pk = PB[4 + (hc // 4) % 2]
                        k.op("pe", lambda e: e.matmul(pk[:, (hc % 4) * 128:(hc % 4 + 1) * 128], lhsT=qT[:, hc, tt * 128:(tt + 1) * 128], rhs=keysT[:, hc, :],
                                                      start=True, stop=True), r=[("qT", hc), keysT], w=[pk])
                        if hc % 4 == 3:
                            k.op("act", lambda e: e.activation(out=S[:, hc - 3:hc + 1, :].rearrange("p a b -> p (a b)"), in_=pk[:], func=AF.Identity),
                                 r=[pk], w=[("S", hc // 4)])
                    for h in range(8):
                        skey = ("S", h // 2)
                        for c2 in range(2):
                            sv = S[:, 2 * h + c2, :]
                            k.op("dve", lambda e: e.max(out=v2[:, c2, 0:8], in_=sv), r=[skey], w=[v2])
                            k.op("dve", lambda e: e.max_index(out=i2u[:, c2, 0:8], in_max=v2[:, c2, 0:8], in_values=sv), r=[skey, v2], w=[i2u])
                            k.op("dve", lambda e: e.match_replace(out=wk[:, 0:128], in_to_replace=v2[:, c2, 0:8], in_values=sv, imm_value=-1e30), r=[skey, v2], w=[wk])
                            k.op("dve", lambda e: e.max(out=v2[:, c2, 8:16], in_=wk[:, 0:128]), r=[wk], w=[v2])
                            k.op("dve", lambda e: e.max_index(out=i2u[:, c2, 8:16], in_max=v2[:, c2, 8:16], in_values=wk[:, 0:128]), r=[wk, v2], w=[i2u])
                        k.op("dve", lambda e: e.tensor_copy(out=i2f[:], in_=i2u[:]), r=[i2u], w=[i2f])
                        k.op("dve", lambda e: e.tensor_scalar(out=i2f[:, 0, :], in0=i2f[:, 0, :], scalar1=128.0, scalar2=None, op0=ALU.mult), r=[i2f], w=[i2f])
                        k.op("dve", lambda e: e.tensor_tensor(out=cand[:], in0=v2[:, 0, :].unsqueeze(2).to_broadcast([128, 16, 16]),
                                                              in1=v2[:, 1, :].unsqueeze(1).to_broadcast([128, 16, 16]), op=ALU.add), r=[v2], w=[cand])
                        k.op("dve", lambda e: e.tensor_tensor(out=cidx[:], in0=i2f[:, 0, :].unsqueeze(2).to_broadcast([128, 16, 16]),
                                                              in1=i2f[:, 1, :].unsqueeze(1).to_broadcast([128, 16, 16]), op=ALU.add), r=[i2f], w=[cidx])
                        cf = cand[:].rearrange("p a b -> p (a b)")
                        xf = cidx[:].rearrange("p a b -> p (a b)")
                        k.op("dve", lambda e: e.max(out=tops[:, 0:8], in_=cf), r=[cand], w=[tops])
                        k.op("dve", lambda e: e.match_replace(out=wk[:], in_to_replace=tops[:, 0:8], in_values=cf, imm_value=-1e30), r=[cand, tops], w=[wk])
                        k.op("dve", lambda e: e.max(out=tops[:, 8:16], in_=wk[:]), r=[wk], w=[tops])
                        k.op("dve", lambda e: e.max_index(out=posu[:, 0:8], in_max=tops[:, 0:8], in_values=cf), r=[cand, tops], w=[posu])
                        k.op("dve", lambda e: e.max_index(out=posu[:, 8:16], in_max=tops[:, 8:16], in_values=wk[:]), r=[wk, tops], w=[posu])
                        k.op("dve", lambda e: e.tensor_copy(out=posf[:], in_=posu[:]), r=[posu], w=[posf])
                        k.op("dve", lambda e: e.memset(ef[:, h * 16:(h + 1) * 16], 0.0), w=[("ef", h)])
                        for kk in range(16):
                            k.op("dve", lambda e: e.scalar_tensor_tensor(out=wk2[:], in0=iot[:], scalar=posf[:, kk:kk + 1], in1=xf,
                                                                         op0=ALU.is_equal, op1=ALU.mult, accum_out=ef[:, h * 16 + kk:h * 16 + kk + 1]),
                                 r=[iot, cidx, posf], w=[wk2, ("ef", h)])
                        k.op("dve", lambda e: e.tensor_scalar(out=ef[:, h * 16:(h + 1) * 16], in0=ef[:, h * 16:(h + 1) * 16], scalar1=16383.0, scalar2=0.0,
                                                              op0=ALU.min, op1=ALU.max), r=[("ef", h)], w=[("ef", h)])
                        k.op("dve", lambda e: e.tensor_scalar(out=sm[:, 0:1], in0=tops[:, 0:1], scalar1=-1.0, scalar2=None, op0=ALU.mult), r=[tops], w=[sm])
                        k.op("dve", lambda e: e.memset(sm[:, 1:2], 0.0), w=[sm])
                        k.op("act", lambda e: e.activation(out=ex[:], in_=tops[:], func=AF.Exp, bias=sm[:, 0:1], accum_out=sm[:, 1:2]), r=[tops, sm], w=[ex, sm])
                        k.op("dve", lambda e: e.reciprocal(out=sm[:, 2:3], in_=sm[:, 1:2]), r=[sm], w=[sm])
                        k.op("dve", lambda e: e.tensor_scalar(out=gt[:, h * 16:(h + 1) * 16], in0=ex[:], scalar1=sm[:, 2:3], scalar2=None, op0=ALU.mult),
                             r=[ex, sm], w=[("gt", h)])
                    k.op("dve", lambda e: e.tensor_copy(out=ei[:], in_=ef[:]), r=[("ef", h) for h in range(8)], w=[ei])
                    k.dma(sc["eidx_d"][rows, :], ei[:], r=[ei], w=["eidx_d"])
                    k.dma(sc["ef_d"][rows, :], ef[:], r=[("ef", h) for h in range(8)], w=["ef_d"])
                    k.dma(sc["gts_d"][rows, :], gt[:], r=[("gt", h) for h in range(8)], w=["gts_d"])
        k.barrier()

        conv_step(1000)
        with contextlib.ExitStack() as st:
            h2f = k.sb(st, "pe_h2f", [128, D], F32)
            h2b = k.sb(st, "pe_h2b", [128, D], BF16)
            A2b = k.sb(st, "pe_A2b", [128, D], F32)
            B2b = k.sb(st, "pe_B2b", [128, D], F32)
            Ug = [k.sb(st, "pe_Ug%d" % i, [128, D], BF16) for i in range(4)]
            Vg = [k.sb(st, "pe_Vg%d" % i, [128, D], BF16) for i in range(4)]
            x1c = [k.sb(st, "pe_x1c%d" % i, [128, 512], F32) for i in range(2)]
            g2c = [k.sb(st, "pe_g2c%d" % i, [128, 512], F32) for i in range(2)]
            junkb = k.sb(st, "pe_junk", [128, D], BF16)
            x1t = k.sb(st, "pe_x1t", [128, D], F32)
            Wall = k.sb(st, "pe_Wall", [128, 128 * 128], BF16)
            ei = k.sb(st, "pe_ei", [128, 128], I32)
            eft = k.sb(st, "pe_eft", [128, 128], F32)
            eiT2 = [k.sb(st, "pe_eiT%d" % i, [128, 128], I32) for i in range(2)]
            gt = k.sb(st, "pe_gt", [128, 128], F32)
            av = k.sb(st, "pe_a", [128, 128], F32)
            wv_ = k.sb(st, "pe_w", [128, 128], F32)
            wT = k.sb(st, "pe_wT", [128, 128], F32)
            ss = k.sb(st, "pe_ss", [128, 4], F32)
            modrow = sc["mod_d"].rearrange("(a f) -> a f", a=1)
            k.op("pool", lambda e: e.memset(Wall[:], 0.0), w=[Wall])
            k.dma(A2b[:], modrow[0:1, 4 * D:5 * D].partition_broadcast(128), r=["mod_d"], w=[A2b])
            k.dma(B2b[:], ins["g2_row"][0:1, :].partition_broadcast(128), w=[B2b])
            k.op("dve", lambda e: e.scalar_tensor_tensor(out=A2b[:], in0=A2b[:], scalar=1.0, in1=B2b[:], op0=ALU.add, op1=ALU.mult), r=[A2b, B2b], w=[A2b])
            k.dma(B2b[:], modrow[0:1, 3 * D:4 * D].partition_broadcast(128), r=["mod_d", A2b], w=[B2b])
            def prep(ti):
                rows = slice(ti * 128, (ti + 1) * 128)
                eT = eiT2[ti % 2]
                k.dma(x1t[:], sc["x1_d"][rows, :], r=["x1_d"], w=[x1t])
                k.dma(ei[:], sc["eidx_d"][rows, :], r=["eidx_d"], w=[ei])
                k.dma(eft[:], sc["ef_d"][rows, :], r=["ef_d"], w=[eft])
                k.dma(gt[:], sc["gts_d"][rows, :], r=["gts_d"], w=[gt])
                k.op("dve", lambda e: e.memset(ss[:], 0.0), w=[ss])
                k.op("act", lambda e: e.activation(out=junkb[:], in_=x1t[:], func=AF.Square, accum_out=ss[:, 0:1]), r=[x1t, ss], w=[junkb, ss])
                k.op("dve", lambda e: e.tensor_scalar(out=ss[:, 1:2], in0=ss[:, 0:1], scalar1=1.0 / D, scalar2=EPS, op0=ALU.mult, op1=ALU.add), r=[ss], w=[ss])
                k.op("act", lambda e: e.activation(out=ss[:, 3:4], in_=ss[:, 1:2], func=AF.Sqrt), r=[ss], w=[ss])
                k.op("dve", lambda e: e.reciprocal(out=ss[:, 2:3], in_=ss[:, 3:4]), r=[ss], w=[ss])
                k.op("dve", lambda e: e.scalar_tensor_tensor(out=h2f[:], in0=x1t[:], scalar=ss[:, 2:3], in1=A2b[:], op0=ALU.mult, op1=ALU.mult), r=[x1t, ss, A2b], w=[h2f])
                k.op("dve", lambda e: e.tensor_tensor(out=h2b[:], in0=h2f[:], in1=B2b[:], op=ALU.add), r=[h2f, B2b], w=[h2b])
                k.op("pe", lambda e: e.transpose(out=PB[0][:, 0:128], in_=eft[:], identity=ident_f[:]), r=[eft, ident_f], w=[PB[0]])
                k.op("dve", lambda e: e.tensor_copy(out=eT[:], in_=PB[0][:, 0:128]), r=[PB[0]], w=[eT])
                k.op("dve", lambda e: e.memset(av[:], 0.0), w=[av])

            def u_step(ti, s_):
                u = Ug[s_ % 4]
                k.gather(u[:], sc["u16_d"][:, :], ei[:, s_:s_ + 1], r=[ei, "u16_d"], w=[u])
                k.op("dve", lambda e: e.scalar_tensor_tensor(out=junkb[:], in0=u[:], scalar=1.0, in1=h2b[:], op0=ALU.mult, op1=ALU.mult,
                                                             accum_out=av[:, s_:s_ + 1]), r=[u, h2b], w=[junkb, ("av", s_)])

            def post(ti):
                k.op("act", lambda e: e.activation(out=wv_[:], in_=av[:], func=AF.Gelu), r=[av] + [("av", s_) for s_ in range(128)], w=[wv_])
                k.op("dve", lambda e: e.tensor_tensor(out=wv_[:], in0=wv_[:], in1=gt[:], op=ALU.mult), r=[wv_, gt], w=[wv_])
                k.op("pe", lambda e: e.transpose(out=PB[0][:, 0:128], in_=wv_[:], identity=ident_f[:]), r=[wv_, ident_f], w=[PB[0]])
                k.op("dve", lambda e: e.tensor_copy(out=Wall[:, 0:128 * 128:129], in_=PB[0][:, 0:128]), r=[PB[0]], w=[Wall])

            def v_step(ti, t_):
                u = Vg[t_ % 4]
                eT = eiT2[ti % 2]
                k.gather(u[:], sc["v16_d"][:, :], eT[:, t_:t_ + 1], r=[eT, "v16_d"], w=[u])
                for nb in range(8):
                    k.op("pe", lambda e: e.matmul(PB[nb][:], lhsT=Wall[:, t_ * 128:(t_ + 1) * 128], rhs=u[:, nb * 512:(nb + 1) * 512],
                                                  start=(t_ == 0), stop=(t_ == 127)), r=[Wall, u], w=[PB[nb]])

            def evac(ti):
                rows = slice(ti * 128, (ti + 1) * 128)
                for nb in range(8):
                    cs = slice(nb * 512, (nb + 1) * 512)
                    xc = x1c[nb % 2]
                    gc = g2c[nb % 2]
                    k.dma(xc[:], sc["x1_d"][rows, cs], r=["x1_d"], w=[xc])
                    k.dma(gc[:], modrow[0:1, 5 * D + nb * 512:5 * D + (nb + 1) * 512].partition_broadcast(128), r=["mod_d"], w=[gc])
                    k.op("dve", lambda e: e.tensor_tensor(out=h2f[:, cs], in0=PB[nb][:], in1=gc[:], op=ALU.mult), r=[PB[nb], gc, h2f], w=[("yo", nb)])
                    k.op("pool", lambda e: e.tensor_tensor(out=h2f[:, cs], in0=h2f[:, cs], in1=xc[:], op=ALU.add), r=[("yo", nb), xc], w=[("yo", nb)])
                k.dma(out[rows, :], h2f[:], r=[("yo", nb) for nb in range(8)], w=["out", h2f])

            prep(0)
            for s_ in range(128):
                u_step(0, s_)
            post(0)
            for ti in range(16):
                nxt = ti + 1 < 16
                if nxt:
                    prep(ti + 1)
                for s_ in range(128):
                    if nxt:
                        u_step(ti + 1, s_)
                    v_step(ti, s_)
                evac(ti)
                if nxt:
                    post(ti + 1)
        k.barrier()

    k.finish()
    es.close()
    return nc, k


def prep_core_inputs(inp, b, consts):
    f = lambda a: np.ascontiguousarray(a, dtype=np.float32)
    d = {}
    d["x"] = f(inp["x"][b])
    d["c_l"] = f(inp["c"][b].reshape(KC, 128).T)
    d["w_ada"] = f(inp["w_ada"][0])
    d["b_ada"] = f(inp["b_ada"][0].reshape(1, -1))
    d["g1_l"] = f(inp["norm1_g"][0].reshape(KC, 128).T)
    d["g2_l"] = f(inp["norm2_g"][0].reshape(KC, 128).T)
    d["g2_row"] = f(inp["norm2_g"][0].reshape(1, -1))
    d["w_in"] = f(inp["w_in"][0])
    d["w_out"] = f(inp["w_out"][0])
    d["w_pool"] = f(inp["w_pool"][0])
    d["pscale_l"] = f(inp["pool_scale"][0].reshape(8, 128).T)
    d["qkg_l"] = f(np.concatenate([inp["q_norm_g"][0][None, :], inp["k_norm_g"][0]], 0).T)
    d["pe_l"] = f(np.transpose(inp["cmp_pe"][0], (0, 2, 1)))
    d["cmp_w1"] = f(inp["cmp_w1"][0])
    d["cmp_w2"] = f(inp["cmp_w2"][0])
    d["w_pq"] = f(inp["w_pq"][0])
    d["peer_keys"] = f(inp["peer_keys"][0].reshape(16, 128, 128))
    d["peer_u"] = f(inp["peer_u"][0])
    d["peer_v"] = f(inp["peer_v"][0])
    d.update(consts)
    return d


def sim_deadlock(k):
    streams = {}
    for e in k.log:
        streams.setdefault(e[0], []).append(e)
    pc = {e: 0 for e in streams}
    sem = {}
    progress = True
    while progress:
        progress = False
        for e, lst in streams.items():
            while pc[e] < len(lst):
                _, kind, s, v = lst[pc[e]]
                if kind == "w":
                    if sem.get(s, 0) >= v:
                        pc[e] += 1
                        progress = True
                    else:
                        break
                else:
                    sem[s] = sem.get(s, 0) + v
                    pc[e] += 1
                    progress = True
    stuck = {e: (pc[e], len(l), l[pc[e]] if pc[e] < len(l) else None) for e, l in streams.items() if pc[e] < len(l)}
    return stuck, sem


_CACHE = {}


def kernel(**inputs):
    inp = {n: np.asarray(v) for n, v in inputs.items()}
    consts = make_consts()
    if "nc" not in _CACHE:
        _CACHE["nc"] = build()[0]
    nc = _CACHE["nc"]
    in_maps = [prep_core_inputs(inp, b, consts) for b in range(8)]
    res = run_bass_kernel_spmd(nc, in_maps, core_ids=list(range(8)))
    return np.stack([np.asarray(r["out"], dtype=np.float32) for r in res.results], axis=0)
```
